# Optimizing a Trainium2 kernel written in Bass

```python
import math
import jax, jax.numpy as jnp
from jax import lax
import numpy as np

D_MODEL = 1024
BATCH = 4
SEQ = 8192
DEPTH = 1

CHUNK = 64
N_MEM = 256
EPS = 1e-6
SB_HEADS = 8
SB_HEAD_DIM = 64
SB_WIDTH = SB_HEADS * SB_HEAD_DIM
SB_Q_BLOCK = 128
GDN_HEADS = 4
GDN_HEAD_DIM = 128
GDN_WIDTH = GDN_HEADS * GDN_HEAD_DIM
CONV_WIDTH = 4
X_HEADS = 4
X_HEAD_DIM = D_MODEL // X_HEADS
PEER_HEADS = 8
PEER_KEYS = 128
PEER_EXPERTS = PEER_KEYS * PEER_KEYS
PEER_TOPK = 16
PEER_QDIM = 256
PEER_TOKEN_BLOCK = 128
IN_SPLIT_SIZES = (SB_WIDTH, SB_WIDTH, SB_WIDTH, GDN_WIDTH, GDN_WIDTH, GDN_WIDTH, GDN_WIDTH,
                  GDN_HEADS, GDN_HEADS, D_MODEL, D_MODEL)
IN_COLS = 3 * SB_WIDTH + 4 * GDN_WIDTH + 2 * GDN_HEADS + 2 * D_MODEL

kernel_name = "hybrid_stickbreak_gdn_peer_block"


def rmsnorm(x, g):
    xf = x.astype(jnp.float32)
    y = xf * lax.rsqrt(jnp.mean(xf * xf, axis=-1, keepdims=True) + EPS)
    return (y * g.astype(jnp.float32)).astype(x.dtype)


def l2norm(x):
    xf = x.astype(jnp.float32)
    return xf * lax.rsqrt(jnp.sum(xf * xf, axis=-1, keepdims=True) + EPS)


def causal_dwconv(x, w):
    K = w.shape[0]
    S = x.shape[1]
    xp = jnp.pad(x, ((0, 0), (K - 1, 0), (0, 0)))
    return sum(xp[:, j:j + S] * w[j] for j in range(K))


def stick_breaking_attention(q, k, v):
    S, d = q.shape[2], q.shape[3]
    scale = d ** -0.5
    outs = []
    for i in range(S // SB_Q_BLOCK):
        q0 = i * SB_Q_BLOCK
        kv_len = q0 + SB_Q_BLOCK
        qb = q[:, :, q0:kv_len]
        kb = k[:, :, :kv_len]
        vb = v[:, :, :kv_len]
        z = jnp.einsum('bhqd,bhkd->bhqk', qb, kb).astype(jnp.float32) * scale
        qpos = q0 + jnp.arange(SB_Q_BLOCK)[:, None]
        kpos = jnp.arange(kv_len)[None, :]
        mask = kpos < qpos
        log_keep = jnp.where(mask, jax.nn.log_sigmoid(-z), 0.0)
        after = lax.cumsum(log_keep, axis=3, reverse=True) - log_keep
        a = jnp.where(mask, jnp.exp(jax.nn.log_sigmoid(z) + after), 0.0)
        outs.append(jnp.einsum('bhqk,bhkd->bhqd', a.astype(v.dtype), vb))
    return jnp.concatenate(outs, axis=2)


def gated_delta_rule(q, k, v, beta, g):
    B, S, H, dk = q.shape
    dv = v.shape[-1]
    N, C = S // CHUNK, CHUNK
    f32 = jnp.float32

    def chunk4(t):
        return jnp.moveaxis(t.astype(f32).reshape(B, N, C, H, t.shape[-1]), 3, 1)

    def chunk3(t):
        return jnp.moveaxis(t.astype(f32).reshape(B, N, C, H), 3, 1)

    q = chunk4(q) * (dk ** -0.5)
    k = chunk4(k)
    v = chunk4(v)
    beta = chunk3(beta)
    gc = jnp.cumsum(chunk3(g), axis=-1)
    idx = jnp.arange(C)
    incl = idx[:, None] >= idx[None, :]
    strict = idx[:, None] > idx[None, :]
    decay = jnp.exp(jnp.where(incl, gc[..., :, None] - gc[..., None, :], -jnp.inf))
    kb = k * beta[..., None]
    vb = v * beta[..., None]
    a = jnp.where(strict, jnp.einsum('bhncd,bhnmd->bhncm', kb, k) * decay, 0.0)
    eye = jnp.eye(C, dtype=f32)
    rhs = jnp.concatenate([vb, kb * jnp.exp(gc)[..., None]], axis=-1)
    sol = lax.linalg.triangular_solve(a + eye, rhs, left_side=True, lower=True, unit_diagonal=True)
    u = sol[..., :dv]
    w = sol[..., dv:]
    qk = jnp.einsum('bhncd,bhnmd->bhncm', q, k) * decay
    q_dec = q * jnp.exp(gc)[..., None]
    k_dec = k * jnp.exp(gc[..., -1:] - gc)[..., None]
    chunk_decay = jnp.exp(gc[..., -1])
    xs = tuple(jnp.moveaxis(t, 2, 0) for t in (q_dec, k_dec, u, w, qk, chunk_decay))

    def step(state, inp):
        qd, kd, u_n, w_n, qk_n, cd = inp
        v_new = u_n - jnp.einsum('bhcd,bhde->bhce', w_n, state)
        o = jnp.einsum('bhcd,bhde->bhce', qd, state) + jnp.einsum('bhcm,bhme->bhce', qk_n, v_new)
        state = state * cd[..., None, None] + jnp.einsum('bhcd,bhce->bhde', kd, v_new)
        return state, o

    state0 = jnp.zeros((B, H, dk, dv), f32)
    _, o = lax.scan(step, state0, xs)
    return jnp.transpose(o, (1, 0, 3, 2, 4)).reshape(B, S, H, dv)


def token_mixer(xn, w_in, gdn_conv, gdn_a_log, gdn_dt_bias, gdn_norm_g, w_sb_up, w_gdn_up, w_mix_out):
    B, S, _ = xn.shape
    proj = xn @ w_in
    offs = [int(o) for o in np.cumsum(IN_SPLIT_SIZES)[:-1]]
    (sb_q, sb_k, sb_v, g_q, g_k, g_v, g_z, g_b, g_a, gate_sb, gate_gdn) = jnp.split(proj, offs, axis=-1)

    def heads_sb(t):
        return t.reshape(B, S, SB_HEADS, SB_HEAD_DIM).transpose(0, 2, 1, 3)
    o_sb = stick_breaking_attention(heads_sb(sb_q), heads_sb(sb_k), heads_sb(sb_v))
    o_sb = o_sb.transpose(0, 2, 1, 3).reshape(B, S, SB_WIDTH)

    qkv = jax.nn.silu(causal_dwconv(jnp.concatenate([g_q, g_k, g_v], axis=-1), gdn_conv))
    cq, ck, cv = jnp.split(qkv, 3, axis=-1)
    cq = l2norm(cq.reshape(B, S, GDN_HEADS, GDN_HEAD_DIM))
    ck = l2norm(ck.reshape(B, S, GDN_HEADS, GDN_HEAD_DIM))
    cv = cv.reshape(B, S, GDN_HEADS, GDN_HEAD_DIM)
    beta = jax.nn.sigmoid(g_b.astype(jnp.float32))
    g = -jnp.exp(gdn_a_log.astype(jnp.float32)) * jax.nn.softplus(g_a.astype(jnp.float32) + gdn_dt_bias.astype(jnp.float32))
    o_g = gated_delta_rule(cq, ck, cv, beta, g).astype(xn.dtype)
    o_g = rmsnorm(o_g, gdn_norm_g) * jax.nn.silu(g_z.reshape(B, S, GDN_HEADS, GDN_HEAD_DIM))
    o_g = o_g.reshape(B, S, GDN_WIDTH)

    merged = jax.nn.sigmoid(gate_sb) * (o_sb @ w_sb_up) + jax.nn.sigmoid(gate_gdn) * (o_g @ w_gdn_up)
    return merged @ w_mix_out


def memory_cross_attention(hn, mn, w_cq, w_ckv, w_co):
    B, S, _ = hn.shape
    M = mn.shape[1]
    q = (hn @ w_cq).reshape(B, S, X_HEADS, X_HEAD_DIM)
    k, v = jnp.split(mn @ w_ckv, 2, axis=-1)
    k = k.reshape(B, M, X_HEADS, X_HEAD_DIM)
    v = v.reshape(B, M, X_HEADS, X_HEAD_DIM)
    s = jnp.einsum('bshd,bmhd->bhsm', q, k).astype(jnp.float32) * (X_HEAD_DIM ** -0.5)
    p = jax.nn.softmax(s, axis=-1).astype(v.dtype)
    o = jnp.einsum('bhsm,bmhd->bshd', p, v).reshape(B, S, D_MODEL)
    return o @ w_co


def peer(xn, w_pq, subkeys, peer_u, peer_v):
    B, S, D = xn.shape
    T = B * S
    xt = xn.reshape(T, D)
    q = (xt @ w_pq).reshape(T, PEER_HEADS, 2, PEER_QDIM // 2)
    s = jnp.einsum('thpd,hpnd->thpn', q, subkeys).astype(jnp.float32)
    top_s, top_i = lax.top_k(s, PEER_TOPK)
    cand_s = top_s[:, :, 0, :, None] + top_s[:, :, 1, None, :]
    cand_i = top_i[:, :, 0, :, None] * PEER_KEYS + top_i[:, :, 1, None, :]
    kk = PEER_TOPK * PEER_TOPK
    best_s, best_pos = lax.top_k(cand_s.reshape(T, PEER_HEADS, kk), PEER_TOPK)
    expert = jnp.take_along_axis(cand_i.reshape(T, PEER_HEADS, kk), best_pos, axis=-1)
    gate = jax.nn.softmax(best_s, axis=-1).astype(xn.dtype)
    nb = T // PEER_TOKEN_BLOCK

    def block(args):
        xb, eb, gb = args
        act = jax.nn.gelu(jnp.einsum('thkd,td->thk', peer_u[eb], xb), approximate=False) * gb
        return jnp.einsum('thk,thkd->td', act, peer_v[eb])

    out = lax.map(block, (xt.reshape(nb, PEER_TOKEN_BLOCK, D),
                          expert.reshape(nb, PEER_TOKEN_BLOCK, PEER_HEADS, PEER_TOPK),
                          gate.reshape(nb, PEER_TOKEN_BLOCK, PEER_HEADS, PEER_TOPK)))
    return out.reshape(B, S, D)


def setup_inputs(seed: int = 0) -> dict:
    key = jax.random.key(seed)
    ks = jax.random.split(key, 24)
    f32 = jnp.float32

    def nrm(k, shape, scale):
        return jax.random.normal(k, shape, f32) * scale

    def gain(k, shape):
        return 1.0 + 0.02 * jax.random.normal(k, shape, f32)

    L, D = DEPTH, D_MODEL
    a_log = jnp.log(jax.random.uniform(ks[5], (L, GDN_HEADS), f32, 1.0, 16.0))
    dt = jnp.exp(jax.random.uniform(ks[6], (L, GDN_HEADS), f32, math.log(1e-3), math.log(1e-1)))
    dt_bias = dt + jnp.log(-jnp.expm1(-dt))
    return {
        "x": nrm(ks[0], (BATCH, SEQ, D), 1.0),
        "mem": nrm(ks[1], (BATCH, N_MEM, D), 1.0),
        "g_mix": gain(ks[2], (L, D)),
        "w_in": nrm(ks[3], (L, D, IN_COLS), D ** -0.5),
        "gdn_conv": nrm(ks[4], (L, CONV_WIDTH, 3 * GDN_WIDTH), CONV_WIDTH ** -0.5),
        "gdn_a_log": a_log,
        "gdn_dt_bias": dt_bias,
        "gdn_norm_g": gain(ks[7], (L, GDN_HEAD_DIM)),
        "w_sb_up": nrm(ks[8], (L, SB_WIDTH, D), SB_WIDTH ** -0.5),
        "w_gdn_up": nrm(ks[9], (L, GDN_WIDTH, D), GDN_WIDTH ** -0.5),
        "w_mix_out": nrm(ks[10], (L, D, D), D ** -0.5),
        "g_cross": gain(ks[11], (L, D)),
        "g_mem": gain(ks[12], (L, D)),
        "w_cq": nrm(ks[13], (L, D, D), D ** -0.5),
        "w_ckv": nrm(ks[14], (L, D, 2 * D), D ** -0.5),
        "w_co": nrm(ks[15], (L, D, D), D ** -0.5),
        "g_peer": gain(ks[16], (L, D)),
        "w_pq": nrm(ks[17], (L, D, PEER_HEADS * PEER_QDIM), D ** -0.5),
        "peer_subkeys": nrm(ks[18], (L, PEER_HEADS, 2, PEER_KEYS, PEER_QDIM // 2), (PEER_QDIM // 2) ** -0.5),
        "peer_u": nrm(ks[19], (L, PEER_EXPERTS, D), D ** -0.5),
        "peer_v": nrm(ks[20], (L, PEER_EXPERTS, D), PEER_HEADS ** -0.5),
        "g_final": gain(ks[21], (D,)),
    }


def reference(x, mem, g_mix, w_in, gdn_conv, gdn_a_log, gdn_dt_bias, gdn_norm_g, w_sb_up, w_gdn_up,
              w_mix_out, g_cross, g_mem, w_cq, w_ckv, w_co, g_peer, w_pq, peer_subkeys, peer_u, peer_v,
              g_final):
    for l in range(DEPTH):
        x = x + token_mixer(rmsnorm(x, g_mix[l]), w_in[l], gdn_conv[l], gdn_a_log[l], gdn_dt_bias[l],
                            gdn_norm_g[l], w_sb_up[l], w_gdn_up[l], w_mix_out[l])
        x = x + memory_cross_attention(rmsnorm(x, g_cross[l]), rmsnorm(mem, g_mem[l]),
                                       w_cq[l], w_ckv[l], w_co[l])
        x = x + peer(rmsnorm(x, g_peer[l]), w_pq[l], peer_subkeys[l], peer_u[l], peer_v[l])
    return rmsnorm(x, g_final)
```

```python
import contextlib
import numpy as np
import concourse.bass as bass
import concourse.mybir as mybir
from concourse.bass_utils import run_bass_kernel_spmd

F32 = mybir.dt.float32
BF16 = mybir.dt.bfloat16
AF = mybir.ActivationFunctionType
ALU = mybir.AluOpType

D = 1024
SEQ = 8192
NOWN = 4096
EPS = 1e-6
ENGS = ("pe", "act", "dve", "pool", "sp")
NEG = -30000.0


class Op:
    __slots__ = ("eng", "fn", "idx", "dma", "deps", "waits", "signal", "rank",
                 "dma_sem", "dma_val")

    def __init__(self, eng, fn, idx, dma):
        self.eng = eng
        self.fn = fn
        self.idx = idx
        self.dma = dma
        self.deps = ()
        self.waits = None
        self.signal = False
        self.rank = 0
        self.dma_sem = -1
        self.dma_val = 0


NSEM = {"pe": 18, "act": 16, "dve": 20, "pool": 6, "sp": 0}
N_DMA_SEMS = 20
SEM_CAP = 2000


class SemState:
    def __init__(self, nc):
        self.c = {e: [nc.alloc_semaphore(name=f"c_{e}_{i}") for i in range(NSEM[e])] for e in ENGS}
        self.d = [nc.alloc_semaphore(name=f"d_{i}") for i in range(N_DMA_SEMS)]
        self.rank = {e: 0 for e in ENGS}
        self.ndma = 0
        for e in ENGS:
            for h in self.c[e]:
                nc.gpsimd.sem_clear(h)
        for h in self.d:
            nc.gpsimd.sem_clear(h)
        nc.all_engine_barrier()


class Prog:
    def __init__(self, nc, sems):
        self.nc = nc
        self.sems = sems
        self.ops = {e: [] for e in ENGS}
        self.last_writer = {}
        self.readers = {}
        self.n_dma_sems = N_DMA_SEMS
        self.dma_ops = []
        self.dma_base = sems.ndma

    def add(self, eng, fn, reads=(), writes=(), dma=False):
        op = Op(eng, fn, len(self.ops[eng]), dma)
        psk = [k for k in reads if k.startswith("ps")]
        if psk:
            reads = [k for k in reads if not k.startswith("ps")]
            writes = list(writes) + [k for k in psk if k not in writes]
        deps = set()
        lw_get = self.last_writer.get
        for k in reads:
            lw = lw_get(k)
            if lw is not None:
                deps.add(lw)
        for k in writes:
            lw = lw_get(k)
            if lw is not None:
                deps.add(lw)
            rs = self.readers.get(k)
            if rs:
                deps.update(rs)
        for k in reads:
            self.readers.setdefault(k, []).append(op)
        for k in writes:
            self.last_writer[k] = op
            self.readers[k] = []
        if dma:
            jl = len(self.dma_ops)
            j = self.dma_base + jl
            op.dma_sem = j % self.n_dma_sems
            op.dma_val = 16 * (j // self.n_dma_sems + 1)
            if jl >= self.n_dma_sems:
                deps.add(self.dma_ops[jl - self.n_dma_sems])
            self.dma_ops.append(op)
        deps.discard(op)
        op.deps = tuple(deps)
        self.ops[eng].append(op)
        return op

    def dma(self, eng, out, in_, reads, writes, **kw):
        return self.add(eng, lambda e: e.dma_start(out=out, in_=in_, **kw),
                        reads, writes, dma=True)

    def finish(self):
        deps = set(self.last_writer.values())
        for rs in self.readers.values():
            deps.update(rs)
        deps.update(self.dma_ops[-self.n_dma_sems:])
        for e in ENGS:
            op = Op(e, lambda eng: None, len(self.ops[e]), False)
            op.deps = tuple(deps)
            self.ops[e].append(op)

    def emit(self):
        nc = self.nc
        for e in ENGS:
            seen = {}
            for op in self.ops[e]:
                waits = []
                for p in sorted(op.deps, key=lambda q: -q.idx):
                    if p.dma:
                        key = ("d", p.dma_sem)
                        if seen.get(key, 0) >= p.dma_val:
                            continue
                        seen[key] = p.dma_val
                        waits.append(p)
                    else:
                        if p.eng == e:
                            if e == "pe" or e == "sp":
                                continue
                            if op.idx - p.idx > 1:
                                continue
                        key = ("c", p.eng)
                        if seen.get(key, -1) >= p.idx:
                            continue
                        seen[key] = p.idx
                        p.signal = True
                        waits.append(p)
                op.waits = waits
        for e in ENGS:
            r = self.sems.rank[e]
            for op in self.ops[e]:
                if op.signal and not op.dma:
                    r += 1
                    op.rank = r
            self.sems.rank[e] = r
            assert r <= NSEM[e] * SEM_CAP, (e, r)
        self.sems.ndma += len(self.dma_ops)
        csems = self.sems.c
        dsems = self.sems.d
        with contextlib.ExitStack() as st:
            block = st.enter_context(nc.Block())

            def run(e):
                def body(eng):
                    for op in self.ops[e]:
                        for p in op.waits:
                            if p.dma:
                                eng.wait_ge(dsems[p.dma_sem], p.dma_val)
                            else:
                                k = (p.rank - 1) // SEM_CAP
                                eng.wait_ge(csems[p.eng][k], (p.rank - 1) % SEM_CAP + 1)
                        ins = op.fn(eng)
                        if ins is None:
                            assert not op.signal and not op.dma
                        elif op.dma:
                            ins.then_inc(dsems[op.dma_sem], 16)
                        elif op.signal:
                            k = (op.rank - 1) // SEM_CAP
                            ins.then_inc(csems[e][k], 1)
                return body

            block.tensor(run("pe"))
            block.scalar(run("act"))
            block.vector(run("dve"))
            block.gpsimd(run("pool"))
            block.sync(run("sp"))


class Phase:
    _cnt = [0]
    _sem = {}

    def __init__(self, nc):
        self.nc = nc
        if id(nc) not in Phase._sem:
            Phase._sem.clear()
            Phase._sem[id(nc)] = SemState(nc)
        Phase._cnt[0] += 1
        self.pid = Phase._cnt[0]

    def __enter__(self):
        self.st = contextlib.ExitStack()
        self.P = Prog(self.nc, Phase._sem[id(self.nc)])
        self.n = 0
        return self

    def sb(self, shape, dtype):
        self.n += 1
        return self.st.enter_context(self.nc.sbuf_tensor(f"t{self.pid}_{self.n}", list(shape), dtype))

    def ps(self, shape=(128, 512), dtype=F32):
        self.n += 1
        return self.st.enter_context(self.nc.psum_tensor(f"p{self.pid}_{self.n}", list(shape), dtype))

    def __exit__(self, *a):
        if a[0] is None:
            self.P.finish()
            self.P.emit()
        self.st.close()
        return False

    def mm(self, out, lhsT, rhs, start, stop, r, w):
        self.P.add("pe", lambda e: e.matmul(out, lhsT=lhsT, rhs=rhs, start=start, stop=stop), r, w)

    def act(self, out, in_, func, r, w, eng="act", **kw):
        self.P.add(eng, lambda e: e.activation(out=out, in_=in_, func=func, **kw), r, w)

    def copy(self, eng, out, in_, r, w):
        if eng == "act":
            self.P.add("act", lambda e: e.copy(out=out, in_=in_), r, w)
        else:
            self.P.add(eng, lambda e: e.tensor_copy(out=out, in_=in_), r, w)

    def tt(self, eng, out, in0, in1, op, r, w):
        self.P.add(eng, lambda e: e.tensor_tensor(out=out, in0=in0, in1=in1, op=op), r, w)

    def ts(self, eng, out, in0, s1, op0, r, w, s2=None, op1=None):
        if op1 is None:
            self.P.add(eng, lambda e: e.tensor_scalar(out=out, in0=in0, scalar1=s1, scalar2=None, op0=op0), r, w)
        else:
            self.P.add(eng, lambda e: e.tensor_scalar(out=out, in0=in0, scalar1=s1, scalar2=s2, op0=op0, op1=op1), r, w)

    def stt(self, out, in0, scalar, in1, op0, op1, r, w):
        self.P.add("dve", lambda e: e.scalar_tensor_tensor(out=out, in0=in0, scalar=scalar, in1=in1, op0=op0, op1=op1), r, w)

    def recip(self, out, in_, r, w):
        self.P.add("dve", lambda e: e.reciprocal(out=out, in_=in_), r, w)

    def memset(self, eng, ap, val, w):
        self.P.add(eng, lambda e: e.memset(ap, val), (), w)

    def dma(self, eng, out, in_, r, w):
        self.P.dma(eng, out, in_, r, w)


def fm(ap, t0, n):
    return ap.rearrange("(c p) t -> p c t", p=128)[:, :, t0:t0 + n]


def norm_tile(ph, xt, sq, xn, gcol, ones32, ps_stat, rt, slot, key, nch=8, width=512):
    for c in range(nch):
        ph.act(sq[:, c, :], xt[:, c, :], AF.Square, [f"xt{slot}"], ["sq"])
    pk = key or "ps_stat"
    for c in range(nch):
        ph.mm(ps_stat[:, 0:width], ones32[:], sq[:, c, :], c == 0, c == nch - 1, ["sq", "ones32"], [pk])
    ph.act(rt[:, 0:width], ps_stat[:, 0:width], AF.Sqrt, [pk], ["rt"], bias=EPS, scale=1.0 / (128 * nch))
    ph.recip(rt[:, 0:width], rt[:, 0:width], ["rt"], ["rt"])
    for c in range(nch):
        ph.stt(xn[:, c, :], xt[:, c, :], gcol[:, c:c + 1], rt[:, 0:width], ALU.mult, ALU.mult,
               [f"xt{slot}", "rt", "gcol"], [f"xn{slot}"])


def phase_inproj_ctx(nc, xT, g_mix, w_fm, w_tm, convw, alog_bc, dtb_bc,
                     kT_scr, v_scr, gq_scr, gk_scr, gv_scr, gb_scr):
    NT = SEQ // 512
    with Phase(nc) as ph:
        wfm = ph.sb([128, 8, 2048], BF16)
        wtm = ph.sb([128, 8, 520], BF16)
        gcol = ph.sb([128, 8], F32)
        cw = ph.sb([128, 12, 4], F32)
        alog = ph.sb([128, 4], F32)
        dtb = ph.sb([128, 4], F32)
        ones32 = ph.sb([128, 128], F32)
        xt = [ph.sb([128, 8, 512], F32) for _ in range(2)]
        sq = ph.sb([128, 8, 512], F32)
        xn = [ph.sb([128, 8, 512], BF16) for _ in range(2)]
        rt = ph.sb([128, 512], F32)
        pre = [ph.sb([128, 515], F32) for _ in range(12)]
        acc = [ph.sb([128, 512], F32) for _ in range(2)]
        sil = [ph.sb([128, 512], F32) for _ in range(2)]
        sq2 = [ph.sb([128, 512], F32) for _ in range(2)]
        rt2 = [ph.sb([128, 512], F32) for _ in range(2)]
        kst = [ph.sb([128, 512], BF16) for _ in range(2)]
        vst = [ph.sb([128, 512], BF16) for _ in range(2)]
        bat = [ph.sb([128, 8], F32) for _ in range(2)]
        ps_stat = ph.ps()
        ps_f = [ph.ps() for _ in range(2)]
        ps_n = [ph.ps() for _ in range(2)]
        ps_t = [ph.ps() for _ in range(2)]

        for q in range(4):
            ph.dma("pool", wfm[:, 2 * q:2 * q + 2, :], fm(w_fm, 0, 2048)[:, 2 * q:2 * q + 2, :], ["w_fm"], ["wfm"])
        ph.dma("pool", wtm[:], fm(w_tm, 0, 520), ["w_tm"], ["wtm"])
        ph.dma("sp", gcol[:], g_mix, [], ["gcol"])
        ph.dma("sp", cw[:], convw.rearrange("(f p) j -> p f j", p=128), [], ["cw"])
        ph.dma("sp", alog[:], alog_bc, [], ["alog"])
        ph.dma("sp", dtb[:], dtb_bc, [], ["dtb"])
        ph.memset("dve", ones32[:], 1.0, ["ones32"])
        for f in range(12):
            ph.memset("pool", pre[f][:, 0:3], 0.0, [f"pre{f}"])
        ph.act(alog[:], alog[:], AF.Exp, ["alog"], ["alog"])

        nfe = 0
        for ti in range(NT):
            s = ti % 2
            t0 = ti * 512
            ph.dma("sp", xt[s][:, 0:4, :], fm(xT, t0, 512)[:, 0:4, :], [], [f"xt{s}"])
            ph.dma("sp", xt[s][:, 4:8, :], fm(xT, t0, 512)[:, 4:8, :], [], [f"xt{s}"])
            norm_tile(ph, xt[s], sq, xn[s], gcol, ones32, ps_stat, rt, s, None)
            for f in range(16):
                pb = ps_f[f % 2]
                pk = f"ps_f{f % 2}"
                for c in range(8):
                    ph.mm(pb[:], wfm[:, c, f * 128:(f + 1) * 128], xn[s][:, c, :], c == 0, c == 7,
                          ["wfm", f"xn{s}"], [pk])
                if f < 4:
                    b = nfe % 2
                    nfe += 1
                    ph.copy("act", kst[b][:], pb[:], [pk], [f"kst{b}"])
                    ph.dma("sp", kT_scr[f * 128:(f + 1) * 128, t0:t0 + 512], kst[b][:], [f"kst{b}"], ["kT_scr"])
                    continue
                g = f - 4
                b = g % 2
                if ti > 0:
                    ph.copy("pool", pre[g][:, 0:3], pre[g][:, 512:515], [f"pre{g}"], [f"pre{g}"])
                ph.copy("act", pre[g][:, 3:515], pb[:], [pk, f"pre{g}"], [f"pre{g}"])
                ph.ts("dve", acc[b][:], pre[g][:, 0:512], cw[:, g, 0:1], ALU.mult, [f"pre{g}", "cw"], [f"acc{b}"])
                for j in range(1, 4):
                    ph.stt(acc[b][:], pre[g][:, j:j + 512], cw[:, g, j:j + 1], acc[b][:], ALU.mult, ALU.add,
                           [f"pre{g}", "cw", f"acc{b}"], [f"acc{b}"])
                ph.act(sil[b][:], acc[b][:], AF.Silu, [f"acc{b}"], [f"sil{b}"])
                if g < 8:
                    pn = ps_n[b]
                    ph.act(sq2[b][:], sil[b][:], AF.Square, [f"sil{b}"], [f"sq2{b}"])
                    ph.mm(pn[:], ones32[:], sq2[b][:], True, True, ["ones32", f"sq2{b}"], [f"ps_n{b}"])
                    ph.act(rt2[b][:], pn[:], AF.Sqrt, [f"ps_n{b}"], [f"rt2{b}"], bias=EPS, scale=1.0)
                    ph.recip(rt2[b][:], rt2[b][:], [f"rt2{b}"], [f"rt2{b}"])
                    scl = 128 ** -0.5 if g < 4 else 1.0
                    ph.stt(sil[b][:], sil[b][:], scl, rt2[b][:], ALU.mult, ALU.mult,
                           [f"sil{b}", f"rt2{b}"], [f"sil{b}"])
                dst = (gq_scr, gk_scr, gv_scr)[g // 4]
                hh = g % 4
                ph.dma("sp", dst[hh * 128:(hh + 1) * 128, t0:t0 + 512], sil[b][:], [f"sil{b}"], ["g_scr"])
            for u in range(4):
                b = u % 2
                pt = ps_t[b]
                pk = f"ps_t{b}"
                for c in range(8):
                    ph.mm(pt[:], xn[s][:, c, u * 128:(u + 1) * 128], wtm[:, c, 0:512], c == 0, c == 7,
                          [f"xn{s}", "wtm"], [pk])
                ph.copy("act", vst[b][:], pt[:], [pk], [f"vst{b}"])
                ph.dma("sp", v_scr[t0 + u * 128:t0 + (u + 1) * 128, :], vst[b][:], [f"vst{b}"], ["v_scr"])
                pn = ps_n[b]
                pnk = f"ps_n{b}"
                for c in range(8):
                    ph.mm(pn[:, 0:8], xn[s][:, c, u * 128:(u + 1) * 128], wtm[:, c, 512:520], c == 0, c == 7,
                          [f"xn{s}", "wtm"], [pnk])
                ph.act(bat[b][:, 0:4], pn[:, 0:4], AF.Sigmoid, [pnk], [f"bat{b}"])
                ph.tt("dve", bat[b][:, 4:8], pn[:, 4:8], dtb[:], ALU.add, [pnk, "dtb"], [f"bat{b}"])
                ph.act(bat[b][:, 4:8], bat[b][:, 4:8], AF.Exp, [f"bat{b}"], [f"bat{b}"])
                ph.act(bat[b][:, 4:8], bat[b][:, 4:8], AF.Ln, [f"bat{b}"], [f"bat{b}"], bias=1.0)
                ph.stt(bat[b][:, 4:8], bat[b][:, 4:8], -1.0, alog[:], ALU.mult, ALU.mult,
                       [f"bat{b}", "alog"], [f"bat{b}"])
                ph.dma("sp", gb_scr[t0 + u * 128:t0 + (u + 1) * 128, :], bat[b][:], [f"bat{b}"], ["gb_scr"])


def phase_inproj_own(nc, xT, g_mix, w_own, qT_scr, zT_scr, gate_scr):
    NT = NOWN // 512
    with Phase(nc) as ph:
        wfm = ph.sb([128, 8, 3072], BF16)
        gcol = ph.sb([128, 8], F32)
        ones32 = ph.sb([128, 128], F32)
        xt = [ph.sb([128, 8, 512], F32) for _ in range(2)]
        sq = ph.sb([128, 8, 512], F32)
        xn = [ph.sb([128, 8, 512], BF16) for _ in range(2)]
        rt = ph.sb([128, 512], F32)
        st16 = [ph.sb([128, 512], BF16) for _ in range(3)]
        st32 = [ph.sb([128, 512], F32) for _ in range(2)]
        ps_stat = ph.ps()
        ps_f = [ph.ps() for _ in range(3)]
        for q in range(8):
            ph.dma("pool", wfm[:, q:q + 1, :], fm(w_own, 0, 3072)[:, q:q + 1, :], ["w"], ["wfm"])
        ph.dma("sp", gcol[:], g_mix, [], ["gcol"])
        ph.memset("dve", ones32[:], 1.0, ["ones32"])
        n16 = 0
        n32 = 0
        for ti in range(NT):
            s = ti % 2
            t0 = ti * 512
            ph.dma("sp", xt[s][:, 0:4, :], fm(xT, t0, 512)[:, 0:4, :], [], [f"xt{s}"])
            ph.dma("sp", xt[s][:, 4:8, :], fm(xT, t0, 512)[:, 4:8, :], [], [f"xt{s}"])
            norm_tile(ph, xt[s], sq, xn[s], gcol, ones32, ps_stat, rt, s, None)
            for f in range(24):
                pb = ps_f[f % 3]
                pk = f"ps_f{f % 3}"
                for c in range(8):
                    ph.mm(pb[:], wfm[:, c, f * 128:(f + 1) * 128], xn[s][:, c, :], c == 0, c == 7,
                          ["wfm", f"xn{s}"], [pk])
                if f < 4:
                    b = n16 % 3
                    n16 += 1
                    ph.act(st16[b][:], pb[:], AF.Copy, [pk], [f"s16{b}"], scale=0.125)
                    ph.dma("sp", qT_scr[f * 128:(f + 1) * 128, t0:t0 + 512], st16[b][:], [f"s16{b}"], ["qT_scr"])
                elif f < 8:
                    b = n32 % 2
                    n32 += 1
                    ph.act(st32[b][:], pb[:], AF.Silu, [pk], [f"s32{b}"])
                    ph.dma("sp", zT_scr[(f - 4) * 128:(f - 3) * 128, t0:t0 + 512], st32[b][:], [f"s32{b}"], ["zT_scr"])
                else:
                    b = n16 % 3
                    n16 += 1
                    ph.act(st16[b][:], pb[:], AF.Sigmoid, [pk], [f"s16{b}"])
                    ph.dma("sp", gate_scr[(f - 8) * 128:(f - 7) * 128, t0:t0 + 512], st16[b][:], [f"s16{b}"], ["gate_scr"])


C_LOW, C_LOWS, C_UP, C_ID, C_TRI = 0, 128, 256, 384, 512


def phase_sb(nc, qT_scr, kT_scr, v_scr, masks, cst, osb_scr, nj=8):
    with Phase(nc) as ph:
        kT = ph.sb([128, SEQ], BF16)
        vv = ph.sb([128, 64, 128], BF16)
        qT = ph.sb([128, NOWN], BF16)
        mk = ph.sb([128, 16, 512], F32)
        tri = ph.sb([128, 128], BF16)
        negones = ph.sb([128, 128], BF16)
        Ls = [ph.sb([128, 512], BF16) for _ in range(2)]
        zm = [ph.sb([128, 512], F32) for _ in range(2)]
        ee = [ph.sb([128, 512], F32) for _ in range(3)]
        LL = [ph.sb([128, 512], BF16) for _ in range(3)]
        eR = [ph.sb([128, 512], F32) for _ in range(2)]
        aa = [ph.sb([128, 512], BF16) for _ in range(3)]
        ost = [ph.sb([64, 512], BF16) for _ in range(2)]
        ps_z = [ph.ps() for _ in range(2)]
        ps_r = [ph.ps() for _ in range(2)]
        ps_o = [ph.ps() for _ in range(2)]

        ph.dma("sp", mk[:, 0:8, :], masks[:, 0:8, :], [], ["mk"])
        ph.dma("sp", mk[:, 8:16, :], masks[:, 8:16, :], [], ["mk"])
        ph.dma("pool", tri[:], cst[:, C_TRI:C_TRI + 128], [], ["tri"])
        ph.memset("dve", negones[:], -1.0, ["negones"])
        it = 0
        nev = 0
        for hp in range(4):
            for q in range(4):
                ph.dma("sp", kT[:, q * 2048:(q + 1) * 2048], kT_scr[hp * 128:(hp + 1) * 128, q * 2048:(q + 1) * 2048],
                       ["kT_scr"], ["kT"])
                ph.dma("sp", vv[:, q * 16:(q + 1) * 16, :],
                       v_scr.rearrange("(n p) f -> p n f", p=128)[:, q * 16:(q + 1) * 16, hp * 128:(hp + 1) * 128],
                       ["v_scr"], ["vv"])
            ph.dma("sp", qT[:], qT_scr[hp * 128:(hp + 1) * 128, :], ["qT_scr"], ["qT"])
            for j in range(nj):
                for hh in range(2):
                    h = 2 * hp + hh
                    pr = slice(hh * 64, hh * 64 + 64)
                    nblk = 8 * (j + 1)
                    po = ps_o[hh]
                    pok = f"ps_o{hh}"
                    pend = None
                    for bi in range(nblk):
                        kb = nblk - 1 - bi
                        i2, i3 = it % 2, it % 3
                        it += 1
                        pz, pzk = ps_z[i2], f"ps_z{i2}"
                        prr, prk = ps_r[i2], f"ps_r{i2}"
                        ph.mm(pz[:], kT[pr, kb * 128:(kb + 1) * 128], qT[pr, j * 512:(j + 1) * 512], True, True,
                              ["kT", "qT"], [pzk])
                        if kb >= nblk - 8:
                            unit = (kb - (nblk - 8)) // 4
                            midx = (j % 2) * 8 + unit * 4 + (kb % 4)
                            ph.tt("dve", zm[i2][:], pz[:], mk[:, midx, :], ALU.add, [pzk, "mk"], [f"zm{i2}"])
                            ph.act(ee[i3][:], zm[i2][:], AF.Exp, [f"zm{i2}"], [f"ee{i3}"])
                        else:
                            ph.act(ee[i3][:], pz[:], AF.Exp, [pzk], [f"ee{i3}"])
                        if pend is not None:
                            pend[0]()
                        ph.act(LL[i3][:], ee[i3][:], AF.Ln, [f"ee{i3}"], [f"LL{i3}"], bias=1.0)
                        ph.mm(prr[:], tri[:], LL[i3][:], True, bi == 0, ["tri", f"LL{i3}"], [prk])
                        if bi > 0:
                            ph.mm(prr[:], negones[:], Ls[(bi - 1) % 2][:], False, True,
                                  ["negones", f"Ls{(bi - 1) % 2}"], [prk])
                        if bi == 0:
                            ph.copy("dve", Ls[0][:], LL[i3][:], [f"LL{i3}"], ["Ls0"])
                        elif bi < nblk - 1:
                            ph.tt("dve", Ls[bi % 2][:], Ls[(bi - 1) % 2][:], LL[i3][:], ALU.add,
                                  [f"Ls{(bi - 1) % 2}", f"LL{i3}"], [f"Ls{bi % 2}"])
                        if pend is not None:
                            pend[1]()

                        def back1(i2=i2, prr=prr, prk=prk):
                            ph.act(eR[i2][:], prr[:], AF.Exp, [prk], [f"eR{i2}"])

                        def back2(i2=i2, i3=i3, kb=kb, bi=bi):
                            ph.tt("dve", aa[i3][:], ee[i3][:], eR[i2][:], ALU.mult, [f"ee{i3}", f"eR{i2}"], [f"aa{i3}"])
                            ph.mm(po[0:64, :], vv[:, kb, hh * 64:(hh + 1) * 64], aa[i3][:], bi == 0, bi == nblk - 1,
                                  ["vv", f"aa{i3}"], [pok])
                        pend = (back1, back2)
                    pend[0]()
                    pend[1]()
                    b = nev % 2
                    nev += 1
                    ph.copy("act", ost[b][:], po[0:64, :], [pok], [f"ost{b}"])
                    ph.dma("sp", osb_scr[h * 64:(h + 1) * 64, j * 512:(j + 1) * 512], ost[b][:], [f"ost{b}"], ["osb_scr"])


def own_tiles(p):
    return [2 * (j // 2) * 2 + (0 if (j % 2 == 0) == (p == 0) else 0) for j in range(8)]


def tile_ids(p):
    out = []
    for j in range(8):
        if (j + p) % 2 == 0:
            out.append(2 * j)
        else:
            out.append(2 * j + 1)
    return out


def build(stop_after=99, debug=False, nj=8, nchunks=64, skip=()):
    nc = bass.Bass("TRN2", target_bir_lowering=False)

    def inp(name, shape, dt=F32):
        return nc.dram_tensor(name, list(shape), dt, kind="ExternalInput").ap()

    def scr(name, shape, dt=F32):
        if debug:
            return nc.dram_tensor(name, list(shape), dt, kind="ExternalOutput").ap()
        return nc.dram_tensor(name, list(shape), dt).ap()

    I = {}
    I["xT_ctx"] = inp("xT_ctx", [D, SEQ])
    I["xT_own"] = inp("xT_own", [D, NOWN])
    I["g_mix"] = inp("g_mix", [128, 8])
    I["w_ctx_fm"] = inp("w_ctx_fm", [D, 2048])
    I["w_ctx_tm"] = inp("w_ctx_tm", [D, 520])
    I["w_own"] = inp("w_own", [D, 3072])
    I["convw"] = inp("convw", [1536, 4])
    I["alog_bc"] = inp("alog_bc", [128, 4])
    I["dtb_bc"] = inp("dtb_bc", [128, 4])
    I["masks"] = inp("masks", [128, 16, 512])
    I["cst"] = inp("cst", [128, 640])
    I["memT"] = inp("memT", [D, 256])
    I["sel"] = inp("sel", [128, 16])
    I["gng"] = inp("gng", [128, 1])
    I["g_cross"] = inp("g_cross", [128, 8])
    I["g_mem"] = inp("g_mem", [128, 8])
    I["g_peer"] = inp("g_peer", [128, 8])
    I["g_final"] = inp("g_final", [128, 8])
    I["w_sb_up"] = inp("w_sb_up", [512, D])
    I["w_gdn_up"] = inp("w_gdn_up", [512, D])
    I["w_mix_out"] = inp("w_mix_out", [D, D])
    I["w_cq"] = inp("w_cq", [D, D])
    I["w_ckv"] = inp("w_ckv", [D, 2 * D])
    I["w_co"] = inp("w_co", [D, D])
    I["w_pq"] = inp("w_pq", [D, 2048])
    I["skT"] = inp("skT", [128, 16, 128])
    I["sel48"] = inp("sel48", [48, 16, 128])
    I["uT"] = inp("uT", [D, 16384])
    I["pv"] = inp("pv", [16384, D])
    outT = nc.dram_tensor("outT", [D, NOWN], F32, kind="ExternalOutput").ap()

    S = {}
    S["kT"] = scr("kT_scr", [512, SEQ], BF16)
    S["v"] = scr("v_scr", [SEQ, 512], BF16)
    S["gq"] = scr("gq_scr", [512, SEQ])
    S["gk"] = scr("gk_scr", [512, SEQ])
    S["gv"] = scr("gv_scr", [512, SEQ])
    S["gb"] = scr("gb_scr", [SEQ, 8])
    S["qT"] = scr("qT_scr", [512, NOWN], BF16)
    S["zT"] = scr("zT_scr", [512, NOWN])
    S["gate"] = scr("gate_scr", [2048, NOWN], BF16)
    S["osb"] = scr("osb_scr", [512, NOWN], BF16)
    S["og"] = scr("og_scr", [512, SEQ])
    S["x2"] = scr("x2_scr", [D, NOWN])
    if debug:
        S["x1"] = scr("x1_scr", [D, NOWN])

    if stop_after >= 1 and 1 not in skip:
        phase_inproj_ctx(nc, I["xT_ctx"], I["g_mix"], I["w_ctx_fm"], I["w_ctx_tm"], I["convw"],
                         I["alog_bc"], I["dtb_bc"], S["kT"], S["v"], S["gq"], S["gk"], S["gv"], S["gb"])
    if stop_after >= 2 and 2 not in skip:
        phase_inproj_own(nc, I["xT_own"], I["g_mix"], I["w_own"], S["qT"], S["zT"], S["gate"])
    if stop_after >= 3 and 3 not in skip:
        phase_sb(nc, S["qT"], S["kT"], S["v"], I["masks"], I["cst"], S["osb"], nj=nj)
    if stop_after >= 4 and 4 not in skip:
        phase_gdn(nc, S["gq"], S["gk"], S["gv"], S["gb"], I["cst"], S["og"], nchunks=nchunks)
    if stop_after >= 5:
        phase_merge_cross(nc, I["xT_own"], I["memT"], S["og"], S["osb"], S["zT"], S["gate"], I["sel"], I["gng"],
                          I["g_cross"], I["g_mem"], I["w_sb_up"], I["w_gdn_up"], I["w_mix_out"], I["w_cq"],
                          I["w_ckv"], I["w_co"], S["x2"], x1_dbg=S["x1"] if debug else None)
    if stop_after >= 6:
        phase_peer(nc, S["x2"], I["g_peer"], I["g_final"], I["w_pq"], I["skT"], I["sel48"], I["cst"],
                   I["uT"], I["pv"], outT)
    return nc


def make_consts():
    i = np.arange(128)
    low = (i[:, None] >= i[None, :]).astype(np.float32)
    lows = (i[:, None] > i[None, :]).astype(np.float32)
    up = (i[:, None] <= i[None, :]).astype(np.float32)
    ident = np.eye(128, dtype=np.float32)
    return np.concatenate([low, lows, up, ident, -low], axis=1)


def make_masks(p):
    m = np.zeros((128, 2, 2, 4, 512), np.float32)
    k = np.arange(128)[:, None]
    q = np.arange(512)[None, :]
    diag = np.stack([np.where(kk * 128 + k < q, 0.0, NEG) for kk in range(4)], 0)
    full = np.zeros((4, 128, 512), np.float32)
    zero = np.full((4, 128, 512), NEG, np.float32)
    for jp in range(2):
        if (p + jp) % 2 == 0:
            u0, u1 = diag, zero
        else:
            u0, u1 = full, diag
        m[:, jp, 0] = u0.transpose(1, 0, 2)
        m[:, jp, 1] = u1.transpose(1, 0, 2)
    return np.ascontiguousarray(m.reshape(128, 16, 512))


def prepare(inputs):
    f = lambda a: np.ascontiguousarray(np.asarray(a, dtype=np.float32))
    x = f(inputs["x"])
    w_in = f(inputs["w_in"])[0]
    o = np.cumsum([0, 512, 512, 512, 512, 512, 512, 512, 4, 4, 1024, 1024])
    col = lambda i: w_in[:, o[i]:o[i + 1]]
    w_ctx_fm = f(np.concatenate([col(1), col(3), col(4), col(5)], 1))
    w_ctx_tm = f(np.concatenate([col(2), col(7), col(8)], 1))
    w_own = f(np.concatenate([col(0), col(6), col(9), col(10)], 1))
    pc8 = lambda a: f(f(a)[0].reshape(8, 128).T)
    mem = f(inputs["mem"])
    shared = dict(
        g_mix=f(f(inputs["g_mix"])[0].reshape(8, 128).T), w_ctx_fm=w_ctx_fm, w_ctx_tm=w_ctx_tm, w_own=w_own,
        convw=f(f(inputs["gdn_conv"])[0].T),
        alog_bc=f(np.broadcast_to(f(inputs["gdn_a_log"])[0][None, :], (128, 4))),
        dtb_bc=f(np.broadcast_to(f(inputs["gdn_dt_bias"])[0][None, :], (128, 4))),
        cst=make_consts(),
        gng=f(f(inputs["gdn_norm_g"])[0].reshape(128, 1)),
        g_cross=pc8(inputs["g_cross"]), g_mem=pc8(inputs["g_mem"]), g_peer=pc8(inputs["g_peer"]),
        g_final=f(f(inputs["g_final"]).reshape(8, 128).T),
        w_sb_up=f(inputs["w_sb_up"])[0], w_gdn_up=f(inputs["w_gdn_up"])[0], w_mix_out=f(inputs["w_mix_out"])[0],
        w_cq=f(inputs["w_cq"])[0], w_ckv=f(inputs["w_ckv"])[0], w_co=f(inputs["w_co"])[0],
        w_pq=f(inputs["w_pq"])[0],
        skT=f(f(inputs["peer_subkeys"])[0].transpose(3, 0, 1, 2).reshape(128, 16, 128)),
        sel48=f((np.arange(48)[:, None, None] % 16 == np.arange(16)[None, :, None]) * np.ones((1, 1, 128))),
        uT=f(f(inputs["peer_u"])[0].T), pv=f(inputs["peer_v"])[0],
    )
    in_maps = []
    for c in range(8):
        b, p = c // 2, c % 2
        tl = tile_ids(p)
        idx = np.concatenate([np.arange(t * 512, (t + 1) * 512) for t in tl])
        m = dict(shared)
        m["xT_ctx"] = f(x[b].T)
        m["xT_own"] = f(x[b][idx].T)
        m["masks"] = make_masks(p)
        m["memT"] = f(mem[b].T)
        cj = np.array([1.0 if (j + p) % 2 == 0 else 0.0 for j in range(8)], np.float32)
        m["sel"] = f(np.broadcast_to(np.concatenate([cj, 1.0 - cj])[None, :], (128, 16)))
        in_maps.append(m)
    return in_maps


def phase_gdn(nc, gq_scr, gk_scr, gv_scr, gb_scr, cst, og_scr, nchunks=64):
    with Phase(nc) as ph:
        cs = ph.sb([128, 640], F32)
        ones = ph.sb([128, 128], F32)
        gb = ph.sb([128, 64, 8], F32)
        nb = ph.sb([128, 64, 4], F32)
        qkv = [[ph.sb([128, 4, 512], F32) for _ in range(3)] for _ in range(2)]
        St = [ph.sb([128, 128], F32) for _ in range(4)]
        names = ["grep", "gT2", "decS", "decT", "egcb", "P0", "P1", "Q0", "Q1", "U0", "U1", "ktm", "vtm", "vb",
                 "kbg", "u", "wT", "qkT", "qdT", "kdec", "vnew", "ost"]
        bufs = [[{nm: ph.sb([128, 128], F32) for nm in names} for _ in range(2)] for _ in range(4)]
        cols = [[ph.sb([128, 8], F32) for _ in range(2)] for _ in range(4)]
        banks = [ph.ps() for _ in range(8)]
        LOW = cs[:, C_LOW:C_LOW + 128]
        LOWS = cs[:, C_LOWS:C_LOWS + 128]
        UP = cs[:, C_UP:C_UP + 128]
        IDN = cs[:, C_ID:C_ID + 128]
        pctr = [0]

        def pst():
            i = pctr[0] % 8
            pctr[0] += 1
            return banks[i][:, 0:128], f"ps{i}"

        ph.dma("sp", cs[:], cst, [], ["cs"])
        ph.dma("sp", gb[:], gb_scr.rearrange("(n p) f -> p n f", p=128), ["gb_scr"], ["gb"])
        ph.memset("dve", ones[:], 1.0, ["ones"])
        for h in range(4):
            ph.memset("pool", St[h][:], 0.0, [f"S{h}"])
        ph.ts("dve", nb[:], gb[:, :, 0:4], -1.0, ALU.mult, ["gb"], ["nb"])

        for n in range(nchunks):
            par = n % 2
            grp, gi = n // 4, n % 4
            gs = grp % 2
            if gi == 0:
                for ti, src in enumerate((gq_scr, gk_scr, gv_scr)):
                    ph.dma("sp", qkv[gs][ti][:], src.rearrange("(h p) t -> p h t", p=128)[:, :, grp * 512:(grp + 1) * 512],
                           ["g_scr"], [f"qkv{gs}{ti}"])
            tsl = slice(gi * 128, (gi + 1) * 128)
            qT = [qkv[gs][0][:, h, tsl] for h in range(4)]
            kT = [qkv[gs][1][:, h, tsl] for h in range(4)]
            vT = [qkv[gs][2][:, h, tsl] for h in range(4)]
            kq, kk_, kv = f"qkv{gs}0", f"qkv{gs}1", f"qkv{gs}2"
            B = [bufs[h][par] for h in range(4)]
            K = lambda nm, h: f"{nm}{h}{par}"
            CL = [cols[h][par] for h in range(4)]
            gcol = [gb[:, n, 4 + h:5 + h] for h in range(4)]
            bcol = [gb[:, n, h:h + 1] for h in range(4)]
            nbcol = [nb[:, n, h:h + 1] for h in range(4)]
            pd, pdT, pgb, pc, pkk, pqk = {}, {}, {}, {}, {}, {}
            for h in range(4):
                ph.ts("dve", B[h]["gT2"][:], LOWS, gcol[h], ALU.mult, ["cs", "gb"], [K("gT2", h)])
                ph.ts("dve", B[h]["grep"][:], ones[:], gcol[h], ALU.mult, ["ones", "gb"], [K("grep", h)])
                p1, k1 = pst()
                ph.mm(p1, kT[h], IDN, True, True, [kk_, "cs"], [k1])
                ph.copy("act", B[h]["ktm"][:], p1, [k1], [K("ktm", h)])
                p2, k2 = pst()
                ph.mm(p2, vT[h], IDN, True, True, [kv, "cs"], [k2])
                ph.copy("dve", B[h]["vtm"][:], p2, [k2], [K("vtm", h)])
            for h in range(4):
                x, xk = pst()
                ph.mm(x, UP, B[h]["gT2"][:], True, True, ["cs", K("gT2", h)], [xk])
                ph.act(B[h]["decS"][:], x, AF.Exp, [xk], [K("decS", h)])
                x, xk = pst()
                ph.mm(x, B[h]["gT2"][:], UP, True, True, ["cs", K("gT2", h)], [xk])
                ph.act(B[h]["decT"][:], x, AF.Exp, [xk], [K("decT", h)])
                x, xk = pst()
                ph.mm(x, B[h]["grep"][:], UP, True, True, ["cs", K("grep", h)], [xk])
                ph.act(B[h]["egcb"][:], x, AF.Exp, [xk], [K("egcb", h)])
                for q3, lm in enumerate((UP, LOWS, ones[:])):
                    x, xk = pst()
                    ph.mm(x, lm, B[h]["grep"][:], True, True, ["cs", "ones", K("grep", h)], [xk])
                    ph.act(CL[h][:, q3:q3 + 1], x[:, 0:1], AF.Exp, [xk], [K("col", h)])
                ph.tt("pool", B[h]["decS"][:], B[h]["decS"][:], LOWS, ALU.mult, [K("decS", h), "cs"], [K("decS", h)])
                ph.tt("pool", B[h]["decT"][:], B[h]["decT"][:], UP, ALU.mult, [K("decT", h), "cs"], [K("decT", h)])
                ph.tt("dve", CL[h][:, 3:4], CL[h][:, 0:1], bcol[h], ALU.mult, [K("col", h), "gb"], [K("col", h)])
                x, xk = pst()
                ph.mm(x, kT[h], kT[h], True, True, [kk_], [xk])
                ph.stt(B[h]["P0"][:], x, nbcol[h], B[h]["decS"][:], ALU.mult, ALU.mult,
                       [xk, "nb", K("decS", h)], [K("P0", h)])
                x, xk = pst()
                ph.mm(x, kT[h], qT[h], True, True, [kk_, kq], [xk])
                ph.tt("dve", B[h]["qkT"][:], x, B[h]["decT"][:], ALU.mult, [xk, K("decT", h)], [K("qkT", h)])
                ph.tt("pool", B[h]["qdT"][:], qT[h], B[h]["egcb"][:], ALU.mult, [kq, K("egcb", h)], [K("qdT", h)])
                ph.ts("dve", B[h]["kdec"][:], B[h]["ktm"][:], CL[h][:, 1:2], ALU.mult, [K("ktm", h), K("col", h)], [K("kdec", h)])
                ph.ts("dve", B[h]["vb"][:], B[h]["vtm"][:], bcol[h], ALU.mult, [K("vtm", h), "gb"], [K("vb", h)])
                ph.ts("dve", B[h]["kbg"][:], B[h]["ktm"][:], CL[h][:, 3:4], ALU.mult, [K("ktm", h), K("col", h)], [K("kbg", h)])
            for h in range(4):
                p1, k1 = pst()
                ph.mm(p1, B[h]["P0"][:], IDN, True, True, [K("P0", h), "cs"], [k1])
                ph.copy("act", B[h]["Q0"][:], p1, [k1], [K("Q0", h)])
                ph.tt("dve", B[h]["U0"][:], p1, IDN, ALU.add, [k1, "cs"], [K("U0", h)])
            for k in range(1, 7):
                a, b = (k - 1) % 2, k % 2
                for h in range(4):
                    Pp, Qp = B[h][f"P{a}"], B[h][f"Q{a}"]
                    p1, k1 = pst()
                    ph.mm(p1, Qp[:], Pp[:], True, True, [K(f"Q{a}", h), K(f"P{a}", h)], [k1])
                    ph.copy("act", B[h][f"P{b}"][:], p1, [k1], [K(f"P{b}", h)])
                if k <= 5:
                    for h in range(4):
                        Pp, Qp = B[h][f"P{a}"], B[h][f"Q{a}"]
                        p2, k2 = pst()
                        ph.mm(p2, Pp[:], Qp[:], True, True, [K(f"Q{a}", h), K(f"P{a}", h)], [k2])
                        ph.copy("dve", B[h][f"Q{b}"][:], p2, [k2], [K(f"Q{b}", h)])
                for h in range(4):
                    Up, Un, Pn = B[h][f"U{a}"], B[h][f"U{b}"], B[h][f"P{b}"]
                    p3, k3 = pst()
                    ph.mm(p3, Pn[:], Up[:], True, True, [K(f"P{b}", h), K(f"U{a}", h)], [k3])
                    ph.tt("dve", Un[:], Up[:], p3, ALU.add, [K(f"U{a}", h), k3], [K(f"U{b}", h)])
            UF = "U0"
            for h in range(4):
                p1, k1 = pst()
                ph.mm(p1, B[h][UF][:], B[h]["vb"][:], True, True, [K(UF, h), K("vb", h)], [k1])
                ph.copy("act", B[h]["u"][:], p1, [k1], [K("u", h)])
                p2, k2 = pst()
                ph.mm(p2, B[h]["kbg"][:], B[h][UF][:], True, True, [K(UF, h), K("kbg", h)], [k2])
                ph.copy("dve", B[h]["wT"][:], p2, [k2], [K("wT", h)])
            pw = {}
            for h in range(4):
                p1, k1 = pst()
                ph.mm(p1, B[h]["wT"][:], St[h][:], True, True, [K("wT", h), f"S{h}"], [k1])
                ph.tt("dve", B[h]["vnew"][:], B[h]["u"][:], p1, ALU.subtract, [K("u", h), k1], [K("vnew", h)])
            for h in range(4):
                p2, k2 = pst()
                ph.mm(p2, St[h][:], B[h]["qdT"][:], True, False, [f"S{h}", K("qdT", h)], [k2])
                ph.mm(p2, B[h]["vnew"][:], B[h]["qkT"][:], False, True, [K("vnew", h), K("qkT", h)], [k2])
                ph.copy("act", B[h]["ost"][:], p2, [k2], [K("ost", h)])
                ph.dma("sp", og_scr[h * 128:(h + 1) * 128, n * 128:(n + 1) * 128], B[h]["ost"][:], [K("ost", h)], ["og_scr"])
            for h in range(4):
                p3, k3 = pst()
                ph.mm(p3, B[h]["kdec"][:], B[h]["vnew"][:], True, True, [K("kdec", h), K("vnew", h)], [k3])
                ph.stt(St[h][:], St[h][:], CL[h][:, 2:3], p3, ALU.mult, ALU.add, [f"S{h}", K("col", h), k3], [f"S{h}"])

def phase_merge_cross(nc, xT_own, memT, og_scr, osb_scr, zT_scr, gate_scr, sel, gng, g_cross, g_mem,
                      w_sb_up, w_gdn_up, w_mix_out, w_cq, w_ckv, w_co, x2_scr, x1_dbg=None):
    NT = NOWN // 512
    with Phase(nc) as ph:
        wsb = ph.sb([64, 8, 1024], BF16)
        wgu = ph.sb([128, 4, 1024], BF16)
        wmo = ph.sb([128, 8, 1024], BF16)
        wcq = ph.sb([128, 8, 1024], BF16)
        wco = ph.sb([128, 8, 1024], BF16)
        kxT = ph.sb([128, 8, 256], BF16)
        vx = ph.sb([128, 2, 1024], BF16)
        ones32 = ph.sb([128, 128], F32)
        ones16 = ph.sb([128, 128], BF16)
        selt = ph.sb([128, 16], F32)
        gn = ph.sb([128, 1], F32)
        gcc = ph.sb([128, 8], F32)
        gmc = ph.sb([128, 8], F32)
        xt = ph.sb([128, 8, 512], F32)
        sq = ph.sb([128, 8, 512], F32)
        xn = ph.sb([128, 8, 512], BF16)
        rt = ph.sb([128, 512], F32)
        oga = sq[:, 0:4, :]
        ogb = sq[:, 4:8, :]
        zt = ph.sb([128, 4, 512], F32)
        ogn = ph.sb([128, 4, 512], BF16)
        osb = ph.sb([64, 8, 512], BF16)
        gt = ph.sb([128, 16, 512], BF16)
        mg = ph.sb([128, 8, 512], BF16)
        t1 = [ph.sb([128, 512], F32) for _ in range(2)]
        qx = ph.sb([128, 8, 512], BF16)
        pT = [ph.sb([128, 512], BF16) for _ in range(4)]
        oT = ph.sb([128, 8, 512], BF16)
        rden = [ph.sb([128, 512], F32) for _ in range(2)]
        ps_stat = ph.ps()
        ps_a = [ph.ps() for _ in range(2)]
        ps_b = [ph.ps() for _ in range(2)]
        ps_c = [ph.ps() for _ in range(2)]

        ph.dma("pool", wsb[:], w_sb_up.rearrange("(h d) f -> d h f", d=64), [], ["wsb"])
        ph.dma("pool", wgu[:], w_gdn_up.rearrange("(h d) f -> d h f", d=128), [], ["wgu"])
        for q in range(2):
            hs = slice(4 * q, 4 * q + 4)
            ph.dma("pool", wmo[:, hs, :], fm(w_mix_out, 0, 1024)[:, hs, :], [], ["wmo"])
            ph.dma("pool", wcq[:, hs, :], fm(w_cq, 0, 1024)[:, hs, :], [], ["wcq"])
            ph.dma("pool", wco[:, hs, :], fm(w_co, 0, 1024)[:, hs, :], [], ["wco"])
        ph.dma("sp", selt[:], sel, [], ["selt"])
        ph.dma("sp", gn[:], gng, [], ["gn"])
        ph.dma("sp", gcc[:], g_cross, [], ["gcol"])
        ph.dma("sp", gmc[:], g_mem, [], ["gmc"])
        ph.memset("dve", ones32[:], 1.0, ["ones32"])
        ph.memset("dve", ones16[:], 1.0, ["ones16"])

        ph.dma("sp", xt[:, :, 0:256], fm(memT, 0, 256), [], ["xt0"])
        for c in range(8):
            ph.act(sq[:, c, 0:256], xt[:, c, 0:256], AF.Square, ["xt0"], ["sq"])
        for c in range(8):
            ph.mm(ps_stat[:, 0:256], ones32[:], sq[:, c, 0:256], c == 0, c == 7, ["sq", "ones32"], ["ps_stat"])
        ph.act(rt[:, 0:256], ps_stat[:, 0:256], AF.Sqrt, ["ps_stat"], ["rt"], bias=EPS, scale=1.0 / 1024)
        ph.recip(rt[:, 0:256], rt[:, 0:256], ["rt"], ["rt"])
        for c in range(8):
            ph.stt(xn[:, c, 0:256], xt[:, c, 0:256], gmc[:, c:c + 1], rt[:, 0:256], ALU.mult, ALU.mult,
                   ["xt0", "rt", "gmc"], ["xn0"])
        wk = gt[:, 0:8, :]
        for blk in range(4):
            ph.dma("pool", wk, fm(w_ckv, 0, 2048)[:, :, blk * 512:(blk + 1) * 512], [], ["gt"])
            if blk < 2:
                for ff in range(4):
                    f = blk * 4 + ff
                    pb = ps_a[ff % 2]
                    for c in range(8):
                        ph.mm(pb[:, 0:256], wk[:, c, ff * 128:(ff + 1) * 128], xn[:, c, 0:256], c == 0, c == 7,
                              ["gt", "xn0"], [f"ps_a{ff % 2}"])
                    ph.copy("act", kxT[:, f, :], pb[:, 0:256], [f"ps_a{ff % 2}"], ["kxT"])
            else:
                for mt in range(2):
                    pb = ps_a[mt]
                    for c in range(8):
                        ph.mm(pb[:], xn[:, c, mt * 128:(mt + 1) * 128], wk[:, c, :], c == 0, c == 7,
                              ["gt", "xn0"], [f"ps_a{mt}"])
                    ph.copy("act", vx[:, mt, (blk - 2) * 512:(blk - 1) * 512], pb[:], [f"ps_a{mt}"], ["vx"])

        for j in range(NT):
            t0 = j * 512
            ph.dma("sp", xt[:, 0:4, :], fm(xT_own, t0, 512)[:, 0:4, :], [], ["xt0"])
            ph.dma("sp", xt[:, 4:8, :], fm(xT_own, t0, 512)[:, 4:8, :], [], ["xt0"])
            ogv = og_scr.rearrange("(h p) t -> p h t", p=128)
            ph.dma("sp", oga, ogv[:, :, (2 * j) * 512:(2 * j + 1) * 512], ["og_scr"], ["sq"])
            ph.dma("sp", ogb, ogv[:, :, (2 * j + 1) * 512:(2 * j + 2) * 512], ["og_scr"], ["sq"])
            ph.dma("sp", zt[:], zT_scr.rearrange("(h p) t -> p h t", p=128)[:, :, t0:t0 + 512], ["zT_scr"], ["zt"])
            ph.dma("sp", osb[:], osb_scr.rearrange("(h d) t -> d h t", d=64)[:, :, t0:t0 + 512], ["osb_scr"], ["osb"])
            gv_ = gate_scr.rearrange("(f p) t -> p f t", p=128)
            ph.dma("sp", gt[:, 0:8, :], gv_[:, 0:8, t0:t0 + 512], ["gate_scr"], ["gt"])
            ph.dma("sp", gt[:, 8:16, :], gv_[:, 8:16, t0:t0 + 512], ["gate_scr"], ["gt"])
            ph.ts("dve", oga, oga, selt[:, j:j + 1], ALU.mult, ["sq", "selt"], ["sq"])
            ph.stt(oga, ogb, selt[:, 8 + j:9 + j], oga, ALU.mult, ALU.add, ["sq", "selt"], ["sq"])
            for hh in range(4):
                pb = ps_a[hh % 2]
                pk = f"ps_a{hh % 2}"
                ph.act(rt[:], oga[:, hh, :], AF.Square, ["sq"], ["rt"])
                ph.mm(pb[:], ones32[:], rt[:], True, True, ["ones32", "rt"], [pk])
                ph.act(t1[hh % 2][:], pb[:], AF.Sqrt, [pk], [f"t1{hh % 2}"], bias=EPS, scale=1.0 / 128)
                ph.recip(t1[hh % 2][:], t1[hh % 2][:], [f"t1{hh % 2}"], [f"t1{hh % 2}"])
                ph.stt(t1[hh % 2][:], oga[:, hh, :], gn[:, 0:1], t1[hh % 2][:], ALU.mult, ALU.mult,
                       ["sq", "gn", f"t1{hh % 2}"], [f"t1{hh % 2}"])
                ph.tt("dve", ogn[:, hh, :], t1[hh % 2][:], zt[:, hh, :], ALU.mult, [f"t1{hh % 2}", "zt"], ["ogn"])
            for f in range(8):
                pa, pak = ps_a[f % 2], f"ps_a{f % 2}"
                pb, pbk = ps_b[f % 2], f"ps_b{f % 2}"
                for h in range(8):
                    ph.mm(pa[:], wsb[:, h, f * 128:(f + 1) * 128], osb[:, h, :], h == 0, h == 7, ["wsb", "osb"], [pak])
                for h in range(4):
                    ph.mm(pb[:], wgu[:, h, f * 128:(f + 1) * 128], ogn[:, h, :], h == 0, h == 3, ["wgu", "ogn"], [pbk])
                ph.tt("dve", t1[0][:], pa[:], gt[:, f, :], ALU.mult, [pak, "gt"], ["t10"])
                ph.tt("dve", t1[1][:], pb[:], gt[:, 8 + f, :], ALU.mult, [pbk, "gt"], ["t11"])
                ph.tt("pool", mg[:, f, :], t1[0][:], t1[1][:], ALU.add, ["t10", "t11"], ["mg"])
            for f in range(8):
                pc, pck = ps_c[f % 2], f"ps_c{f % 2}"
                for c in range(8):
                    ph.mm(pc[:], wmo[:, c, f * 128:(f + 1) * 128], mg[:, c, :], c == 0, c == 7, ["wmo", "mg"], [pck])
                ph.tt("dve", xt[:, f, :], xt[:, f, :], pc[:], ALU.add, ["xt0", pck], ["xt0"])
            if x1_dbg is not None:
                ph.dma("sp", fm(x1_dbg, t0, 512), xt[:], ["xt0"], ["x1_dbg"])
            norm_tile(ph, xt, sq, xn, gcc, ones32, ps_stat, rt, 0, None)
            for f in range(8):
                pc, pck = ps_c[f % 2], f"ps_c{f % 2}"
                for c in range(8):
                    ph.mm(pc[:], wcq[:, c, f * 128:(f + 1) * 128], xn[:, c, :], c == 0, c == 7, ["wcq", "xn0"], [pck])
                ph.act(qx[:, f, :], pc[:], AF.Copy, [pck], ["qx"], scale=1.0 / 16)
            for hd in range(4):
                for mt in range(2):
                    pa, pak = ps_a[mt], f"ps_a{mt}"
                    for cc in range(2):
                        ph.mm(pa[:], kxT[:, 2 * hd + cc, mt * 128:(mt + 1) * 128], qx[:, 2 * hd + cc, :], cc == 0, cc == 1,
                              ["kxT", "qx"], [pak])
                    i4 = (hd % 2) * 2 + mt
                    ph.act(pT[i4][:], pa[:], AF.Exp, [pak], [f"pT{i4}"])
                pb, pbk = ps_b[hd % 2], f"ps_b{hd % 2}"
                for mt in range(2):
                    i4 = (hd % 2) * 2 + mt
                    ph.mm(pb[:], ones16[:], pT[i4][:], mt == 0, mt == 1, ["ones16", f"pT{i4}"], [pbk])
                rd = rden[hd % 2]
                ph.recip(rd[:], pb[:], [pbk], [f"rden{hd % 2}"])
                for cc in range(2):
                    pc, pck = ps_c[cc], f"ps_c{cc}"
                    for mt in range(2):
                        i4 = (hd % 2) * 2 + mt
                        ph.mm(pc[:], vx[:, mt, hd * 256 + cc * 128:hd * 256 + (cc + 1) * 128], pT[i4][:], mt == 0, mt == 1,
                              ["vx", f"pT{i4}"], [pck])
                    ph.tt("dve", oT[:, 2 * hd + cc, :], pc[:], rd[:], ALU.mult, [pck, f"rden{hd % 2}"], ["oT"])
            for f in range(8):
                pc, pck = ps_c[f % 2], f"ps_c{f % 2}"
                for c in range(8):
                    ph.mm(pc[:], wco[:, c, f * 128:(f + 1) * 128], oT[:, c, :], c == 0, c == 7, ["wco", "oT"], [pck])
                ph.tt("dve", xt[:, f, :], xt[:, f, :], pc[:], ALU.add, ["xt0", pck], ["xt0"])
            ph.dma("sp", fm(x2_scr, t0, 512)[:, 0:4, :], xt[:, 0:4, :], ["xt0"], ["x2_scr"])
            ph.dma("sp", fm(x2_scr, t0, 512)[:, 4:8, :], xt[:, 4:8, :], ["xt0"], ["x2_scr"])


DELTA = 2e-5


def phase_peer(nc, x2_scr, g_peer, g_final, w_pq, skT_in, sel_in, cst, uT, pv, outT, ntiles=8, negroups=32):
    with Phase(nc) as ph:
        wpq = ph.sb([128, 8, 2048], BF16)
        skT = ph.sb([128, 16, 128], BF16)
        selm = ph.sb([48, 16, 128], BF16)
        idb = ph.sb([128, 128], BF16)
        ones32 = ph.sb([128, 128], F32)
        gpc = ph.sb([128, 8], F32)
        gfc = ph.sb([128, 8], F32)
        xt = ph.sb([128, 8, 512], F32)
        sq = ph.sb([128, 8, 512], F32)
        xn = ph.sb([128, 8, 512], BF16)
        rt = ph.sb([128, 512], F32)
        qT = ph.sb([128, 16, 512], BF16)
        top = ph.sb([128, 16, 16], F32)
        cand = ph.sb([128, 256], F32)
        cand2 = ph.sb([128, 256], F32)
        best = ph.sb([128, 16], F32)
        junk = ph.sb([128, 16], F32)
        sm = ph.sb([128, 8], F32)
        rws = ph.sb([128, 16], F32)
        res = ph.sb([128, 16], F32)
        spl = ph.sb([128, 48], BF16)
        rows = ph.sb([48, 512], BF16)
        theta = ph.sb([128, 8, 512], F32)
        ug = [ph.sb([128, 8, 512], BF16) for _ in range(2)]
        vg = [ph.sb([128, 4, 1024], BF16) for _ in range(2)]
        gA = [ph.sb([128, 512], F32) for _ in range(2)]
        EE = [ph.sb([128, 512], BF16) for _ in range(3)]
        MK = [ph.sb([128, 512], BF16) for _ in range(3)]
        GH = [ph.sb([128, 512], BF16) for _ in range(3)]
        Hg = [ph.sb([128, 4, 512], BF16) for _ in range(2)]
        ps_q = [ph.ps() for _ in range(2)]
        ps_stat = ps_q[0]
        ps_A = [ph.ps()]
        ps_P = [ph.ps() for _ in range(4)]
        ps_O = ph.ps()

        for q in range(4):
            ph.dma("pool", wpq[:, 2 * q:2 * q + 2, :], fm(w_pq, 0, 2048)[:, 2 * q:2 * q + 2, :], [], ["wpq"])
        ph.dma("pool", skT[:], skT_in, [], ["skT"])
        ph.dma("pool", selm[:], sel_in, [], ["selm"])
        ph.dma("pool", idb[:], cst[:, C_ID:C_ID + 128], [], ["idb"])
        ph.dma("sp", gpc[:], g_peer, [], ["gcol"])
        ph.dma("sp", gfc[:], g_final, [], ["gfc"])
        ph.memset("dve", ones32[:], 1.0, ["ones32"])

        s_tm = sq[:, 0:4, :].rearrange("p a (b c) -> p (a b) c", c=128)
        s_w = sq[:, 4:8, :].rearrange("p a (b c) -> p (a b) c", c=128)
        ndma = 0
        for j in range(ntiles):
            t0 = j * 512
            ph.dma("sp", xt[:, 0:4, :], fm(x2_scr, t0, 512)[:, 0:4, :], ["x2_scr"], ["xt0"])
            ph.dma("sp", xt[:, 4:8, :], fm(x2_scr, t0, 512)[:, 4:8, :], ["x2_scr"], ["xt0"])
            norm_tile(ph, xt, sq, xn, gpc, ones32, ps_stat, rt, 0, "ps_q0")
            for f in range(16):
                pb, pk = ps_q[f % 2], f"ps_q{f % 2}"
                for c in range(8):
                    ph.mm(pb[:], wpq[:, c, f * 128:(f + 1) * 128], xn[:, c, :], c == 0, c == 7, ["wpq", "xn0"], [pk])
                ph.copy("act", qT[:, f, :], pb[:], [pk], ["qT"])
            for u in range(4):
                us = slice(u * 128, (u + 1) * 128)
                for g4 in range(4):
                    pb, pk = ps_q[g4 % 2], f"ps_q{g4 % 2}"
                    for k4 in range(4):
                        hp = g4 * 4 + k4
                        ph.mm(pb[:, k4 * 128:(k4 + 1) * 128], qT[:, hp, us], skT[:, hp, :], True, True, ["qT", "skT"], [pk])
                    ph.copy("act", s_tm[:, g4 * 4:(g4 + 1) * 4, :], pb[:].rearrange("p (a c) -> p a c", c=128), [pk], ["sq"])
                for hp in range(16):
                    ph.P.add("dve", lambda e, hp=hp: e.max(out=top[:, hp, 0:8], in_=s_tm[:, hp, :]), ["sq"], ["top"])
                    ph.P.add("dve", lambda e, hp=hp: e.match_replace(out=s_w[:, hp, :], in_to_replace=top[:, hp, 0:8],
                                                                     in_values=s_tm[:, hp, :], imm_value=-1e30),
                             ["sq", "top"], ["sq"])
                    ph.P.add("dve", lambda e, hp=hp: e.max(out=top[:, hp, 8:16], in_=s_w[:, hp, :]), ["sq"], ["top"])
                for h in range(8):
                    c3 = cand[:].rearrange("p (a b) -> p a b", a=16)
                    in0 = top[:, 2 * h, :].rearrange("p (a o) -> p a o", o=1).to_broadcast([128, 16, 16])
                    in1 = top[:, 2 * h + 1, :].rearrange("p (o b) -> p o b", o=1).to_broadcast([128, 16, 16])
                    ph.tt("dve", c3, in0, in1, ALU.add, ["top"], ["cand"])
                    ph.P.add("dve", lambda e: e.max(out=best[:, 0:8], in_=cand[:]), ["cand"], ["best"])
                    ph.P.add("dve", lambda e: e.match_replace(out=cand2[:], in_to_replace=best[:, 0:8], in_values=cand[:],
                                                              imm_value=-1e30), ["cand", "best"], ["cand2"])
                    ph.P.add("dve", lambda e: e.max(out=best[:, 8:16], in_=cand2[:]), ["cand2"], ["best"])
                    ph.ts("dve", sm[:, 0:1], best[:, 0:1], -1.0, ALU.mult, ["best"], ["sm"])
                    ph.memset("dve", sm[:, 1:2], 0.0, ["sm"])
                    ph.act(junk[:], best[:], AF.Exp, ["best", "sm"], ["junk", "sm"], bias=sm[:, 0:1], scale=1.0,
                           accum_out=sm[:, 1:2])
                    ph.act(sm[:, 2:3], sm[:, 1:2], AF.Ln, ["sm"], ["sm"])
                    ph.tt("dve", rws[:, 8 + h:9 + h], sm[:, 0:1], sm[:, 2:3], ALU.subtract, ["sm"], ["rws"])
                    ph.stt(rws[:, h:h + 1], best[:, 15:16], -DELTA, rws[:, 8 + h:9 + h], ALU.add, ALU.add,
                           ["best", "rws"], ["rws"])
                ph.copy("dve", spl[:, 0:16], rws[:], ["rws"], ["spl"])
                ph.tt("dve", res[:], rws[:], spl[:, 0:16], ALU.subtract, ["rws", "spl"], ["res"])
                ph.copy("dve", spl[:, 16:32], res[:], ["res"], ["spl"])
                ph.tt("dve", res[:], res[:], spl[:, 16:32], ALU.subtract, ["res", "spl"], ["res"])
                ph.copy("dve", spl[:, 32:48], res[:], ["res"], ["spl"])
                pb, pk = ps_q[u % 2], f"ps_q{u % 2}"
                ph.mm(pb[0:48, 0:128], spl[:], idb[:], True, True, ["spl", "idb"], [pk])
                ph.copy("act", rows[:, us], pb[0:48, 0:128], [pk], ["rows"])
            for h in range(8):
                pb, pk = ps_q[h % 2], f"ps_q{h % 2}"
                ph.mm(pb[:], selm[:, h, :], rows[:], True, True, ["selm", "rows"], [pk])
                ph.copy("act", theta[:, h, :], pb[:], [pk], ["theta"])
            it = 0
            def load_eg(eg):
                gs = eg % 2
                ph.dma("pool", ug[gs][:], fm(uT, eg * 512, 512), [], [f"ug{gs}"])
                ph.dma("pool", vg[gs][:], pv.rearrange("(g p) d -> p g d", p=128)[:, eg * 4:(eg + 1) * 4, :], [], [f"vg{gs}"])

            load_eg(0)
            for eg in range(negroups):
                gs = eg % 2
                if eg + 1 < negroups:
                    load_eg(eg + 1)
                for ii in range(4):
                    i = eg * 4 + ii
                    a2 = i % 2
                    pa, pak = ps_A[0], "ps_A0"
                    for c in range(8):
                        ph.mm(pa[:], ug[gs][:, c, ii * 128:(ii + 1) * 128], xn[:, c, :], c == 0, c == 7,
                              [f"ug{gs}", "xn0"], [pak])
                    ph.act(gA[a2][:], pa[:], AF.Gelu, [pak], [f"gA{a2}"])
                    pend = None
                    for h in range(8):
                        p2, p3 = it % 4, it % 3
                        it += 1
                        pp, ppk = ps_P[p2], f"ps_P{p2}"
                        ph.mm(pp[:], skT[:, 2 * h + 1, :], qT[:, 2 * h + 1, :], True, False, ["skT", "qT"], [ppk])
                        ph.mm(pp[:], skT[:, 2 * h, i:i + 1].to_broadcast([128, 128]), qT[:, 2 * h, :], False, False,
                              ["skT", "qT"], [ppk])
                        ph.mm(pp[:], selm[:, 8 + h, :], rows[:], False, True, ["selm", "rows"], [ppk])
                        ph.act(EE[p3][:], pp[:], AF.Exp, [ppk], [f"EE{p3}"])
                        ph.tt("dve", MK[p3][:], pp[:], theta[:, h, :], ALU.is_ge, [ppk, "theta"], [f"MK{p3}"])
                        if pend is not None:
                            pend()

                        def back(h=h, p3=p3):
                            ph.tt("dve", GH[p3][:], MK[p3][:], EE[p3][:], ALU.mult, [f"MK{p3}", f"EE{p3}"], [f"GH{p3}"])
                            ph.mm(ps_q[a2][:], idb[:], GH[p3][:], h == 0, h == 7, ["idb", f"GH{p3}"], [f"ps_q{a2}"])
                        pend = back
                    pend()
                    ph.tt("dve", Hg[gs][:, ii, :], gA[a2][:], ps_q[a2][:], ALU.mult, [f"gA{a2}", f"ps_q{a2}"], [f"Hg{gs}"])
                for f in range(8):
                    for ii in range(4):
                        ph.mm(ps_O[:], vg[gs][:, ii, f * 128:(f + 1) * 128], Hg[gs][:, ii, :], ii == 0, ii == 3,
                              [f"vg{gs}", f"Hg{gs}"], ["ps_O"])
                    ph.tt("dve", xt[:, f, :], xt[:, f, :], ps_O[:], ALU.add, ["xt0", "ps_O"], ["xt0"])
            for c in range(8):
                ph.act(sq[:, c, :], xt[:, c, :], AF.Square, ["xt0"], ["sq"])
            for c in range(8):
                ph.mm(ps_stat[:], ones32[:], sq[:, c, :], c == 0, c == 7, ["sq", "ones32"], ["ps_q0"])
            ph.act(rt[:], ps_stat[:], AF.Sqrt, ["ps_q0"], ["rt"], bias=EPS, scale=1.0 / 1024)
            ph.recip(rt[:], rt[:], ["rt"], ["rt"])
            for c in range(8):
                ph.stt(sq[:, c, :], xt[:, c, :], gfc[:, c:c + 1], rt[:], ALU.mult, ALU.mult, ["xt0", "rt", "gfc"], ["sq"])
            ph.dma("sp", fm(outT, t0, 512)[:, 0:4, :], sq[:, 0:4, :], ["sq"], ["outT"])
            ph.dma("sp", fm(outT, t0, 512)[:, 4:8, :], sq[:, 4:8, :], ["sq"], ["outT"])


_NC = None


def kernel(**inputs):
    global _NC
    if _NC is None:
        _NC = build()
    maps = prepare(inputs)
    res = run_bass_kernel_spmd(_NC, maps, core_ids=list(range(8)))
    out = np.empty((4, SEQ, D), np.float32)
    for c in range(8):
        b, p = c // 2, c % 2
        o = np.asarray(res.results[c]["outT"]).T
        for j, t in enumerate(tile_ids(p)):
            out[b, t * 512:(t + 1) * 512] = o[j * 512:(j + 1) * 512]
    return out
```

```python
import contextlib
import numpy as np
import concourse.bass as bass
import concourse.mybir as mybir
from concourse.bass_utils import run_bass_kernel_spmd

F32 = mybir.dt.float32
BF16 = mybir.dt.bfloat16
AF = mybir.ActivationFunctionType
ALU = mybir.AluOpType

D = 1024
SEQ = 8192
NOWN = 4096
EPS = 1e-6
ENGS = ("pe", "act", "dve", "pool", "sp")
NEG = -30000.0


class Op:
    __slots__ = ("eng", "fn", "idx", "dma", "deps", "waits", "signal", "rank",
                 "dma_sem", "dma_val")

    def __init__(self, eng, fn, idx, dma):
        self.eng = eng
        self.fn = fn
        self.idx = idx
        self.dma = dma
        self.deps = ()
        self.waits = None
        self.signal = False
        self.rank = 0
        self.dma_sem = -1
        self.dma_val = 0


NSEM = {"pe": 18, "act": 16, "dve": 20, "pool": 6, "sp": 0}
N_DMA_SEMS = 20
SEM_CAP = 2000


class SemState:
    def __init__(self, nc):
        self.c = {e: [nc.alloc_semaphore(name=f"c_{e}_{i}") for i in range(NSEM[e])] for e in ENGS}
        self.d = [nc.alloc_semaphore(name=f"d_{i}") for i in range(N_DMA_SEMS)]
        self.rank = {e: 0 for e in ENGS}
        self.ndma = 0
        for e in ENGS:
            for h in self.c[e]:
                nc.gpsimd.sem_clear(h)
        for h in self.d:
            nc.gpsimd.sem_clear(h)
        nc.all_engine_barrier()


class Prog:
    def __init__(self, nc, sems):
        self.nc = nc
        self.sems = sems
        self.ops = {e: [] for e in ENGS}
        self.last_writer = {}
        self.readers = {}
        self.n_dma_sems = N_DMA_SEMS
        self.dma_ops = []
        self.dma_base = sems.ndma

    def add(self, eng, fn, reads=(), writes=(), dma=False):
        op = Op(eng, fn, len(self.ops[eng]), dma)
        psk = [k for k in reads if k.startswith("ps")]
        if psk:
            reads = [k for k in reads if not k.startswith("ps")]
            writes = list(writes) + [k for k in psk if k not in writes]
        deps = set()
        lw_get = self.last_writer.get
        for k in reads:
            lw = lw_get(k)
            if lw is not None:
                deps.add(lw)
        for k in writes:
            lw = lw_get(k)
            if lw is not None:
                deps.add(lw)
            rs = self.readers.get(k)
            if rs:
                deps.update(rs)
        for k in reads:
            self.readers.setdefault(k, []).append(op)
        for k in writes:
            self.last_writer[k] = op
            self.readers[k] = []
        if dma:
            jl = len(self.dma_ops)
            j = self.dma_base + jl
            op.dma_sem = j % self.n_dma_sems
            op.dma_val = 16 * (j // self.n_dma_sems + 1)
            if jl >= self.n_dma_sems:
                deps.add(self.dma_ops[jl - self.n_dma_sems])
            self.dma_ops.append(op)
        deps.discard(op)
        op.deps = tuple(deps)
        self.ops[eng].append(op)
        return op

    def dma(self, eng, out, in_, reads, writes, **kw):
        return self.add(eng, lambda e: e.dma_start(out=out, in_=in_, **kw),
                        reads, writes, dma=True)

    def finish(self):
        deps = set(self.last_writer.values())
        for rs in self.readers.values():
            deps.update(rs)
        deps.update(self.dma_ops[-self.n_dma_sems:])
        for e in ENGS:
            op = Op(e, lambda eng: None, len(self.ops[e]), False)
            op.deps = tuple(deps)
            self.ops[e].append(op)

    def emit(self):
        nc = self.nc
        for e in ENGS:
            seen = {}
            for op in self.ops[e]:
                waits = []
                for p in sorted(op.deps, key=lambda q: -q.idx):
                    if p.dma:
                        key = ("d", p.dma_sem)
                        if seen.get(key, 0) >= p.dma_val:
                            continue
                        seen[key] = p.dma_val
                        waits.append(p)
                    else:
                        if p.eng == e:
                            if e == "pe" or e == "sp":
                                continue
                            if op.idx - p.idx > 1:
                                continue
                        key = ("c", p.eng)
                        if seen.get(key, -1) >= p.idx:
                            continue
                        seen[key] = p.idx
                        p.signal = True
                        waits.append(p)
                op.waits = waits
        for e in ENGS:
            r = self.sems.rank[e]
            for op in self.ops[e]:
                if op.signal and not op.dma:
                    r += 1
                    op.rank = r
            self.sems.rank[e] = r
            assert r <= NSEM[e] * SEM_CAP, (e, r)
        self.sems.ndma += len(self.dma_ops)
        csems = self.sems.c
        dsems = self.sems.d
        with contextlib.ExitStack() as st:
            block = st.enter_context(nc.Block())

            def run(e):
                def body(eng):
                    for op in self.ops[e]:
                        for p in op.waits:
                            if p.dma:
                                eng.wait_ge(dsems[p.dma_sem], p.dma_val)
                            else:
                                k = (p.rank - 1) // SEM_CAP
                                eng.wait_ge(csems[p.eng][k], (p.rank - 1) % SEM_CAP + 1)
                        ins = op.fn(eng)
                        if ins is None:
                            assert not op.signal and not op.dma
                        elif op.dma:
                            ins.then_inc(dsems[op.dma_sem], 16)
                        elif op.signal:
                            k = (op.rank - 1) // SEM_CAP
                            ins.then_inc(csems[e][k], 1)
                return body

            block.tensor(run("pe"))
            block.scalar(run("act"))
            block.vector(run("dve"))
            block.gpsimd(run("pool"))
            block.sync(run("sp"))


class Phase:
    _cnt = [0]
    _sem = {}

    def __init__(self, nc):
        self.nc = nc
        if id(nc) not in Phase._sem:
            Phase._sem.clear()
            Phase._sem[id(nc)] = SemState(nc)
        Phase._cnt[0] += 1
        self.pid = Phase._cnt[0]

    def __enter__(self):
        self.st = contextlib.ExitStack()
        self.P = Prog(self.nc, Phase._sem[id(self.nc)])
        self.n = 0
        return self

    def sb(self, shape, dtype):
        self.n += 1
        return self.st.enter_context(self.nc.sbuf_tensor(f"t{self.pid}_{self.n}", list(shape), dtype))

    def ps(self, shape=(128, 512), dtype=F32):
        self.n += 1
        return self.st.enter_context(self.nc.psum_tensor(f"p{self.pid}_{self.n}", list(shape), dtype))

    def __exit__(self, *a):
        if a[0] is None:
            self.P.finish()
            self.P.emit()
        self.st.close()
        return False

    def mm(self, out, lhsT, rhs, start, stop, r, w):
        self.P.add("pe", lambda e: e.matmul(out, lhsT=lhsT, rhs=rhs, start=start, stop=stop), r, w)

    def act(self, out, in_, func, r, w, eng="act", **kw):
        self.P.add(eng, lambda e: e.activation(out=out, in_=in_, func=func, **kw), r, w)

    def copy(self, eng, out, in_, r, w):
        if eng == "act":
            self.P.add("act", lambda e: e.copy(out=out, in_=in_), r, w)
        else:
            self.P.add(eng, lambda e: e.tensor_copy(out=out, in_=in_), r, w)

    def tt(self, eng, out, in0, in1, op, r, w):
        self.P.add(eng, lambda e: e.tensor_tensor(out=out, in0=in0, in1=in1, op=op), r, w)

    def ts(self, eng, out, in0, s1, op0, r, w, s2=None, op1=None):
        if op1 is None:
            self.P.add(eng, lambda e: e.tensor_scalar(out=out, in0=in0, scalar1=s1, scalar2=None, op0=op0), r, w)
        else:
            self.P.add(eng, lambda e: e.tensor_scalar(out=out, in0=in0, scalar1=s1, scalar2=s2, op0=op0, op1=op1), r, w)

    def stt(self, out, in0, scalar, in1, op0, op1, r, w):
        self.P.add("dve", lambda e: e.scalar_tensor_tensor(out=out, in0=in0, scalar=scalar, in1=in1, op0=op0, op1=op1), r, w)

    def recip(self, out, in_, r, w):
        self.P.add("dve", lambda e: e.reciprocal(out=out, in_=in_), r, w)

    def memset(self, eng, ap, val, w):
        self.P.add(eng, lambda e: e.memset(ap, val), (), w)

    def dma(self, eng, out, in_, r, w):
        self.P.dma(eng, out, in_, r, w)


def fm(ap, t0, n):
    return ap.rearrange("(c p) t -> p c t", p=128)[:, :, t0:t0 + n]


def norm_tile(ph, xt, sq, xn, gcol, ones32, ps_stat, rt, slot, key, nch=8, width=512):
    for c in range(nch):
        ph.act(sq[:, c, :], xt[:, c, :], AF.Square, [f"xt{slot}"], ["sq"])
    pk = key or "ps_stat"
    for c in range(nch):
        ph.mm(ps_stat[:, 0:width], ones32[:], sq[:, c, :], c == 0, c == nch - 1, ["sq", "ones32"], [pk])
    ph.act(rt[:, 0:width], ps_stat[:, 0:width], AF.Sqrt, [pk], ["rt"], bias=EPS, scale=1.0 / (128 * nch))
    ph.recip(rt[:, 0:width], rt[:, 0:width], ["rt"], ["rt"])
    for c in range(nch):
        ph.stt(xn[:, c, :], xt[:, c, :], gcol[:, c:c + 1], rt[:, 0:width], ALU.mult, ALU.mult,
               [f"xt{slot}", "rt", "gcol"], [f"xn{slot}"])


def phase_inproj_ctx(nc, xT, g_mix, w_fm, w_tm, convw, alog_bc, dtb_bc,
                     kT_scr, v_scr, gq_scr, gk_scr, gv_scr, gb_scr):
    NT = SEQ // 512
    with Phase(nc) as ph:
        wfm = ph.sb([128, 8, 2048], BF16)
        wtm = ph.sb([128, 8, 520], BF16)
        gcol = ph.sb([128, 8], F32)
        cw = ph.sb([128, 12, 4], F32)
        alog = ph.sb([128, 4], F32)
        dtb = ph.sb([128, 4], F32)
        ones32 = ph.sb([128, 128], F32)
        xt = [ph.sb([128, 8, 512], F32) for _ in range(2)]
        sq = ph.sb([128, 8, 512], F32)
        xn = [ph.sb([128, 8, 512], BF16) for _ in range(2)]
        rt = ph.sb([128, 512], F32)
        pre = [ph.sb([128, 515], F32) for _ in range(12)]
        acc = [ph.sb([128, 512], F32) for _ in range(2)]
        sil = [ph.sb([128, 512], F32) for _ in range(2)]
        sq2 = [ph.sb([128, 512], F32) for _ in range(2)]
        rt2 = [ph.sb([128, 512], F32) for _ in range(2)]
        kst = [ph.sb([128, 512], BF16) for _ in range(2)]
        vst = [ph.sb([128, 512], BF16) for _ in range(2)]
        bat = [ph.sb([128, 8], F32) for _ in range(2)]
        ps_stat = ph.ps()
        ps_f = [ph.ps() for _ in range(2)]
        ps_n = [ph.ps() for _ in range(2)]
        ps_t = [ph.ps() for _ in range(2)]

        for q in range(4):
            ph.dma("pool", wfm[:, 2 * q:2 * q + 2, :], fm(w_fm, 0, 2048)[:, 2 * q:2 * q + 2, :], ["w_fm"], ["wfm"])
        ph.dma("pool", wtm[:], fm(w_tm, 0, 520), ["w_tm"], ["wtm"])
        ph.dma("sp", gcol[:], g_mix, [], ["gcol"])
        ph.dma("sp", cw[:], convw.rearrange("(f p) j -> p f j", p=128), [], ["cw"])
        ph.dma("sp", alog[:], alog_bc, [], ["alog"])
        ph.dma("sp", dtb[:], dtb_bc, [], ["dtb"])
        ph.memset("dve", ones32[:], 1.0, ["ones32"])
        for f in range(12):
            ph.memset("pool", pre[f][:, 0:3], 0.0, [f"pre{f}"])
        ph.act(alog[:], alog[:], AF.Exp, ["alog"], ["alog"])

        nfe = 0
        for ti in range(NT):
            s = ti % 2
            t0 = ti * 512
            ph.dma("sp", xt[s][:, 0:4, :], fm(xT, t0, 512)[:, 0:4, :], [], [f"xt{s}"])
            ph.dma("sp", xt[s][:, 4:8, :], fm(xT, t0, 512)[:, 4:8, :], [], [f"xt{s}"])
            norm_tile(ph, xt[s], sq, xn[s], gcol, ones32, ps_stat, rt, s, None)
            for f in range(16):
                pb = ps_f[f % 2]
                pk = f"ps_f{f % 2}"
                for c in range(8):
                    ph.mm(pb[:], wfm[:, c, f * 128:(f + 1) * 128], xn[s][:, c, :], c == 0, c == 7,
                          ["wfm", f"xn{s}"], [pk])
                if f < 4:
                    b = nfe % 2
                    nfe += 1
                    ph.copy("act", kst[b][:], pb[:], [pk], [f"kst{b}"])
                    ph.dma("sp", kT_scr[f * 128:(f + 1) * 128, t0:t0 + 512], kst[b][:], [f"kst{b}"], ["kT_scr"])
                    continue
                g = f - 4
                b = g % 2
                if ti > 0:
                    ph.copy("pool", pre[g][:, 0:3], pre[g][:, 512:515], [f"pre{g}"], [f"pre{g}"])
                ph.copy("act", pre[g][:, 3:515], pb[:], [pk, f"pre{g}"], [f"pre{g}"])
                ph.ts("dve", acc[b][:], pre[g][:, 0:512], cw[:, g, 0:1], ALU.mult, [f"pre{g}", "cw"], [f"acc{b}"])
                for j in range(1, 4):
                    ph.stt(acc[b][:], pre[g][:, j:j + 512], cw[:, g, j:j + 1], acc[b][:], ALU.mult, ALU.add,
                           [f"pre{g}", "cw", f"acc{b}"], [f"acc{b}"])
                ph.act(sil[b][:], acc[b][:], AF.Silu, [f"acc{b}"], [f"sil{b}"])
                if g < 8:
                    pn = ps_n[b]
                    ph.act(sq2[b][:], sil[b][:], AF.Square, [f"sil{b}"], [f"sq2{b}"])
                    ph.mm(pn[:], ones32[:], sq2[b][:], True, True, ["ones32", f"sq2{b}"], [f"ps_n{b}"])
                    ph.act(rt2[b][:], pn[:], AF.Sqrt, [f"ps_n{b}"], [f"rt2{b}"], bias=EPS, scale=1.0)
                    ph.recip(rt2[b][:], rt2[b][:], [f"rt2{b}"], [f"rt2{b}"])
                    scl = 128 ** -0.5 if g < 4 else 1.0
                    ph.stt(sil[b][:], sil[b][:], scl, rt2[b][:], ALU.mult, ALU.mult,
                           [f"sil{b}", f"rt2{b}"], [f"sil{b}"])
                dst = (gq_scr, gk_scr, gv_scr)[g // 4]
                hh = g % 4
                ph.dma("sp", dst[hh * 128:(hh + 1) * 128, t0:t0 + 512], sil[b][:], [f"sil{b}"], ["g_scr"])
            for u in range(4):
                b = u % 2
                pt = ps_t[b]
                pk = f"ps_t{b}"
                for c in range(8):
                    ph.mm(pt[:], xn[s][:, c, u * 128:(u + 1) * 128], wtm[:, c, 0:512], c == 0, c == 7,
                          [f"xn{s}", "wtm"], [pk])
                ph.copy("act", vst[b][:], pt[:], [pk], [f"vst{b}"])
                ph.dma("sp", v_scr[t0 + u * 128:t0 + (u + 1) * 128, :], vst[b][:], [f"vst{b}"], ["v_scr"])
                pn = ps_n[b]
                pnk = f"ps_n{b}"
                for c in range(8):
                    ph.mm(pn[:, 0:8], xn[s][:, c, u * 128:(u + 1) * 128], wtm[:, c, 512:520], c == 0, c == 7,
                          [f"xn{s}", "wtm"], [pnk])
                ph.act(bat[b][:, 0:4], pn[:, 0:4], AF.Sigmoid, [pnk], [f"bat{b}"])
                ph.tt("dve", bat[b][:, 4:8], pn[:, 4:8], dtb[:], ALU.add, [pnk, "dtb"], [f"bat{b}"])
                ph.act(bat[b][:, 4:8], bat[b][:, 4:8], AF.Exp, [f"bat{b}"], [f"bat{b}"])
                ph.act(bat[b][:, 4:8], bat[b][:, 4:8], AF.Ln, [f"bat{b}"], [f"bat{b}"], bias=1.0)
                ph.stt(bat[b][:, 4:8], bat[b][:, 4:8], -1.0, alog[:], ALU.mult, ALU.mult,
                       [f"bat{b}", "alog"], [f"bat{b}"])
                ph.dma("sp", gb_scr[t0 + u * 128:t0 + (u + 1) * 128, :], bat[b][:], [f"bat{b}"], ["gb_scr"])


def phase_inproj_own(nc, xT, g_mix, w_own, qT_scr, zT_scr, gate_scr):
    NT = NOWN // 512
    with Phase(nc) as ph:
        wfm = ph.sb([128, 8, 3072], BF16)
        gcol = ph.sb([128, 8], F32)
        ones32 = ph.sb([128, 128], F32)
        xt = [ph.sb([128, 8, 512], F32) for _ in range(2)]
        sq = ph.sb([128, 8, 512], F32)
        xn = [ph.sb([128, 8, 512], BF16) for _ in range(2)]
        rt = ph.sb([128, 512], F32)
        st16 = [ph.sb([128, 512], BF16) for _ in range(3)]
        st32 = [ph.sb([128, 512], F32) for _ in range(2)]
        ps_stat = ph.ps()
        ps_f = [ph.ps() for _ in range(3)]
        for q in range(8):
            ph.dma("pool", wfm[:, q:q + 1, :], fm(w_own, 0, 3072)[:, q:q + 1, :], ["w"], ["wfm"])
        ph.dma("sp", gcol[:], g_mix, [], ["gcol"])
        ph.memset("dve", ones32[:], 1.0, ["ones32"])
        n16 = 0
        n32 = 0
        for ti in range(NT):
            s = ti % 2
            t0 = ti * 512
            ph.dma("sp", xt[s][:, 0:4, :], fm(xT, t0, 512)[:, 0:4, :], [], [f"xt{s}"])
            ph.dma("sp", xt[s][:, 4:8, :], fm(xT, t0, 512)[:, 4:8, :], [], [f"xt{s}"])
            norm_tile(ph, xt[s], sq, xn[s], gcol, ones32, ps_stat, rt, s, None)
            for f in range(24):
                pb = ps_f[f % 3]
                pk = f"ps_f{f % 3}"
                for c in range(8):
                    ph.mm(pb[:], wfm[:, c, f * 128:(f + 1) * 128], xn[s][:, c, :], c == 0, c == 7,
                          ["wfm", f"xn{s}"], [pk])
                if f < 4:
                    b = n16 % 3
                    n16 += 1
                    ph.act(st16[b][:], pb[:], AF.Copy, [pk], [f"s16{b}"], scale=0.125)
                    ph.dma("sp", qT_scr[f * 128:(f + 1) * 128, t0:t0 + 512], st16[b][:], [f"s16{b}"], ["qT_scr"])
                elif f < 8:
                    b = n32 % 2
                    n32 += 1
                    ph.act(st32[b][:], pb[:], AF.Silu, [pk], [f"s32{b}"])
                    ph.dma("sp", zT_scr[(f - 4) * 128:(f - 3) * 128, t0:t0 + 512], st32[b][:], [f"s32{b}"], ["zT_scr"])
                else:
                    b = n16 % 3
                    n16 += 1
                    ph.act(st16[b][:], pb[:], AF.Sigmoid, [pk], [f"s16{b}"])
                    ph.dma("sp", gate_scr[(f - 8) * 128:(f - 7) * 128, t0:t0 + 512], st16[b][:], [f"s16{b}"], ["gate_scr"])


C_LOW, C_LOWS, C_UP, C_ID, C_TRI = 0, 128, 256, 384, 512


def phase_sb(nc, qT_scr, kT_scr, v_scr, masks, cst, osb_scr, nj=8):
    with Phase(nc) as ph:
        kT = ph.sb([128, SEQ], BF16)
        vv = ph.sb([128, 64, 128], BF16)
        qT = ph.sb([128, NOWN], BF16)
        mk = ph.sb([128, 16, 512], F32)
        tri = ph.sb([128, 128], BF16)
        negones = ph.sb([128, 128], BF16)
        Ls = [ph.sb([128, 512], BF16) for _ in range(2)]
        zm = [ph.sb([128, 512], F32) for _ in range(2)]
        ee = [ph.sb([128, 512], F32) for _ in range(3)]
        LL = [ph.sb([128, 512], BF16) for _ in range(3)]
        eR = [ph.sb([128, 512], F32) for _ in range(2)]
        aa = [ph.sb([128, 512], BF16) for _ in range(3)]
        ost = [ph.sb([64, 512], BF16) for _ in range(2)]
        ps_z = [ph.ps() for _ in range(2)]
        ps_r = [ph.ps() for _ in range(2)]
        ps_o = [ph.ps() for _ in range(2)]

        ph.dma("sp", mk[:, 0:8, :], masks[:, 0:8, :], [], ["mk"])
        ph.dma("sp", mk[:, 8:16, :], masks[:, 8:16, :], [], ["mk"])
        ph.dma("pool", tri[:], cst[:, C_TRI:C_TRI + 128], [], ["tri"])
        ph.memset("dve", negones[:], -1.0, ["negones"])
        it = 0
        nev = 0
        for hp in range(4):
            for q in range(4):
                ph.dma("sp", kT[:, q * 2048:(q + 1) * 2048], kT_scr[hp * 128:(hp + 1) * 128, q * 2048:(q + 1) * 2048],
                       ["kT_scr"], ["kT"])
                ph.dma("sp", vv[:, q * 16:(q + 1) * 16, :],
                       v_scr.rearrange("(n p) f -> p n f", p=128)[:, q * 16:(q + 1) * 16, hp * 128:(hp + 1) * 128],
                       ["v_scr"], ["vv"])
            ph.dma("sp", qT[:], qT_scr[hp * 128:(hp + 1) * 128, :], ["qT_scr"], ["qT"])
            for j in range(nj):
                for hh in range(2):
                    h = 2 * hp + hh
                    pr = slice(hh * 64, hh * 64 + 64)
                    nblk = 8 * (j + 1)
                    po = ps_o[hh]
                    pok = f"ps_o{hh}"
                    base = it
                    it += nblk

                    def F1(bi):
                        n = base + bi
                        kb = nblk - 1 - bi
                        i2, i3 = n % 2, n % 3
                        pz, pzk = ps_z[i2], f"ps_z{i2}"
                        ph.mm(pz[:], kT[pr, kb * 128:(kb + 1) * 128], qT[pr, j * 512:(j + 1) * 512], True, True,
                              ["kT", "qT"], [pzk])
                        if kb >= nblk - 8:
                            unit = (kb - (nblk - 8)) // 4
                            midx = (j % 2) * 8 + unit * 4 + (kb % 4)
                            ph.tt("dve", zm[i2][:], pz[:], mk[:, midx, :], ALU.add, [pzk, "mk"], [f"zm{i2}"])
                            ph.act(ee[i3][:], zm[i2][:], AF.Exp, [f"zm{i2}"], [f"ee{i3}"])
                        else:
                            ph.act(ee[i3][:], pz[:], AF.Exp, [pzk], [f"ee{i3}"])

                    def F2(bi):
                        n = base + bi
                        i2, i3 = n % 2, n % 3
                        prr, prk = ps_r[i2], f"ps_r{i2}"
                        ph.act(LL[i3][:], ee[i3][:], AF.Ln, [f"ee{i3}"], [f"LL{i3}"], bias=1.0)
                        ph.mm(prr[:], tri[:], LL[i3][:], True, bi == 0, ["tri", f"LL{i3}"], [prk])
                        if bi > 0:
                            ph.mm(prr[:], negones[:], Ls[(bi - 1) % 2][:], False, True,
                                  ["negones", f"Ls{(bi - 1) % 2}"], [prk])
                        if bi == 0:
                            ph.copy("dve", Ls[0][:], LL[i3][:], [f"LL{i3}"], ["Ls0"])
                        elif bi < nblk - 1:
                            ph.tt("dve", Ls[bi % 2][:], Ls[(bi - 1) % 2][:], LL[i3][:], ALU.add,
                                  [f"Ls{(bi - 1) % 2}", f"LL{i3}"], [f"Ls{bi % 2}"])

                    def B1(bi):
                        n = base + bi
                        i2 = n % 2
                        ph.act(eR[i2][:], ps_r[i2][:], AF.Exp, [f"ps_r{i2}"], [f"eR{i2}"])

                    def B2(bi):
                        n = base + bi
                        kb = nblk - 1 - bi
                        i2, i3 = n % 2, n % 3
                        ph.tt("dve", aa[i3][:], ee[i3][:], eR[i2][:], ALU.mult, [f"ee{i3}", f"eR{i2}"], [f"aa{i3}"])
                        ph.mm(po[0:64, :], vv[:, kb, hh * 64:(hh + 1) * 64], aa[i3][:], bi == 0, bi == nblk - 1,
                              ["vv", f"aa{i3}"], [pok])

                    F1(0)
                    for t in range(nblk + 1):
                        if t + 1 < nblk:
                            F1(t + 1)
                        if t >= 1:
                            B1(t - 1)
                        if t < nblk:
                            F2(t)
                        if t >= 1:
                            B2(t - 1)
                    b = nev % 2
                    nev += 1
                    ph.copy("act", ost[b][:], po[0:64, :], [pok], [f"ost{b}"])
                    ph.dma("sp", osb_scr[h * 64:(h + 1) * 64, j * 512:(j + 1) * 512], ost[b][:], [f"ost{b}"], ["osb_scr"])


def own_tiles(p):
    return [2 * (j // 2) * 2 + (0 if (j % 2 == 0) == (p == 0) else 0) for j in range(8)]


def tile_ids(p):
    out = []
    for j in range(8):
        if (j + p) % 2 == 0:
            out.append(2 * j)
        else:
            out.append(2 * j + 1)
    return out


def build(stop_after=99, debug=False, nj=8, nchunks=64, skip=()):
    nc = bass.Bass("TRN2", target_bir_lowering=False)

    def inp(name, shape, dt=F32):
        return nc.dram_tensor(name, list(shape), dt, kind="ExternalInput").ap()

    def scr(name, shape, dt=F32):
        if debug:
            return nc.dram_tensor(name, list(shape), dt, kind="ExternalOutput").ap()
        return nc.dram_tensor(name, list(shape), dt).ap()

    I = {}
    I["xT_ctx"] = inp("xT_ctx", [D, SEQ])
    I["xT_own"] = inp("xT_own", [D, NOWN])
    I["g_mix"] = inp("g_mix", [128, 8])
    I["w_ctx_fm"] = inp("w_ctx_fm", [D, 2048])
    I["w_ctx_tm"] = inp("w_ctx_tm", [D, 520])
    I["w_own"] = inp("w_own", [D, 3072])
    I["convw"] = inp("convw", [1536, 4])
    I["alog_bc"] = inp("alog_bc", [128, 4])
    I["dtb_bc"] = inp("dtb_bc", [128, 4])
    I["masks"] = inp("masks", [128, 16, 512])
    I["cst"] = inp("cst", [128, 640])
    I["memT"] = inp("memT", [D, 256])
    I["sel"] = inp("sel", [128, 16])
    I["gng"] = inp("gng", [128, 1])
    I["g_cross"] = inp("g_cross", [128, 8])
    I["g_mem"] = inp("g_mem", [128, 8])
    I["g_peer"] = inp("g_peer", [128, 8])
    I["g_final"] = inp("g_final", [128, 8])
    I["w_sb_up"] = inp("w_sb_up", [512, D])
    I["w_gdn_up"] = inp("w_gdn_up", [512, D])
    I["w_mix_out"] = inp("w_mix_out", [D, D])
    I["w_cq"] = inp("w_cq", [D, D])
    I["w_ckv"] = inp("w_ckv", [D, 2 * D])
    I["w_co"] = inp("w_co", [D, D])
    I["w_pq"] = inp("w_pq", [D, 2048])
    I["skT"] = inp("skT", [128, 16, 128])
    I["sel48"] = inp("sel48", [48, 16, 128])
    I["uT"] = inp("uT", [D, 16384])
    I["pv"] = inp("pv", [16384, D])
    outT = nc.dram_tensor("outT", [D, NOWN], F32, kind="ExternalOutput").ap()

    S = {}
    S["kT"] = scr("kT_scr", [512, SEQ], BF16)
    S["v"] = scr("v_scr", [SEQ, 512], BF16)
    S["gq"] = scr("gq_scr", [512, SEQ])
    S["gk"] = scr("gk_scr", [512, SEQ])
    S["gv"] = scr("gv_scr", [512, SEQ])
    S["gb"] = scr("gb_scr", [SEQ, 8])
    S["qT"] = scr("qT_scr", [512, NOWN], BF16)
    S["zT"] = scr("zT_scr", [512, NOWN])
    S["gate"] = scr("gate_scr", [2048, NOWN], BF16)
    S["osb"] = scr("osb_scr", [512, NOWN], BF16)
    S["og"] = scr("og_scr", [512, SEQ])
    S["x2"] = scr("x2_scr", [D, NOWN])
    if debug:
        S["x1"] = scr("x1_scr", [D, NOWN])

    if stop_after >= 1 and 1 not in skip:
        phase_inproj_ctx(nc, I["xT_ctx"], I["g_mix"], I["w_ctx_fm"], I["w_ctx_tm"], I["convw"],
                         I["alog_bc"], I["dtb_bc"], S["kT"], S["v"], S["gq"], S["gk"], S["gv"], S["gb"])
    if stop_after >= 2 and 2 not in skip:
        phase_inproj_own(nc, I["xT_own"], I["g_mix"], I["w_own"], S["qT"], S["zT"], S["gate"])
    if stop_after >= 3 and 3 not in skip:
        phase_sb(nc, S["qT"], S["kT"], S["v"], I["masks"], I["cst"], S["osb"], nj=nj)
    if stop_after >= 4 and 4 not in skip:
        phase_gdn(nc, S["gq"], S["gk"], S["gv"], S["gb"], I["cst"], S["og"], nchunks=nchunks)
    if stop_after >= 5:
        phase_merge_cross(nc, I["xT_own"], I["memT"], S["og"], S["osb"], S["zT"], S["gate"], I["sel"], I["gng"],
                          I["g_cross"], I["g_mem"], I["w_sb_up"], I["w_gdn_up"], I["w_mix_out"], I["w_cq"],
                          I["w_ckv"], I["w_co"], S["x2"], x1_dbg=S["x1"] if debug else None)
    if stop_after >= 6:
        phase_peer(nc, S["x2"], I["g_peer"], I["g_final"], I["w_pq"], I["skT"], I["sel48"], I["cst"],
                   I["uT"], I["pv"], outT)
    return nc


def make_consts():
    i = np.arange(128)
    low = (i[:, None] >= i[None, :]).astype(np.float32)
    lows = (i[:, None] > i[None, :]).astype(np.float32)
    up = (i[:, None] <= i[None, :]).astype(np.float32)
    ident = np.eye(128, dtype=np.float32)
    return np.concatenate([low, lows, up, ident, -low], axis=1)


def make_masks(p):
    m = np.zeros((128, 2, 2, 4, 512), np.float32)
    k = np.arange(128)[:, None]
    q = np.arange(512)[None, :]
    diag = np.stack([np.where(kk * 128 + k < q, 0.0, NEG) for kk in range(4)], 0)
    full = np.zeros((4, 128, 512), np.float32)
    zero = np.full((4, 128, 512), NEG, np.float32)
    for jp in range(2):
        if (p + jp) % 2 == 0:
            u0, u1 = diag, zero
        else:
            u0, u1 = full, diag
        m[:, jp, 0] = u0.transpose(1, 0, 2)
        m[:, jp, 1] = u1.transpose(1, 0, 2)
    return np.ascontiguousarray(m.reshape(128, 16, 512))


def prepare(inputs):
    f = lambda a: np.ascontiguousarray(np.asarray(a, dtype=np.float32))
    x = f(inputs["x"])
    w_in = f(inputs["w_in"])[0]
    o = np.cumsum([0, 512, 512, 512, 512, 512, 512, 512, 4, 4, 1024, 1024])
    col = lambda i: w_in[:, o[i]:o[i + 1]]
    w_ctx_fm = f(np.concatenate([col(1), col(3), col(4), col(5)], 1))
    w_ctx_tm = f(np.concatenate([col(2), col(7), col(8)], 1))
    w_own = f(np.concatenate([col(0), col(6), col(9), col(10)], 1))
    pc8 = lambda a: f(f(a)[0].reshape(8, 128).T)
    mem = f(inputs["mem"])
    shared = dict(
        g_mix=f(f(inputs["g_mix"])[0].reshape(8, 128).T), w_ctx_fm=w_ctx_fm, w_ctx_tm=w_ctx_tm, w_own=w_own,
        convw=f(f(inputs["gdn_conv"])[0].T),
        alog_bc=f(np.broadcast_to(f(inputs["gdn_a_log"])[0][None, :], (128, 4))),
        dtb_bc=f(np.broadcast_to(f(inputs["gdn_dt_bias"])[0][None, :], (128, 4))),
        cst=make_consts(),
        gng=f(f(inputs["gdn_norm_g"])[0].reshape(128, 1)),
        g_cross=pc8(inputs["g_cross"]), g_mem=pc8(inputs["g_mem"]), g_peer=pc8(inputs["g_peer"]),
        g_final=f(f(inputs["g_final"]).reshape(8, 128).T),
        w_sb_up=f(inputs["w_sb_up"])[0], w_gdn_up=f(inputs["w_gdn_up"])[0], w_mix_out=f(inputs["w_mix_out"])[0],
        w_cq=f(inputs["w_cq"])[0], w_ckv=f(inputs["w_ckv"])[0], w_co=f(inputs["w_co"])[0],
        w_pq=f(inputs["w_pq"])[0],
        skT=f(f(inputs["peer_subkeys"])[0].transpose(3, 0, 1, 2).reshape(128, 16, 128)),
        sel48=f((np.arange(48)[:, None, None] % 16 == np.arange(16)[None, :, None]) * np.ones((1, 1, 128))),
        uT=f(f(inputs["peer_u"])[0].T), pv=f(inputs["peer_v"])[0],
    )
    in_maps = []
    for c in range(8):
        b, p = c // 2, c % 2
        tl = tile_ids(p)
        idx = np.concatenate([np.arange(t * 512, (t + 1) * 512) for t in tl])
        m = dict(shared)
        m["xT_ctx"] = f(x[b].T)
        m["xT_own"] = f(x[b][idx].T)
        m["masks"] = make_masks(p)
        m["memT"] = f(mem[b].T)
        cj = np.array([1.0 if (j + p) % 2 == 0 else 0.0 for j in range(8)], np.float32)
        m["sel"] = f(np.broadcast_to(np.concatenate([cj, 1.0 - cj])[None, :], (128, 16)))
        in_maps.append(m)
    return in_maps


def phase_gdn(nc, gq_scr, gk_scr, gv_scr, gb_scr, cst, og_scr, nchunks=64):
    with Phase(nc) as ph:
        cs = ph.sb([128, 640], F32)
        ones = ph.sb([128, 128], F32)
        gb = ph.sb([128, 64, 8], F32)
        nb = ph.sb([128, 64, 4], F32)
        qkv = [[ph.sb([128, 4, 512], F32) for _ in range(3)] for _ in range(2)]
        St = [ph.sb([128, 128], F32) for _ in range(4)]
        names = ["grep", "gT2", "decS", "decT", "egcb", "P0", "P1", "Q0", "Q1", "U0", "U1", "ktm", "vtm", "vb",
                 "kbg", "u", "wT", "qkT", "qdT", "kdec", "vnew", "ost"]
        bufs = [[{nm: ph.sb([128, 128], F32) for nm in names} for _ in range(2)] for _ in range(4)]
        cols = [[ph.sb([128, 8], F32) for _ in range(2)] for _ in range(4)]
        banks = [ph.ps() for _ in range(8)]
        LOW = cs[:, C_LOW:C_LOW + 128]
        LOWS = cs[:, C_LOWS:C_LOWS + 128]
        UP = cs[:, C_UP:C_UP + 128]
        IDN = cs[:, C_ID:C_ID + 128]
        pctr = [0]

        def pst():
            i = pctr[0] % 8
            pctr[0] += 1
            return banks[i][:, 0:128], f"ps{i}"

        ph.dma("sp", cs[:], cst, [], ["cs"])
        ph.dma("sp", gb[:], gb_scr.rearrange("(n p) f -> p n f", p=128), ["gb_scr"], ["gb"])
        ph.memset("dve", ones[:], 1.0, ["ones"])
        for h in range(4):
            ph.memset("pool", St[h][:], 0.0, [f"S{h}"])
        ph.ts("dve", nb[:], gb[:, :, 0:4], -1.0, ALU.mult, ["gb"], ["nb"])

        for n in range(nchunks):
            par = n % 2
            grp, gi = n // 4, n % 4
            gs = grp % 2
            if gi == 0:
                for ti, src in enumerate((gq_scr, gk_scr, gv_scr)):
                    ph.dma("sp", qkv[gs][ti][:], src.rearrange("(h p) t -> p h t", p=128)[:, :, grp * 512:(grp + 1) * 512],
                           ["g_scr"], [f"qkv{gs}{ti}"])
            tsl = slice(gi * 128, (gi + 1) * 128)
            qT = [qkv[gs][0][:, h, tsl] for h in range(4)]
            kT = [qkv[gs][1][:, h, tsl] for h in range(4)]
            vT = [qkv[gs][2][:, h, tsl] for h in range(4)]
            kq, kk_, kv = f"qkv{gs}0", f"qkv{gs}1", f"qkv{gs}2"
            B = [bufs[h][par] for h in range(4)]
            K = lambda nm, h: f"{nm}{h}{par}"
            CL = [cols[h][par] for h in range(4)]
            gcol = [gb[:, n, 4 + h:5 + h] for h in range(4)]
            bcol = [gb[:, n, h:h + 1] for h in range(4)]
            nbcol = [nb[:, n, h:h + 1] for h in range(4)]
            pd, pdT, pgb, pc, pkk, pqk = {}, {}, {}, {}, {}, {}
            for h in range(4):
                ph.ts("dve", B[h]["gT2"][:], LOWS, gcol[h], ALU.mult, ["cs", "gb"], [K("gT2", h)])
                ph.ts("dve", B[h]["grep"][:], ones[:], gcol[h], ALU.mult, ["ones", "gb"], [K("grep", h)])
                p1, k1 = pst()
                ph.mm(p1, kT[h], IDN, True, True, [kk_, "cs"], [k1])
                ph.copy("act", B[h]["ktm"][:], p1, [k1], [K("ktm", h)])
                p2, k2 = pst()
                ph.mm(p2, vT[h], IDN, True, True, [kv, "cs"], [k2])
                ph.copy("dve", B[h]["vtm"][:], p2, [k2], [K("vtm", h)])
            for h in range(4):
                x, xk = pst()
                ph.mm(x, UP, B[h]["gT2"][:], True, True, ["cs", K("gT2", h)], [xk])
                ph.act(B[h]["decS"][:], x, AF.Exp, [xk], [K("decS", h)])
                x, xk = pst()
                ph.mm(x, B[h]["gT2"][:], UP, True, True, ["cs", K("gT2", h)], [xk])
                ph.act(B[h]["decT"][:], x, AF.Exp, [xk], [K("decT", h)])
                x, xk = pst()
                ph.mm(x, B[h]["grep"][:], UP, True, True, ["cs", K("grep", h)], [xk])
                ph.act(B[h]["egcb"][:], x, AF.Exp, [xk], [K("egcb", h)])
                for q3, lm in enumerate((UP, LOWS, ones[:])):
                    x, xk = pst()
                    ph.mm(x, lm, B[h]["grep"][:], True, True, ["cs", "ones", K("grep", h)], [xk])
                    ph.act(CL[h][:, q3:q3 + 1], x[:, 0:1], AF.Exp, [xk], [K("col", h)])
                ph.tt("pool", B[h]["decS"][:], B[h]["decS"][:], LOWS, ALU.mult, [K("decS", h), "cs"], [K("decS", h)])
                ph.tt("pool", B[h]["decT"][:], B[h]["decT"][:], UP, ALU.mult, [K("decT", h), "cs"], [K("decT", h)])
                ph.tt("dve", CL[h][:, 3:4], CL[h][:, 0:1], bcol[h], ALU.mult, [K("col", h), "gb"], [K("col", h)])
                x, xk = pst()
                ph.mm(x, kT[h], kT[h], True, True, [kk_], [xk])
                ph.stt(B[h]["P0"][:], x, nbcol[h], B[h]["decS"][:], ALU.mult, ALU.mult,
                       [xk, "nb", K("decS", h)], [K("P0", h)])
                x, xk = pst()
                ph.mm(x, kT[h], qT[h], True, True, [kk_, kq], [xk])
                ph.tt("dve", B[h]["qkT"][:], x, B[h]["decT"][:], ALU.mult, [xk, K("decT", h)], [K("qkT", h)])
                ph.tt("pool", B[h]["qdT"][:], qT[h], B[h]["egcb"][:], ALU.mult, [kq, K("egcb", h)], [K("qdT", h)])
                ph.ts("dve", B[h]["kdec"][:], B[h]["ktm"][:], CL[h][:, 1:2], ALU.mult, [K("ktm", h), K("col", h)], [K("kdec", h)])
                ph.ts("dve", B[h]["vb"][:], B[h]["vtm"][:], bcol[h], ALU.mult, [K("vtm", h), "gb"], [K("vb", h)])
                ph.ts("dve", B[h]["kbg"][:], B[h]["ktm"][:], CL[h][:, 3:4], ALU.mult, [K("ktm", h), K("col", h)], [K("kbg", h)])
            for h in range(4):
                p1, k1 = pst()
                ph.mm(p1, B[h]["P0"][:], IDN, True, True, [K("P0", h), "cs"], [k1])
                ph.copy("act", B[h]["Q0"][:], p1, [k1], [K("Q0", h)])
                ph.tt("dve", B[h]["U0"][:], p1, IDN, ALU.add, [k1, "cs"], [K("U0", h)])
            for k in range(1, 7):
                a, b = (k - 1) % 2, k % 2
                for h in range(4):
                    Pp, Qp = B[h][f"P{a}"], B[h][f"Q{a}"]
                    p1, k1 = pst()
                    ph.mm(p1, Qp[:], Pp[:], True, True, [K(f"Q{a}", h), K(f"P{a}", h)], [k1])
                    ph.copy("act", B[h][f"P{b}"][:], p1, [k1], [K(f"P{b}", h)])
                if k <= 5:
                    for h in range(4):
                        Pp, Qp = B[h][f"P{a}"], B[h][f"Q{a}"]
                        p2, k2 = pst()
                        ph.mm(p2, Pp[:], Qp[:], True, True, [K(f"Q{a}", h), K(f"P{a}", h)], [k2])
                        ph.copy("dve", B[h][f"Q{b}"][:], p2, [k2], [K(f"Q{b}", h)])
                for h in range(4):
                    Up, Un, Pn = B[h][f"U{a}"], B[h][f"U{b}"], B[h][f"P{b}"]
                    p3, k3 = pst()
                    ph.mm(p3, Pn[:], Up[:], True, True, [K(f"P{b}", h), K(f"U{a}", h)], [k3])
                    ph.tt("dve", Un[:], Up[:], p3, ALU.add, [K(f"U{a}", h), k3], [K(f"U{b}", h)])
            UF = "U0"
            for h in range(4):
                p1, k1 = pst()
                ph.mm(p1, B[h][UF][:], B[h]["vb"][:], True, True, [K(UF, h), K("vb", h)], [k1])
                ph.copy("act", B[h]["u"][:], p1, [k1], [K("u", h)])
                p2, k2 = pst()
                ph.mm(p2, B[h]["kbg"][:], B[h][UF][:], True, True, [K(UF, h), K("kbg", h)], [k2])
                ph.copy("dve", B[h]["wT"][:], p2, [k2], [K("wT", h)])
            pw = {}
            for h in range(4):
                p1, k1 = pst()
                ph.mm(p1, B[h]["wT"][:], St[h][:], True, True, [K("wT", h), f"S{h}"], [k1])
                ph.tt("dve", B[h]["vnew"][:], B[h]["u"][:], p1, ALU.subtract, [K("u", h), k1], [K("vnew", h)])
            for h in range(4):
                p2, k2 = pst()
                ph.mm(p2, St[h][:], B[h]["qdT"][:], True, False, [f"S{h}", K("qdT", h)], [k2])
                ph.mm(p2, B[h]["vnew"][:], B[h]["qkT"][:], False, True, [K("vnew", h), K("qkT", h)], [k2])
                ph.copy("act", B[h]["ost"][:], p2, [k2], [K("ost", h)])
                ph.dma("sp", og_scr[h * 128:(h + 1) * 128, n * 128:(n + 1) * 128], B[h]["ost"][:], [K("ost", h)], ["og_scr"])
            for h in range(4):
                p3, k3 = pst()
                ph.mm(p3, B[h]["kdec"][:], B[h]["vnew"][:], True, True, [K("kdec", h), K("vnew", h)], [k3])
                ph.stt(St[h][:], St[h][:], CL[h][:, 2:3], p3, ALU.mult, ALU.add, [f"S{h}", K("col", h), k3], [f"S{h}"])

def phase_merge_cross(nc, xT_own, memT, og_scr, osb_scr, zT_scr, gate_scr, sel, gng, g_cross, g_mem,
                      w_sb_up, w_gdn_up, w_mix_out, w_cq, w_ckv, w_co, x2_scr, x1_dbg=None):
    NT = NOWN // 512
    with Phase(nc) as ph:
        wsb = ph.sb([64, 8, 1024], BF16)
        wgu = ph.sb([128, 4, 1024], BF16)
        wmo = ph.sb([128, 8, 1024], BF16)
        wcq = ph.sb([128, 8, 1024], BF16)
        wco = ph.sb([128, 8, 1024], BF16)
        kxT = ph.sb([128, 8, 256], BF16)
        vx = ph.sb([128, 2, 1024], BF16)
        ones32 = ph.sb([128, 128], F32)
        ones16 = ph.sb([128, 128], BF16)
        selt = ph.sb([128, 16], F32)
        gn = ph.sb([128, 1], F32)
        gcc = ph.sb([128, 8], F32)
        gmc = ph.sb([128, 8], F32)
        xt = ph.sb([128, 8, 512], F32)
        sq = ph.sb([128, 8, 512], F32)
        xn = ph.sb([128, 8, 512], BF16)
        rt = ph.sb([128, 512], F32)
        oga = sq[:, 0:4, :]
        ogb = sq[:, 4:8, :]
        zt = ph.sb([128, 4, 512], F32)
        ogn = ph.sb([128, 4, 512], BF16)
        osb = ph.sb([64, 8, 512], BF16)
        gt = ph.sb([128, 16, 512], BF16)
        mg = ph.sb([128, 8, 512], BF16)
        t1 = [ph.sb([128, 512], F32) for _ in range(2)]
        qx = ph.sb([128, 8, 512], BF16)
        pT = [ph.sb([128, 512], BF16) for _ in range(4)]
        oT = ph.sb([128, 8, 512], BF16)
        rden = [ph.sb([128, 512], F32) for _ in range(2)]
        ps_stat = ph.ps()
        ps_a = [ph.ps() for _ in range(2)]
        ps_b = [ph.ps() for _ in range(2)]
        ps_c = [ph.ps() for _ in range(2)]

        ph.dma("pool", wsb[:], w_sb_up.rearrange("(h d) f -> d h f", d=64), [], ["wsb"])
        ph.dma("pool", wgu[:], w_gdn_up.rearrange("(h d) f -> d h f", d=128), [], ["wgu"])
        for q in range(2):
            hs = slice(4 * q, 4 * q + 4)
            ph.dma("pool", wmo[:, hs, :], fm(w_mix_out, 0, 1024)[:, hs, :], [], ["wmo"])
            ph.dma("pool", wcq[:, hs, :], fm(w_cq, 0, 1024)[:, hs, :], [], ["wcq"])
            ph.dma("pool", wco[:, hs, :], fm(w_co, 0, 1024)[:, hs, :], [], ["wco"])
        ph.dma("sp", selt[:], sel, [], ["selt"])
        ph.dma("sp", gn[:], gng, [], ["gn"])
        ph.dma("sp", gcc[:], g_cross, [], ["gcol"])
        ph.dma("sp", gmc[:], g_mem, [], ["gmc"])
        ph.memset("dve", ones32[:], 1.0, ["ones32"])
        ph.memset("dve", ones16[:], 1.0, ["ones16"])

        ph.dma("sp", xt[:, :, 0:256], fm(memT, 0, 256), [], ["xt0"])
        for c in range(8):
            ph.act(sq[:, c, 0:256], xt[:, c, 0:256], AF.Square, ["xt0"], ["sq"])
        for c in range(8):
            ph.mm(ps_stat[:, 0:256], ones32[:], sq[:, c, 0:256], c == 0, c == 7, ["sq", "ones32"], ["ps_stat"])
        ph.act(rt[:, 0:256], ps_stat[:, 0:256], AF.Sqrt, ["ps_stat"], ["rt"], bias=EPS, scale=1.0 / 1024)
        ph.recip(rt[:, 0:256], rt[:, 0:256], ["rt"], ["rt"])
        for c in range(8):
            ph.stt(xn[:, c, 0:256], xt[:, c, 0:256], gmc[:, c:c + 1], rt[:, 0:256], ALU.mult, ALU.mult,
                   ["xt0", "rt", "gmc"], ["xn0"])
        wk = gt[:, 0:8, :]
        for blk in range(4):
            ph.dma("pool", wk, fm(w_ckv, 0, 2048)[:, :, blk * 512:(blk + 1) * 512], [], ["gt"])
            if blk < 2:
                for ff in range(4):
                    f = blk * 4 + ff
                    pb = ps_a[ff % 2]
                    for c in range(8):
                        ph.mm(pb[:, 0:256], wk[:, c, ff * 128:(ff + 1) * 128], xn[:, c, 0:256], c == 0, c == 7,
                              ["gt", "xn0"], [f"ps_a{ff % 2}"])
                    ph.copy("act", kxT[:, f, :], pb[:, 0:256], [f"ps_a{ff % 2}"], ["kxT"])
            else:
                for mt in range(2):
                    pb = ps_a[mt]
                    for c in range(8):
                        ph.mm(pb[:], xn[:, c, mt * 128:(mt + 1) * 128], wk[:, c, :], c == 0, c == 7,
                              ["gt", "xn0"], [f"ps_a{mt}"])
                    ph.copy("act", vx[:, mt, (blk - 2) * 512:(blk - 1) * 512], pb[:], [f"ps_a{mt}"], ["vx"])

        for j in range(NT):
            t0 = j * 512
            ph.dma("sp", xt[:, 0:4, :], fm(xT_own, t0, 512)[:, 0:4, :], [], ["xt0"])
            ph.dma("sp", xt[:, 4:8, :], fm(xT_own, t0, 512)[:, 4:8, :], [], ["xt0"])
            ogv = og_scr.rearrange("(h p) t -> p h t", p=128)
            ph.dma("sp", oga, ogv[:, :, (2 * j) * 512:(2 * j + 1) * 512], ["og_scr"], ["sq"])
            ph.dma("sp", ogb, ogv[:, :, (2 * j + 1) * 512:(2 * j + 2) * 512], ["og_scr"], ["sq"])
            ph.dma("sp", zt[:], zT_scr.rearrange("(h p) t -> p h t", p=128)[:, :, t0:t0 + 512], ["zT_scr"], ["zt"])
            ph.dma("sp", osb[:], osb_scr.rearrange("(h d) t -> d h t", d=64)[:, :, t0:t0 + 512], ["osb_scr"], ["osb"])
            gv_ = gate_scr.rearrange("(f p) t -> p f t", p=128)
            ph.dma("sp", gt[:, 0:8, :], gv_[:, 0:8, t0:t0 + 512], ["gate_scr"], ["gt"])
            ph.dma("sp", gt[:, 8:16, :], gv_[:, 8:16, t0:t0 + 512], ["gate_scr"], ["gt"])
            ph.ts("dve", oga, oga, selt[:, j:j + 1], ALU.mult, ["sq", "selt"], ["sq"])
            ph.stt(oga, ogb, selt[:, 8 + j:9 + j], oga, ALU.mult, ALU.add, ["sq", "selt"], ["sq"])
            for hh in range(4):
                pb = ps_a[hh % 2]
                pk = f"ps_a{hh % 2}"
                ph.act(rt[:], oga[:, hh, :], AF.Square, ["sq"], ["rt"])
                ph.mm(pb[:], ones32[:], rt[:], True, True, ["ones32", "rt"], [pk])
                ph.act(t1[hh % 2][:], pb[:], AF.Sqrt, [pk], [f"t1{hh % 2}"], bias=EPS, scale=1.0 / 128)
                ph.recip(t1[hh % 2][:], t1[hh % 2][:], [f"t1{hh % 2}"], [f"t1{hh % 2}"])
                ph.stt(t1[hh % 2][:], oga[:, hh, :], gn[:, 0:1], t1[hh % 2][:], ALU.mult, ALU.mult,
                       ["sq", "gn", f"t1{hh % 2}"], [f"t1{hh % 2}"])
                ph.tt("dve", ogn[:, hh, :], t1[hh % 2][:], zt[:, hh, :], ALU.mult, [f"t1{hh % 2}", "zt"], ["ogn"])
            for f in range(8):
                pa, pak = ps_a[f % 2], f"ps_a{f % 2}"
                pb, pbk = ps_b[f % 2], f"ps_b{f % 2}"
                for h in range(8):
                    ph.mm(pa[:], wsb[:, h, f * 128:(f + 1) * 128], osb[:, h, :], h == 0, h == 7, ["wsb", "osb"], [pak])
                for h in range(4):
                    ph.mm(pb[:], wgu[:, h, f * 128:(f + 1) * 128], ogn[:, h, :], h == 0, h == 3, ["wgu", "ogn"], [pbk])
                ph.tt("dve", t1[0][:], pa[:], gt[:, f, :], ALU.mult, [pak, "gt"], ["t10"])
                ph.tt("dve", t1[1][:], pb[:], gt[:, 8 + f, :], ALU.mult, [pbk, "gt"], ["t11"])
                ph.tt("pool", mg[:, f, :], t1[0][:], t1[1][:], ALU.add, ["t10", "t11"], ["mg"])
            for f in range(8):
                pc, pck = ps_c[f % 2], f"ps_c{f % 2}"
                for c in range(8):
                    ph.mm(pc[:], wmo[:, c, f * 128:(f + 1) * 128], mg[:, c, :], c == 0, c == 7, ["wmo", "mg"], [pck])
                ph.tt("dve", xt[:, f, :], xt[:, f, :], pc[:], ALU.add, ["xt0", pck], ["xt0"])
            if x1_dbg is not None:
                ph.dma("sp", fm(x1_dbg, t0, 512), xt[:], ["xt0"], ["x1_dbg"])
            norm_tile(ph, xt, sq, xn, gcc, ones32, ps_stat, rt, 0, None)
            for f in range(8):
                pc, pck = ps_c[f % 2], f"ps_c{f % 2}"
                for c in range(8):
                    ph.mm(pc[:], wcq[:, c, f * 128:(f + 1) * 128], xn[:, c, :], c == 0, c == 7, ["wcq", "xn0"], [pck])
                ph.act(qx[:, f, :], pc[:], AF.Copy, [pck], ["qx"], scale=1.0 / 16)
            for hd in range(4):
                for mt in range(2):
                    pa, pak = ps_a[mt], f"ps_a{mt}"
                    for cc in range(2):
                        ph.mm(pa[:], kxT[:, 2 * hd + cc, mt * 128:(mt + 1) * 128], qx[:, 2 * hd + cc, :], cc == 0, cc == 1,
                              ["kxT", "qx"], [pak])
                    i4 = (hd % 2) * 2 + mt
                    ph.act(pT[i4][:], pa[:], AF.Exp, [pak], [f"pT{i4}"])
                pb, pbk = ps_b[hd % 2], f"ps_b{hd % 2}"
                for mt in range(2):
                    i4 = (hd % 2) * 2 + mt
                    ph.mm(pb[:], ones16[:], pT[i4][:], mt == 0, mt == 1, ["ones16", f"pT{i4}"], [pbk])
                rd = rden[hd % 2]
                ph.recip(rd[:], pb[:], [pbk], [f"rden{hd % 2}"])
                for cc in range(2):
                    pc, pck = ps_c[cc], f"ps_c{cc}"
                    for mt in range(2):
                        i4 = (hd % 2) * 2 + mt
                        ph.mm(pc[:], vx[:, mt, hd * 256 + cc * 128:hd * 256 + (cc + 1) * 128], pT[i4][:], mt == 0, mt == 1,
                              ["vx", f"pT{i4}"], [pck])
                    ph.tt("dve", oT[:, 2 * hd + cc, :], pc[:], rd[:], ALU.mult, [pck, f"rden{hd % 2}"], ["oT"])
            for f in range(8):
                pc, pck = ps_c[f % 2], f"ps_c{f % 2}"
                for c in range(8):
                    ph.mm(pc[:], wco[:, c, f * 128:(f + 1) * 128], oT[:, c, :], c == 0, c == 7, ["wco", "oT"], [pck])
                ph.tt("dve", xt[:, f, :], xt[:, f, :], pc[:], ALU.add, ["xt0", pck], ["xt0"])
            ph.dma("sp", fm(x2_scr, t0, 512)[:, 0:4, :], xt[:, 0:4, :], ["xt0"], ["x2_scr"])
            ph.dma("sp", fm(x2_scr, t0, 512)[:, 4:8, :], xt[:, 4:8, :], ["xt0"], ["x2_scr"])


DELTA = 2e-5


def phase_peer(nc, x2_scr, g_peer, g_final, w_pq, skT_in, sel_in, cst, uT, pv, outT, ntiles=8, negroups=32):
    with Phase(nc) as ph:
        wpq = ph.sb([128, 8, 2048], BF16)
        skT = ph.sb([128, 16, 128], BF16)
        selm = ph.sb([48, 16, 128], BF16)
        idb = ph.sb([128, 128], BF16)
        ones32 = ph.sb([128, 128], F32)
        gpc = ph.sb([128, 8], F32)
        gfc = ph.sb([128, 8], F32)
        xt = ph.sb([128, 8, 512], F32)
        sq = ph.sb([128, 8, 512], F32)
        xn = ph.sb([128, 8, 512], BF16)
        rt = ph.sb([128, 512], F32)
        qT = ph.sb([128, 16, 512], BF16)
        top = ph.sb([128, 16, 16], F32)
        cand = ph.sb([128, 256], F32)
        cand2 = ph.sb([128, 256], F32)
        best = ph.sb([128, 16], F32)
        junk = ph.sb([128, 16], F32)
        sm = ph.sb([128, 8], F32)
        rws = ph.sb([128, 16], F32)
        res = ph.sb([128, 16], F32)
        spl = ph.sb([128, 48], BF16)
        rows = ph.sb([48, 512], BF16)
        theta = ph.sb([128, 8, 512], F32)
        ug = [ph.sb([128, 8, 512], BF16) for _ in range(2)]
        vg = [ph.sb([128, 4, 1024], BF16) for _ in range(2)]
        gA = [ph.sb([128, 512], F32) for _ in range(2)]
        EE = [ph.sb([128, 512], BF16) for _ in range(4)]
        MK = [ph.sb([128, 512], BF16) for _ in range(4)]
        GH = [ph.sb([128, 512], BF16) for _ in range(6)]
        Hg = [ph.sb([128, 4, 512], BF16) for _ in range(2)]
        ps_q = [ph.ps() for _ in range(2)]
        ps_stat = ps_q[0]
        ps_A = [ph.ps()]
        ps_P = [ph.ps() for _ in range(4)]
        ps_O = ph.ps()

        for q in range(4):
            ph.dma("pool", wpq[:, 2 * q:2 * q + 2, :], fm(w_pq, 0, 2048)[:, 2 * q:2 * q + 2, :], [], ["wpq"])
        ph.dma("pool", skT[:], skT_in, [], ["skT"])
        ph.dma("pool", selm[:], sel_in, [], ["selm"])
        ph.dma("pool", idb[:], cst[:, C_ID:C_ID + 128], [], ["idb"])
        ph.dma("sp", gpc[:], g_peer, [], ["gcol"])
        ph.dma("sp", gfc[:], g_final, [], ["gfc"])
        ph.memset("dve", ones32[:], 1.0, ["ones32"])

        s_tm = sq[:, 0:4, :].rearrange("p a (b c) -> p (a b) c", c=128)
        s_w = sq[:, 4:8, :].rearrange("p a (b c) -> p (a b) c", c=128)
        ndma = 0
        for j in range(ntiles):
            t0 = j * 512
            ph.dma("sp", xt[:, 0:4, :], fm(x2_scr, t0, 512)[:, 0:4, :], ["x2_scr"], ["xt0"])
            ph.dma("sp", xt[:, 4:8, :], fm(x2_scr, t0, 512)[:, 4:8, :], ["x2_scr"], ["xt0"])
            norm_tile(ph, xt, sq, xn, gpc, ones32, ps_stat, rt, 0, "ps_q0")
            for f in range(16):
                pb, pk = ps_q[f % 2], f"ps_q{f % 2}"
                for c in range(8):
                    ph.mm(pb[:], wpq[:, c, f * 128:(f + 1) * 128], xn[:, c, :], c == 0, c == 7, ["wpq", "xn0"], [pk])
                ph.copy("act", qT[:, f, :], pb[:], [pk], ["qT"])
            for u in range(4):
                us = slice(u * 128, (u + 1) * 128)
                for g4 in range(4):
                    pb, pk = ps_q[g4 % 2], f"ps_q{g4 % 2}"
                    for k4 in range(4):
                        hp = g4 * 4 + k4
                        ph.mm(pb[:, k4 * 128:(k4 + 1) * 128], qT[:, hp, us], skT[:, hp, :], True, True, ["qT", "skT"], [pk])
                    ph.copy("act", s_tm[:, g4 * 4:(g4 + 1) * 4, :], pb[:].rearrange("p (a c) -> p a c", c=128), [pk], ["sq"])
                for hp in range(16):
                    ph.P.add("dve", lambda e, hp=hp: e.max(out=top[:, hp, 0:8], in_=s_tm[:, hp, :]), ["sq"], ["top"])
                    ph.P.add("dve", lambda e, hp=hp: e.match_replace(out=s_w[:, hp, :], in_to_replace=top[:, hp, 0:8],
                                                                     in_values=s_tm[:, hp, :], imm_value=-1e30),
                             ["sq", "top"], ["sq"])
                    ph.P.add("dve", lambda e, hp=hp: e.max(out=top[:, hp, 8:16], in_=s_w[:, hp, :]), ["sq"], ["top"])
                for h in range(8):
                    c3 = cand[:].rearrange("p (a b) -> p a b", a=16)
                    in0 = top[:, 2 * h, :].rearrange("p (a o) -> p a o", o=1).to_broadcast([128, 16, 16])
                    in1 = top[:, 2 * h + 1, :].rearrange("p (o b) -> p o b", o=1).to_broadcast([128, 16, 16])
                    ph.tt("dve", c3, in0, in1, ALU.add, ["top"], ["cand"])
                    ph.P.add("dve", lambda e: e.max(out=best[:, 0:8], in_=cand[:]), ["cand"], ["best"])
                    ph.P.add("dve", lambda e: e.match_replace(out=cand2[:], in_to_replace=best[:, 0:8], in_values=cand[:],
                                                              imm_value=-1e30), ["cand", "best"], ["cand2"])
                    ph.P.add("dve", lambda e: e.max(out=best[:, 8:16], in_=cand2[:]), ["cand2"], ["best"])
                    ph.ts("dve", sm[:, 0:1], best[:, 0:1], -1.0, ALU.mult, ["best"], ["sm"])
                    ph.memset("dve", sm[:, 1:2], 0.0, ["sm"])
                    ph.act(junk[:], best[:], AF.Exp, ["best", "sm"], ["junk", "sm"], bias=sm[:, 0:1], scale=1.0,
                           accum_out=sm[:, 1:2])
                    ph.act(sm[:, 2:3], sm[:, 1:2], AF.Ln, ["sm"], ["sm"])
                    ph.tt("dve", rws[:, 8 + h:9 + h], sm[:, 0:1], sm[:, 2:3], ALU.subtract, ["sm"], ["rws"])
                    ph.stt(rws[:, h:h + 1], best[:, 15:16], -DELTA, rws[:, 8 + h:9 + h], ALU.add, ALU.add,
                           ["best", "rws"], ["rws"])
                ph.copy("dve", spl[:, 0:16], rws[:], ["rws"], ["spl"])
                ph.tt("dve", res[:], rws[:], spl[:, 0:16], ALU.subtract, ["rws", "spl"], ["res"])
                ph.copy("dve", spl[:, 16:32], res[:], ["res"], ["spl"])
                ph.tt("dve", res[:], res[:], spl[:, 16:32], ALU.subtract, ["res", "spl"], ["res"])
                ph.copy("dve", spl[:, 32:48], res[:], ["res"], ["spl"])
                pb, pk = ps_q[u % 2], f"ps_q{u % 2}"
                ph.mm(pb[0:48, 0:128], spl[:], idb[:], True, True, ["spl", "idb"], [pk])
                ph.copy("act", rows[:, us], pb[0:48, 0:128], [pk], ["rows"])
            for h in range(8):
                pb, pk = ps_q[h % 2], f"ps_q{h % 2}"
                ph.mm(pb[:], selm[:, h, :], rows[:], True, True, ["selm", "rows"], [pk])
                ph.copy("act", theta[:, h, :], pb[:], [pk], ["theta"])
            it = 0
            def load_eg(eg):
                gs = eg % 2
                ph.dma("pool", ug[gs][:], fm(uT, eg * 512, 512), [], [f"ug{gs}"])
                ph.dma("pool", vg[gs][:], pv.rearrange("(g p) d -> p g d", p=128)[:, eg * 4:(eg + 1) * 4, :], [], [f"vg{gs}"])

            load_eg(0)
            for eg in range(negroups):
                gs = eg % 2
                if eg + 1 < negroups:
                    load_eg(eg + 1)
                def SA(ii):
                    i = eg * 4 + ii
                    a2 = i % 2
                    for c in range(8):
                        ph.mm(ps_A[0][:], ug[gs][:, c, ii * 128:(ii + 1) * 128], xn[:, c, :], c == 0, c == 7,
                              [f"ug{gs}", "xn0"], ["ps_A0"])
                    ph.act(gA[a2][:], ps_A[0][:], AF.Gelu, ["ps_A0"], [f"gA{a2}"])

                def A_(n):
                    ii, h = n // 8, n % 8
                    i = eg * 4 + ii
                    g = base + n
                    p4 = g % 4
                    pp, ppk = ps_P[p4], f"ps_P{p4}"
                    ph.mm(pp[:], skT[:, 2 * h + 1, :], qT[:, 2 * h + 1, :], True, False, ["skT", "qT"], [ppk])
                    ph.mm(pp[:], skT[:, 2 * h, i:i + 1].to_broadcast([128, 128]), qT[:, 2 * h, :], False, False,
                          ["skT", "qT"], [ppk])
                    ph.mm(pp[:], selm[:, 8 + h, :], rows[:], False, True, ["selm", "rows"], [ppk])
                    ph.act(EE[p4][:], pp[:], AF.Exp, [ppk], [f"EE{p4}"])
                    ph.tt("dve", MK[p4][:], pp[:], theta[:, h, :], ALU.is_ge, [ppk, "theta"], [f"MK{p4}"])

                def B_(n):
                    g = base + n
                    p4, p6 = g % 4, g % 6
                    ph.tt("dve", GH[p6][:], MK[p4][:], EE[p4][:], ALU.mult, [f"MK{p4}", f"EE{p4}"], [f"GH{p6}"])

                def C_(n):
                    ii, h = n // 8, n % 8
                    i = eg * 4 + ii
                    a2 = i % 2
                    p6 = (base + n) % 6
                    ph.mm(ps_q[a2][:], idb[:], GH[p6][:], h == 0, h == 7, ["idb", f"GH{p6}"], [f"ps_q{a2}"])
                    if h == 7:
                        ph.tt("dve", Hg[gs][:, ii, :], gA[a2][:], ps_q[a2][:], ALU.mult, [f"gA{a2}", f"ps_q{a2}"], [f"Hg{gs}"])

                base = it
                it += 32
                for n in range(32 + 3):
                    if n < 32:
                        if n % 8 == 0:
                            SA(n // 8)
                        A_(n)
                    if 1 <= n <= 32:
                        B_(n - 1)
                    if n >= 3:
                        C_(n - 3)
                for f in range(8):
                    for ii in range(4):
                        ph.mm(ps_O[:], vg[gs][:, ii, f * 128:(f + 1) * 128], Hg[gs][:, ii, :], ii == 0, ii == 3,
                              [f"vg{gs}", f"Hg{gs}"], ["ps_O"])
                    ph.tt("dve", xt[:, f, :], xt[:, f, :], ps_O[:], ALU.add, ["xt0", "ps_O"], ["xt0"])
            for c in range(8):
                ph.act(sq[:, c, :], xt[:, c, :], AF.Square, ["xt0"], ["sq"])
            for c in range(8):
                ph.mm(ps_stat[:], ones32[:], sq[:, c, :], c == 0, c == 7, ["sq", "ones32"], ["ps_q0"])
            ph.act(rt[:], ps_stat[:], AF.Sqrt, ["ps_q0"], ["rt"], bias=EPS, scale=1.0 / 1024)
            ph.recip(rt[:], rt[:], ["rt"], ["rt"])
            for c in range(8):
                ph.stt(sq[:, c, :], xt[:, c, :], gfc[:, c:c + 1], rt[:], ALU.mult, ALU.mult, ["xt0", "rt", "gfc"], ["sq"])
            ph.dma("sp", fm(outT, t0, 512)[:, 0:4, :], sq[:, 0:4, :], ["sq"], ["outT"])
            ph.dma("sp", fm(outT, t0, 512)[:, 4:8, :], sq[:, 4:8, :], ["sq"], ["outT"])


_NC = None


def kernel(**inputs):
    global _NC
    if _NC is None:
        _NC = build()
    maps = prepare(inputs)
    res = run_bass_kernel_spmd(_NC, maps, core_ids=list(range(8)))
    out = np.empty((4, SEQ, D), np.float32)
    for c in range(8):
        b, p = c // 2, c % 2
        o = np.asarray(res.results[c]["outT"]).T
        for j, t in enumerate(tile_ids(p)):
            out[b, t * 512:(t + 1) * 512] = o[j * 512:(j + 1) * 512]
    return out
```

```python
import contextlib
import numpy as np
import concourse.bass as bass
import concourse.mybir as mybir
from concourse.bass_utils import run_bass_kernel_spmd

F32 = mybir.dt.float32
BF16 = mybir.dt.bfloat16
AF = mybir.ActivationFunctionType
ALU = mybir.AluOpType

D = 1024
SEQ = 8192
NOWN = 4096
EPS = 1e-6
ENGS = ("pe", "act", "dve", "pool", "sp")
NEG = -30000.0


class Op:
    __slots__ = ("eng", "fn", "idx", "dma", "deps", "waits", "signal", "rank",
                 "dma_sem", "dma_val")

    def __init__(self, eng, fn, idx, dma):
        self.eng = eng
        self.fn = fn
        self.idx = idx
        self.dma = dma
        self.deps = ()
        self.waits = None
        self.signal = False
        self.rank = 0
        self.dma_sem = -1
        self.dma_val = 0


NSEM = {"pe": 18, "act": 16, "dve": 20, "pool": 6, "sp": 0}
N_DMA_SEMS = 20
SEM_CAP = 2000


class SemState:
    def __init__(self, nc):
        self.c = {e: [nc.alloc_semaphore(name=f"c_{e}_{i}") for i in range(NSEM[e])] for e in ENGS}
        self.d = [nc.alloc_semaphore(name=f"d_{i}") for i in range(N_DMA_SEMS)]
        self.rank = {e: 0 for e in ENGS}
        self.ndma = 0
        for e in ENGS:
            for h in self.c[e]:
                nc.gpsimd.sem_clear(h)
        for h in self.d:
            nc.gpsimd.sem_clear(h)
        nc.all_engine_barrier()


class Prog:
    def __init__(self, nc, sems):
        self.nc = nc
        self.sems = sems
        self.ops = {e: [] for e in ENGS}
        self.last_writer = {}
        self.readers = {}
        self.n_dma_sems = N_DMA_SEMS
        self.dma_ops = []
        self.dma_base = sems.ndma

    def add(self, eng, fn, reads=(), writes=(), dma=False):
        op = Op(eng, fn, len(self.ops[eng]), dma)
        psk = [k for k in reads if k.startswith("ps")]
        if psk:
            reads = [k for k in reads if not k.startswith("ps")]
            writes = list(writes) + [k for k in psk if k not in writes]
        deps = set()
        lw_get = self.last_writer.get
        for k in reads:
            lw = lw_get(k)
            if lw is not None:
                deps.add(lw)
        for k in writes:
            lw = lw_get(k)
            if lw is not None:
                deps.add(lw)
            rs = self.readers.get(k)
            if rs:
                deps.update(rs)
        for k in reads:
            self.readers.setdefault(k, []).append(op)
        for k in writes:
            self.last_writer[k] = op
            self.readers[k] = []
        if dma:
            jl = len(self.dma_ops)
            j = self.dma_base + jl
            op.dma_sem = j % self.n_dma_sems
            op.dma_val = 16 * (j // self.n_dma_sems + 1)
            if jl >= self.n_dma_sems:
                deps.add(self.dma_ops[jl - self.n_dma_sems])
            self.dma_ops.append(op)
        deps.discard(op)
        op.deps = tuple(deps)
        self.ops[eng].append(op)
        return op

    def dma(self, eng, out, in_, reads, writes, **kw):
        return self.add(eng, lambda e: e.dma_start(out=out, in_=in_, **kw),
                        reads, writes, dma=True)

    def finish(self):
        deps = set(self.last_writer.values())
        for rs in self.readers.values():
            deps.update(rs)
        deps.update(self.dma_ops[-self.n_dma_sems:])
        for e in ENGS:
            op = Op(e, lambda eng: None, len(self.ops[e]), False)
            op.deps = tuple(deps)
            self.ops[e].append(op)

    def emit(self):
        nc = self.nc
        for e in ENGS:
            seen = {}
            for op in self.ops[e]:
                waits = []
                for p in sorted(op.deps, key=lambda q: -q.idx):
                    if p.dma:
                        key = ("d", p.dma_sem)
                        if seen.get(key, 0) >= p.dma_val:
                            continue
                        seen[key] = p.dma_val
                        waits.append(p)
                    else:
                        if p.eng == e:
                            if e == "pe" or e == "sp":
                                continue
                            if op.idx - p.idx > 1:
                                continue
                        key = ("c", p.eng)
                        if seen.get(key, -1) >= p.idx:
                            continue
                        seen[key] = p.idx
                        p.signal = True
                        waits.append(p)
                op.waits = waits
        for e in ENGS:
            r = self.sems.rank[e]
            for op in self.ops[e]:
                if op.signal and not op.dma:
                    r += 1
                    op.rank = r
            self.sems.rank[e] = r
            assert r <= NSEM[e] * SEM_CAP, (e, r)
        self.sems.ndma += len(self.dma_ops)
        csems = self.sems.c
        dsems = self.sems.d
        with contextlib.ExitStack() as st:
            block = st.enter_context(nc.Block())

            def run(e):
                def body(eng):
                    for op in self.ops[e]:
                        for p in op.waits:
                            if p.dma:
                                eng.wait_ge(dsems[p.dma_sem], p.dma_val)
                            else:
                                k = (p.rank - 1) // SEM_CAP
                                eng.wait_ge(csems[p.eng][k], (p.rank - 1) % SEM_CAP + 1)
                        ins = op.fn(eng)
                        if ins is None:
                            assert not op.signal and not op.dma
                        elif op.dma:
                            ins.then_inc(dsems[op.dma_sem], 16)
                        elif op.signal:
                            k = (op.rank - 1) // SEM_CAP
                            ins.then_inc(csems[e][k], 1)
                return body

            block.tensor(run("pe"))
            block.scalar(run("act"))
            block.vector(run("dve"))
            block.gpsimd(run("pool"))
            block.sync(run("sp"))


class Phase:
    _cnt = [0]
    _sem = {}

    def __init__(self, nc):
        self.nc = nc
        if id(nc) not in Phase._sem:
            Phase._sem.clear()
            Phase._sem[id(nc)] = SemState(nc)
        Phase._cnt[0] += 1
        self.pid = Phase._cnt[0]

    def __enter__(self):
        self.st = contextlib.ExitStack()
        self.P = Prog(self.nc, Phase._sem[id(self.nc)])
        self.n = 0
        return self

    def sb(self, shape, dtype):
        self.n += 1
        return self.st.enter_context(self.nc.sbuf_tensor(f"t{self.pid}_{self.n}", list(shape), dtype))

    def ps(self, shape=(128, 512), dtype=F32):
        self.n += 1
        return self.st.enter_context(self.nc.psum_tensor(f"p{self.pid}_{self.n}", list(shape), dtype))

    def __exit__(self, *a):
        if a[0] is None:
            self.P.finish()
            self.P.emit()
        self.st.close()
        return False

    def mm(self, out, lhsT, rhs, start, stop, r, w):
        self.P.add("pe", lambda e: e.matmul(out, lhsT=lhsT, rhs=rhs, start=start, stop=stop), r, w)

    def act(self, out, in_, func, r, w, eng="act", **kw):
        self.P.add(eng, lambda e: e.activation(out=out, in_=in_, func=func, **kw), r, w)

    def copy(self, eng, out, in_, r, w):
        if eng == "act":
            self.P.add("act", lambda e: e.copy(out=out, in_=in_), r, w)
        else:
            self.P.add(eng, lambda e: e.tensor_copy(out=out, in_=in_), r, w)

    def tt(self, eng, out, in0, in1, op, r, w):
        self.P.add(eng, lambda e: e.tensor_tensor(out=out, in0=in0, in1=in1, op=op), r, w)

    def ts(self, eng, out, in0, s1, op0, r, w, s2=None, op1=None):
        if op1 is None:
            self.P.add(eng, lambda e: e.tensor_scalar(out=out, in0=in0, scalar1=s1, scalar2=None, op0=op0), r, w)
        else:
            self.P.add(eng, lambda e: e.tensor_scalar(out=out, in0=in0, scalar1=s1, scalar2=s2, op0=op0, op1=op1), r, w)

    def stt(self, out, in0, scalar, in1, op0, op1, r, w):
        self.P.add("dve", lambda e: e.scalar_tensor_tensor(out=out, in0=in0, scalar=scalar, in1=in1, op0=op0, op1=op1), r, w)

    def recip(self, out, in_, r, w):
        self.P.add("dve", lambda e: e.reciprocal(out=out, in_=in_), r, w)

    def memset(self, eng, ap, val, w):
        self.P.add(eng, lambda e: e.memset(ap, val), (), w)

    def dma(self, eng, out, in_, r, w):
        self.P.dma(eng, out, in_, r, w)


def fm(ap, t0, n):
    return ap.rearrange("(c p) t -> p c t", p=128)[:, :, t0:t0 + n]


def norm_tile(ph, xt, sq, xn, gcol, ones32, ps_stat, rt, slot, key, nch=8, width=512):
    for c in range(nch):
        ph.act(sq[:, c, :], xt[:, c, :], AF.Square, [f"xt{slot}"], ["sq"])
    pk = key or "ps_stat"
    for c in range(nch):
        ph.mm(ps_stat[:, 0:width], ones32[:], sq[:, c, :], c == 0, c == nch - 1, ["sq", "ones32"], [pk])
    ph.act(rt[:, 0:width], ps_stat[:, 0:width], AF.Sqrt, [pk], ["rt"], bias=EPS, scale=1.0 / (128 * nch))
    ph.recip(rt[:, 0:width], rt[:, 0:width], ["rt"], ["rt"])
    for c in range(nch):
        ph.stt(xn[:, c, :], xt[:, c, :], gcol[:, c:c + 1], rt[:, 0:width], ALU.mult, ALU.mult,
               [f"xt{slot}", "rt", "gcol"], [f"xn{slot}"])


def phase_inproj_ctx(nc, xT, g_mix, w_fm, w_tm, convw, alog_bc, dtb_bc,
                     kT_scr, v_scr, gq_scr, gk_scr, gv_scr, gb_scr):
    NT = SEQ // 512
    with Phase(nc) as ph:
        wfm = ph.sb([128, 8, 2048], BF16)
        wtm = ph.sb([128, 8, 520], BF16)
        gcol = ph.sb([128, 8], F32)
        cw = ph.sb([128, 12, 4], F32)
        alog = ph.sb([128, 4], F32)
        dtb = ph.sb([128, 4], F32)
        ones32 = ph.sb([128, 128], F32)
        xt = [ph.sb([128, 8, 512], F32) for _ in range(2)]
        sq = ph.sb([128, 8, 512], F32)
        xn = [ph.sb([128, 8, 512], BF16) for _ in range(2)]
        rt = ph.sb([128, 512], F32)
        pre = [ph.sb([128, 515], F32) for _ in range(12)]
        acc = [ph.sb([128, 512], F32) for _ in range(2)]
        sil = [ph.sb([128, 512], F32) for _ in range(2)]
        sq2 = [ph.sb([128, 512], F32) for _ in range(2)]
        rt2 = [ph.sb([128, 512], F32) for _ in range(2)]
        kst = [ph.sb([128, 512], BF16) for _ in range(2)]
        vst = [ph.sb([128, 512], BF16) for _ in range(2)]
        bat = [ph.sb([128, 8], F32) for _ in range(2)]
        ps_stat = ph.ps()
        ps_f = [ph.ps() for _ in range(2)]
        ps_n = [ph.ps() for _ in range(2)]
        ps_t = [ph.ps() for _ in range(2)]

        for q in range(4):
            ph.dma("pool", wfm[:, 2 * q:2 * q + 2, :], fm(w_fm, 0, 2048)[:, 2 * q:2 * q + 2, :], ["w_fm"], ["wfm"])
        ph.dma("pool", wtm[:], fm(w_tm, 0, 520), ["w_tm"], ["wtm"])
        ph.dma("sp", gcol[:], g_mix, [], ["gcol"])
        ph.dma("sp", cw[:], convw.rearrange("(f p) j -> p f j", p=128), [], ["cw"])
        ph.dma("sp", alog[:], alog_bc, [], ["alog"])
        ph.dma("sp", dtb[:], dtb_bc, [], ["dtb"])
        ph.memset("dve", ones32[:], 1.0, ["ones32"])
        for f in range(12):
            ph.memset("pool", pre[f][:, 0:3], 0.0, [f"pre{f}"])
        ph.act(alog[:], alog[:], AF.Exp, ["alog"], ["alog"])

        nfe = 0
        for ti in range(NT):
            s = ti % 2
            t0 = ti * 512
            ph.dma("sp", xt[s][:, 0:4, :], fm(xT, t0, 512)[:, 0:4, :], [], [f"xt{s}"])
            ph.dma("sp", xt[s][:, 4:8, :], fm(xT, t0, 512)[:, 4:8, :], [], [f"xt{s}"])
            norm_tile(ph, xt[s], sq, xn[s], gcol, ones32, ps_stat, rt, s, None)
            for f in range(16):
                pb = ps_f[f % 2]
                pk = f"ps_f{f % 2}"
                for c in range(8):
                    ph.mm(pb[:], wfm[:, c, f * 128:(f + 1) * 128], xn[s][:, c, :], c == 0, c == 7,
                          ["wfm", f"xn{s}"], [pk])
                if f < 4:
                    b = nfe % 2
                    nfe += 1
                    ph.copy("act", kst[b][:], pb[:], [pk], [f"kst{b}"])
                    ph.dma("sp", kT_scr[f * 128:(f + 1) * 128, t0:t0 + 512], kst[b][:], [f"kst{b}"], ["kT_scr"])
                    continue
                g = f - 4
                b = g % 2
                if ti > 0:
                    ph.copy("pool", pre[g][:, 0:3], pre[g][:, 512:515], [f"pre{g}"], [f"pre{g}"])
                ph.copy("act", pre[g][:, 3:515], pb[:], [pk, f"pre{g}"], [f"pre{g}"])
                ph.ts("dve", acc[b][:], pre[g][:, 0:512], cw[:, g, 0:1], ALU.mult, [f"pre{g}", "cw"], [f"acc{b}"])
                for j in range(1, 4):
                    ph.stt(acc[b][:], pre[g][:, j:j + 512], cw[:, g, j:j + 1], acc[b][:], ALU.mult, ALU.add,
                           [f"pre{g}", "cw", f"acc{b}"], [f"acc{b}"])
                ph.act(sil[b][:], acc[b][:], AF.Silu, [f"acc{b}"], [f"sil{b}"])
                if g < 8:
                    pn = ps_n[b]
                    ph.act(sq2[b][:], sil[b][:], AF.Square, [f"sil{b}"], [f"sq2{b}"])
                    ph.mm(pn[:], ones32[:], sq2[b][:], True, True, ["ones32", f"sq2{b}"], [f"ps_n{b}"])
                    ph.act(rt2[b][:], pn[:], AF.Sqrt, [f"ps_n{b}"], [f"rt2{b}"], bias=EPS, scale=1.0)
                    ph.recip(rt2[b][:], rt2[b][:], [f"rt2{b}"], [f"rt2{b}"])
                    scl = 128 ** -0.5 if g < 4 else 1.0
                    ph.stt(sil[b][:], sil[b][:], scl, rt2[b][:], ALU.mult, ALU.mult,
                           [f"sil{b}", f"rt2{b}"], [f"sil{b}"])
                dst = (gq_scr, gk_scr, gv_scr)[g // 4]
                hh = g % 4
                ph.dma("sp", dst[hh * 128:(hh + 1) * 128, t0:t0 + 512], sil[b][:], [f"sil{b}"], ["g_scr"])
            for u in range(4):
                b = u % 2
                pt = ps_t[b]
                pk = f"ps_t{b}"
                for c in range(8):
                    ph.mm(pt[:], xn[s][:, c, u * 128:(u + 1) * 128], wtm[:, c, 0:512], c == 0, c == 7,
                          [f"xn{s}", "wtm"], [pk])
                ph.copy("act", vst[b][:], pt[:], [pk], [f"vst{b}"])
                ph.dma("sp", v_scr[t0 + u * 128:t0 + (u + 1) * 128, :], vst[b][:], [f"vst{b}"], ["v_scr"])
                pn = ps_n[b]
                pnk = f"ps_n{b}"
                for c in range(8):
                    ph.mm(pn[:, 0:8], xn[s][:, c, u * 128:(u + 1) * 128], wtm[:, c, 512:520], c == 0, c == 7,
                          [f"xn{s}", "wtm"], [pnk])
                ph.act(bat[b][:, 0:4], pn[:, 0:4], AF.Sigmoid, [pnk], [f"bat{b}"])
                ph.tt("dve", bat[b][:, 4:8], pn[:, 4:8], dtb[:], ALU.add, [pnk, "dtb"], [f"bat{b}"])
                ph.act(bat[b][:, 4:8], bat[b][:, 4:8], AF.Exp, [f"bat{b}"], [f"bat{b}"])
                ph.act(bat[b][:, 4:8], bat[b][:, 4:8], AF.Ln, [f"bat{b}"], [f"bat{b}"], bias=1.0)
                ph.stt(bat[b][:, 4:8], bat[b][:, 4:8], -1.0, alog[:], ALU.mult, ALU.mult,
                       [f"bat{b}", "alog"], [f"bat{b}"])
                ph.dma("sp", gb_scr[t0 + u * 128:t0 + (u + 1) * 128, :], bat[b][:], [f"bat{b}"], ["gb_scr"])


def phase_inproj_own(nc, xT, g_mix, w_own, qT_scr, zT_scr, gate_scr):
    NT = NOWN // 512
    with Phase(nc) as ph:
        wfm = ph.sb([128, 8, 3072], BF16)
        gcol = ph.sb([128, 8], F32)
        ones32 = ph.sb([128, 128], F32)
        xt = [ph.sb([128, 8, 512], F32) for _ in range(2)]
        sq = ph.sb([128, 8, 512], F32)
        xn = [ph.sb([128, 8, 512], BF16) for _ in range(2)]
        rt = ph.sb([128, 512], F32)
        st16 = [ph.sb([128, 512], BF16) for _ in range(3)]
        st32 = [ph.sb([128, 512], F32) for _ in range(2)]
        ps_stat = ph.ps()
        ps_f = [ph.ps() for _ in range(3)]
        for q in range(8):
            ph.dma("pool", wfm[:, q:q + 1, :], fm(w_own, 0, 3072)[:, q:q + 1, :], ["w"], ["wfm"])
        ph.dma("sp", gcol[:], g_mix, [], ["gcol"])
        ph.memset("dve", ones32[:], 1.0, ["ones32"])
        n16 = 0
        n32 = 0
        for ti in range(NT):
            s = ti % 2
            t0 = ti * 512
            ph.dma("sp", xt[s][:, 0:4, :], fm(xT, t0, 512)[:, 0:4, :], [], [f"xt{s}"])
            ph.dma("sp", xt[s][:, 4:8, :], fm(xT, t0, 512)[:, 4:8, :], [], [f"xt{s}"])
            norm_tile(ph, xt[s], sq, xn[s], gcol, ones32, ps_stat, rt, s, None)
            for f in range(24):
                pb = ps_f[f % 3]
                pk = f"ps_f{f % 3}"
                for c in range(8):
                    ph.mm(pb[:], wfm[:, c, f * 128:(f + 1) * 128], xn[s][:, c, :], c == 0, c == 7,
                          ["wfm", f"xn{s}"], [pk])
                if f < 4:
                    b = n16 % 3
                    n16 += 1
                    ph.act(st16[b][:], pb[:], AF.Copy, [pk], [f"s16{b}"], scale=0.125)
                    ph.dma("sp", qT_scr[f * 128:(f + 1) * 128, t0:t0 + 512], st16[b][:], [f"s16{b}"], ["qT_scr"])
                elif f < 8:
                    b = n32 % 2
                    n32 += 1
                    ph.act(st32[b][:], pb[:], AF.Silu, [pk], [f"s32{b}"])
                    ph.dma("sp", zT_scr[(f - 4) * 128:(f - 3) * 128, t0:t0 + 512], st32[b][:], [f"s32{b}"], ["zT_scr"])
                else:
                    b = n16 % 3
                    n16 += 1
                    ph.act(st16[b][:], pb[:], AF.Sigmoid, [pk], [f"s16{b}"])
                    ph.dma("sp", gate_scr[(f - 8) * 128:(f - 7) * 128, t0:t0 + 512], st16[b][:], [f"s16{b}"], ["gate_scr"])


C_LOW, C_LOWS, C_UP, C_ID, C_TRI = 0, 128, 256, 384, 512


def phase_sb(nc, qT_scr, kT_scr, v_scr, masks, cst, osb_scr, nj=8):
    with Phase(nc) as ph:
        kT = ph.sb([128, SEQ], BF16)
        vv = ph.sb([128, 64, 128], BF16)
        qT = ph.sb([128, NOWN], BF16)
        mk = ph.sb([128, 16, 512], F32)
        tri = ph.sb([128, 128], BF16)
        negones = ph.sb([128, 128], BF16)
        Ls = [ph.sb([128, 512], BF16) for _ in range(2)]
        zm = [ph.sb([128, 512], F32) for _ in range(2)]
        ee = [ph.sb([128, 512], F32) for _ in range(3)]
        LL = [ph.sb([128, 512], BF16) for _ in range(3)]
        eR = [ph.sb([128, 512], F32) for _ in range(2)]
        aa = [ph.sb([128, 512], BF16) for _ in range(3)]
        ost = [ph.sb([64, 512], BF16) for _ in range(2)]
        ps_z = [ph.ps() for _ in range(2)]
        ps_r = [ph.ps() for _ in range(2)]
        ps_o = [ph.ps() for _ in range(2)]

        ph.dma("sp", mk[:, 0:8, :], masks[:, 0:8, :], [], ["mk"])
        ph.dma("sp", mk[:, 8:16, :], masks[:, 8:16, :], [], ["mk"])
        ph.dma("pool", tri[:], cst[:, C_TRI:C_TRI + 128], [], ["tri"])
        ph.memset("dve", negones[:], -1.0, ["negones"])
        it = 0
        nev = 0
        for hp in range(4):
            for q in range(4):
                ph.dma("sp", kT[:, q * 2048:(q + 1) * 2048], kT_scr[hp * 128:(hp + 1) * 128, q * 2048:(q + 1) * 2048],
                       ["kT_scr"], ["kT"])
                ph.dma("sp", vv[:, q * 16:(q + 1) * 16, :],
                       v_scr.rearrange("(n p) f -> p n f", p=128)[:, q * 16:(q + 1) * 16, hp * 128:(hp + 1) * 128],
                       ["v_scr"], ["vv"])
            ph.dma("sp", qT[:], qT_scr[hp * 128:(hp + 1) * 128, :], ["qT_scr"], ["qT"])
            for j in range(nj):
                for hh in range(2):
                    h = 2 * hp + hh
                    pr = slice(hh * 64, hh * 64 + 64)
                    nblk = 8 * (j + 1)
                    po = ps_o[hh]
                    pok = f"ps_o{hh}"
                    base = it
                    it += nblk

                    def F1(bi):
                        n = base + bi
                        kb = nblk - 1 - bi
                        i2, i3 = n % 2, n % 3
                        pz, pzk = ps_z[i2], f"ps_z{i2}"
                        ph.mm(pz[:], kT[pr, kb * 128:(kb + 1) * 128], qT[pr, j * 512:(j + 1) * 512], True, True,
                              ["kT", "qT"], [pzk])
                        if kb >= nblk - 8:
                            unit = (kb - (nblk - 8)) // 4
                            midx = (j % 2) * 8 + unit * 4 + (kb % 4)
                            ph.tt("dve", zm[i2][:], pz[:], mk[:, midx, :], ALU.add, [pzk, "mk"], [f"zm{i2}"])
                            ph.act(ee[i3][:], zm[i2][:], AF.Exp, [f"zm{i2}"], [f"ee{i3}"])
                        else:
                            ph.act(ee[i3][:], pz[:], AF.Exp, [pzk], [f"ee{i3}"])

                    def F2(bi):
                        n = base + bi
                        i2, i3 = n % 2, n % 3
                        prr, prk = ps_r[i2], f"ps_r{i2}"
                        ph.act(LL[i3][:], ee[i3][:], AF.Ln, [f"ee{i3}"], [f"LL{i3}"], bias=1.0)
                        ph.mm(prr[:], tri[:], LL[i3][:], True, bi == 0, ["tri", f"LL{i3}"], [prk])
                        if bi > 0:
                            ph.mm(prr[:], negones[:], Ls[(bi - 1) % 2][:], False, True,
                                  ["negones", f"Ls{(bi - 1) % 2}"], [prk])
                        if bi == 0:
                            ph.copy("dve", Ls[0][:], LL[i3][:], [f"LL{i3}"], ["Ls0"])
                        elif bi < nblk - 1:
                            ph.tt("dve", Ls[bi % 2][:], Ls[(bi - 1) % 2][:], LL[i3][:], ALU.add,
                                  [f"Ls{(bi - 1) % 2}", f"LL{i3}"], [f"Ls{bi % 2}"])

                    def B1(bi):
                        n = base + bi
                        i2 = n % 2
                        ph.act(eR[i2][:], ps_r[i2][:], AF.Exp, [f"ps_r{i2}"], [f"eR{i2}"])

                    def B2(bi):
                        n = base + bi
                        kb = nblk - 1 - bi
                        i2, i3 = n % 2, n % 3
                        ph.tt("dve", aa[i3][:], ee[i3][:], eR[i2][:], ALU.mult, [f"ee{i3}", f"eR{i2}"], [f"aa{i3}"])
                        ph.mm(po[0:64, :], vv[:, kb, hh * 64:(hh + 1) * 64], aa[i3][:], bi == 0, bi == nblk - 1,
                              ["vv", f"aa{i3}"], [pok])

                    F1(0)
                    for t in range(nblk + 1):
                        if t + 1 < nblk:
                            F1(t + 1)
                        if t >= 1:
                            B1(t - 1)
                        if t < nblk:
                            F2(t)
                        if t >= 1:
                            B2(t - 1)
                    b = nev % 2
                    nev += 1
                    ph.copy("act", ost[b][:], po[0:64, :], [pok], [f"ost{b}"])
                    ph.dma("sp", osb_scr[h * 64:(h + 1) * 64, j * 512:(j + 1) * 512], ost[b][:], [f"ost{b}"], ["osb_scr"])


def own_tiles(p):
    return [2 * (j // 2) * 2 + (0 if (j % 2 == 0) == (p == 0) else 0) for j in range(8)]


def tile_ids(p):
    out = []
    for j in range(8):
        if (j + p) % 2 == 0:
            out.append(2 * j)
        else:
            out.append(2 * j + 1)
    return out


def build(stop_after=99, debug=False, nj=8, nchunks=64, skip=()):
    nc = bass.Bass("TRN2", target_bir_lowering=False)

    def inp(name, shape, dt=F32):
        return nc.dram_tensor(name, list(shape), dt, kind="ExternalInput").ap()

    def scr(name, shape, dt=F32):
        if debug:
            return nc.dram_tensor(name, list(shape), dt, kind="ExternalOutput").ap()
        return nc.dram_tensor(name, list(shape), dt).ap()

    I = {}
    I["xT_ctx"] = inp("xT_ctx", [D, SEQ])
    I["xT_own"] = inp("xT_own", [D, NOWN])
    I["g_mix"] = inp("g_mix", [128, 8])
    I["w_ctx_fm"] = inp("w_ctx_fm", [D, 2048])
    I["w_ctx_tm"] = inp("w_ctx_tm", [D, 520])
    I["w_own"] = inp("w_own", [D, 3072])
    I["convw"] = inp("convw", [1536, 4])
    I["alog_bc"] = inp("alog_bc", [128, 4])
    I["dtb_bc"] = inp("dtb_bc", [128, 4])
    I["masks"] = inp("masks", [128, 16, 512])
    I["cst"] = inp("cst", [128, 640])
    I["memT"] = inp("memT", [D, 256])
    I["sel"] = inp("sel", [128, 16])
    I["gng"] = inp("gng", [128, 1])
    I["g_cross"] = inp("g_cross", [128, 8])
    I["g_mem"] = inp("g_mem", [128, 8])
    I["g_peer"] = inp("g_peer", [128, 8])
    I["g_final"] = inp("g_final", [128, 8])
    I["w_sb_up"] = inp("w_sb_up", [512, D])
    I["w_gdn_up"] = inp("w_gdn_up", [512, D])
    I["w_mix_out"] = inp("w_mix_out", [D, D])
    I["w_cq"] = inp("w_cq", [D, D])
    I["w_ckv"] = inp("w_ckv", [D, 2 * D])
    I["w_co"] = inp("w_co", [D, D])
    I["w_pq"] = inp("w_pq", [D, 2048])
    I["skT"] = inp("skT", [128, 16, 128])
    I["sel48"] = inp("sel48", [48, 16, 128])
    I["uT"] = inp("uT", [D, 16384])
    I["pv"] = inp("pv", [16384, D])
    outT = nc.dram_tensor("outT", [D, NOWN], F32, kind="ExternalOutput").ap()

    S = {}
    S["kT"] = scr("kT_scr", [512, SEQ], BF16)
    S["v"] = scr("v_scr", [SEQ, 512], BF16)
    S["gq"] = scr("gq_scr", [512, SEQ])
    S["gk"] = scr("gk_scr", [512, SEQ])
    S["gv"] = scr("gv_scr", [512, SEQ])
    S["gb"] = scr("gb_scr", [SEQ, 8])
    S["qT"] = scr("qT_scr", [512, NOWN], BF16)
    S["zT"] = scr("zT_scr", [512, NOWN])
    S["gate"] = scr("gate_scr", [2048, NOWN], BF16)
    S["osb"] = scr("osb_scr", [512, NOWN], BF16)
    S["og"] = scr("og_scr", [512, SEQ])
    S["x2"] = scr("x2_scr", [D, NOWN])
    if debug:
        S["x1"] = scr("x1_scr", [D, NOWN])

    if stop_after >= 1 and 1 not in skip:
        phase_inproj_ctx(nc, I["xT_ctx"], I["g_mix"], I["w_ctx_fm"], I["w_ctx_tm"], I["convw"],
                         I["alog_bc"], I["dtb_bc"], S["kT"], S["v"], S["gq"], S["gk"], S["gv"], S["gb"])
    if stop_after >= 2 and 2 not in skip:
        phase_inproj_own(nc, I["xT_own"], I["g_mix"], I["w_own"], S["qT"], S["zT"], S["gate"])
    if stop_after >= 3 and 3 not in skip:
        phase_sb(nc, S["qT"], S["kT"], S["v"], I["masks"], I["cst"], S["osb"], nj=nj)
    if stop_after >= 4 and 4 not in skip:
        phase_gdn(nc, S["gq"], S["gk"], S["gv"], S["gb"], I["cst"], S["og"], nchunks=nchunks)
    if stop_after >= 5:
        phase_merge_cross(nc, I["xT_own"], I["memT"], S["og"], S["osb"], S["zT"], S["gate"], I["sel"], I["gng"],
                          I["g_cross"], I["g_mem"], I["w_sb_up"], I["w_gdn_up"], I["w_mix_out"], I["w_cq"],
                          I["w_ckv"], I["w_co"], S["x2"], x1_dbg=S["x1"] if debug else None)
    if stop_after >= 6:
        phase_peer(nc, S["x2"], I["g_peer"], I["g_final"], I["w_pq"], I["skT"], I["sel48"], I["cst"],
                   I["uT"], I["pv"], outT)
    return nc


def make_consts():
    i = np.arange(128)
    low = (i[:, None] >= i[None, :]).astype(np.float32)
    lows = (i[:, None] > i[None, :]).astype(np.float32)
    up = (i[:, None] <= i[None, :]).astype(np.float32)
    ident = np.eye(128, dtype=np.float32)
    return np.concatenate([low, lows, up, ident, -low], axis=1)


def make_masks(p):
    m = np.zeros((128, 2, 2, 4, 512), np.float32)
    k = np.arange(128)[:, None]
    q = np.arange(512)[None, :]
    diag = np.stack([np.where(kk * 128 + k < q, 0.0, NEG) for kk in range(4)], 0)
    full = np.zeros((4, 128, 512), np.float32)
    zero = np.full((4, 128, 512), NEG, np.float32)
    for jp in range(2):
        if (p + jp) % 2 == 0:
            u0, u1 = diag, zero
        else:
            u0, u1 = full, diag
        m[:, jp, 0] = u0.transpose(1, 0, 2)
        m[:, jp, 1] = u1.transpose(1, 0, 2)
    return np.ascontiguousarray(m.reshape(128, 16, 512))


def prepare(inputs):
    f = lambda a: np.ascontiguousarray(np.asarray(a, dtype=np.float32))
    x = f(inputs["x"])
    w_in = f(inputs["w_in"])[0]
    o = np.cumsum([0, 512, 512, 512, 512, 512, 512, 512, 4, 4, 1024, 1024])
    col = lambda i: w_in[:, o[i]:o[i + 1]]
    w_ctx_fm = f(np.concatenate([col(1), col(3), col(4), col(5)], 1))
    w_ctx_tm = f(np.concatenate([col(2), col(7), col(8)], 1))
    w_own = f(np.concatenate([col(0), col(6), col(9), col(10)], 1))
    pc8 = lambda a: f(f(a)[0].reshape(8, 128).T)
    mem = f(inputs["mem"])
    shared = dict(
        g_mix=f(f(inputs["g_mix"])[0].reshape(8, 128).T), w_ctx_fm=w_ctx_fm, w_ctx_tm=w_ctx_tm, w_own=w_own,
        convw=f(f(inputs["gdn_conv"])[0].T),
        alog_bc=f(np.broadcast_to(f(inputs["gdn_a_log"])[0][None, :], (128, 4))),
        dtb_bc=f(np.broadcast_to(f(inputs["gdn_dt_bias"])[0][None, :], (128, 4))),
        cst=make_consts(),
        gng=f(f(inputs["gdn_norm_g"])[0].reshape(128, 1)),
        g_cross=pc8(inputs["g_cross"]), g_mem=pc8(inputs["g_mem"]), g_peer=pc8(inputs["g_peer"]),
        g_final=f(f(inputs["g_final"]).reshape(8, 128).T),
        w_sb_up=f(inputs["w_sb_up"])[0], w_gdn_up=f(inputs["w_gdn_up"])[0], w_mix_out=f(inputs["w_mix_out"])[0],
        w_cq=f(inputs["w_cq"])[0], w_ckv=f(inputs["w_ckv"])[0], w_co=f(inputs["w_co"])[0],
        w_pq=f(inputs["w_pq"])[0],
        skT=f(f(inputs["peer_subkeys"])[0].transpose(3, 0, 1, 2).reshape(128, 16, 128)),
        sel48=f((np.arange(48)[:, None, None] % 16 == np.arange(16)[None, :, None]) * np.ones((1, 1, 128))),
        uT=f(f(inputs["peer_u"])[0].T), pv=f(inputs["peer_v"])[0],
    )
    in_maps = []
    for c in range(8):
        b, p = c // 2, c % 2
        tl = tile_ids(p)
        idx = np.concatenate([np.arange(t * 512, (t + 1) * 512) for t in tl])
        m = dict(shared)
        m["xT_ctx"] = f(x[b].T)
        m["xT_own"] = f(x[b][idx].T)
        m["masks"] = make_masks(p)
        m["memT"] = f(mem[b].T)
        cj = np.array([1.0 if (j + p) % 2 == 0 else 0.0 for j in range(8)], np.float32)
        m["sel"] = f(np.broadcast_to(np.concatenate([cj, 1.0 - cj])[None, :], (128, 16)))
        in_maps.append(m)
    return in_maps


def phase_gdn(nc, gq_scr, gk_scr, gv_scr, gb_scr, cst, og_scr, nchunks=64):
    with Phase(nc) as ph:
        cs = ph.sb([128, 640], F32)
        ones = ph.sb([128, 128], F32)
        gb = ph.sb([128, 64, 8], F32)
        nb = ph.sb([128, 64, 4], F32)
        qkv = [[ph.sb([128, 4, 512], F32) for _ in range(3)] for _ in range(2)]
        St = [ph.sb([128, 128], F32) for _ in range(4)]
        names = ["grep", "gT2", "decS", "decT", "egcb", "P0", "P1", "Q0", "Q1", "U0", "U1", "ktm", "vtm", "vb",
                 "kbg", "u", "wT", "qkT", "qdT", "kdec", "vnew", "ost"]
        bufs = [[{nm: ph.sb([128, 128], F32) for nm in names} for _ in range(2)] for _ in range(4)]
        cols = [[ph.sb([128, 8], F32) for _ in range(2)] for _ in range(4)]
        banks = [ph.ps() for _ in range(8)]
        LOW = cs[:, C_LOW:C_LOW + 128]
        LOWS = cs[:, C_LOWS:C_LOWS + 128]
        UP = cs[:, C_UP:C_UP + 128]
        IDN = cs[:, C_ID:C_ID + 128]
        pctr = [0]

        def pst():
            i = pctr[0] % 8
            pctr[0] += 1
            return banks[i][:, 0:128], f"ps{i}"

        ph.dma("sp", cs[:], cst, [], ["cs"])
        ph.dma("sp", gb[:], gb_scr.rearrange("(n p) f -> p n f", p=128), ["gb_scr"], ["gb"])
        ph.memset("dve", ones[:], 1.0, ["ones"])
        for h in range(4):
            ph.memset("pool", St[h][:], 0.0, [f"S{h}"])
        ph.ts("dve", nb[:], gb[:, :, 0:4], -1.0, ALU.mult, ["gb"], ["nb"])

        for n in range(nchunks):
            par = n % 2
            grp, gi = n // 4, n % 4
            gs = grp % 2
            if gi == 0:
                for ti, src in enumerate((gq_scr, gk_scr, gv_scr)):
                    ph.dma("sp", qkv[gs][ti][:], src.rearrange("(h p) t -> p h t", p=128)[:, :, grp * 512:(grp + 1) * 512],
                           ["g_scr"], [f"qkv{gs}{ti}"])
            tsl = slice(gi * 128, (gi + 1) * 128)
            qT = [qkv[gs][0][:, h, tsl] for h in range(4)]
            kT = [qkv[gs][1][:, h, tsl] for h in range(4)]
            vT = [qkv[gs][2][:, h, tsl] for h in range(4)]
            kq, kk_, kv = f"qkv{gs}0", f"qkv{gs}1", f"qkv{gs}2"
            B = [bufs[h][par] for h in range(4)]
            K = lambda nm, h: f"{nm}{h}{par}"
            CL = [cols[h][par] for h in range(4)]
            gcol = [gb[:, n, 4 + h:5 + h] for h in range(4)]
            bcol = [gb[:, n, h:h + 1] for h in range(4)]
            nbcol = [nb[:, n, h:h + 1] for h in range(4)]
            pd, pdT, pgb, pc, pkk, pqk = {}, {}, {}, {}, {}, {}
            for h in range(4):
                ph.ts("dve", B[h]["gT2"][:], LOWS, gcol[h], ALU.mult, ["cs", "gb"], [K("gT2", h)])
                ph.ts("dve", B[h]["grep"][:], ones[:], gcol[h], ALU.mult, ["ones", "gb"], [K("grep", h)])
                p1, k1 = pst()
                ph.mm(p1, kT[h], IDN, True, True, [kk_, "cs"], [k1])
                ph.copy("act", B[h]["ktm"][:], p1, [k1], [K("ktm", h)])
                p2, k2 = pst()
                ph.mm(p2, vT[h], IDN, True, True, [kv, "cs"], [k2])
                ph.copy("dve", B[h]["vtm"][:], p2, [k2], [K("vtm", h)])
            for h in range(4):
                x, xk = pst()
                ph.mm(x, UP, B[h]["gT2"][:], True, True, ["cs", K("gT2", h)], [xk])
                ph.act(B[h]["decS"][:], x, AF.Exp, [xk], [K("decS", h)])
                x, xk = pst()
                ph.mm(x, B[h]["gT2"][:], UP, True, True, ["cs", K("gT2", h)], [xk])
                ph.act(B[h]["decT"][:], x, AF.Exp, [xk], [K("decT", h)])
                x, xk = pst()
                ph.mm(x, B[h]["grep"][:], UP, True, True, ["cs", K("grep", h)], [xk])
                ph.act(B[h]["egcb"][:], x, AF.Exp, [xk], [K("egcb", h)])
                for q3, lm in enumerate((UP, LOWS, ones[:])):
                    x, xk = pst()
                    ph.mm(x, lm, B[h]["grep"][:], True, True, ["cs", "ones", K("grep", h)], [xk])
                    ph.act(CL[h][:, q3:q3 + 1], x[:, 0:1], AF.Exp, [xk], [K("col", h)])
                ph.tt("pool", B[h]["decS"][:], B[h]["decS"][:], LOWS, ALU.mult, [K("decS", h), "cs"], [K("decS", h)])
                ph.tt("pool", B[h]["decT"][:], B[h]["decT"][:], UP, ALU.mult, [K("decT", h), "cs"], [K("decT", h)])
                ph.tt("dve", CL[h][:, 3:4], CL[h][:, 0:1], bcol[h], ALU.mult, [K("col", h), "gb"], [K("col", h)])
                x, xk = pst()
                ph.mm(x, kT[h], kT[h], True, True, [kk_], [xk])
                ph.stt(B[h]["P0"][:], x, nbcol[h], B[h]["decS"][:], ALU.mult, ALU.mult,
                       [xk, "nb", K("decS", h)], [K("P0", h)])
                x, xk = pst()
                ph.mm(x, kT[h], qT[h], True, True, [kk_, kq], [xk])
                ph.tt("dve", B[h]["qkT"][:], x, B[h]["decT"][:], ALU.mult, [xk, K("decT", h)], [K("qkT", h)])
                ph.tt("pool", B[h]["qdT"][:], qT[h], B[h]["egcb"][:], ALU.mult, [kq, K("egcb", h)], [K("qdT", h)])
                ph.ts("dve", B[h]["kdec"][:], B[h]["ktm"][:], CL[h][:, 1:2], ALU.mult, [K("ktm", h), K("col", h)], [K("kdec", h)])
                ph.ts("dve", B[h]["vb"][:], B[h]["vtm"][:], bcol[h], ALU.mult, [K("vtm", h), "gb"], [K("vb", h)])
                ph.ts("dve", B[h]["kbg"][:], B[h]["ktm"][:], CL[h][:, 3:4], ALU.mult, [K("ktm", h), K("col", h)], [K("kbg", h)])
            for h in range(4):
                p1, k1 = pst()
                ph.mm(p1, B[h]["P0"][:], IDN, True, True, [K("P0", h), "cs"], [k1])
                ph.copy("act", B[h]["Q0"][:], p1, [k1], [K("Q0", h)])
                ph.tt("dve", B[h]["U0"][:], p1, IDN, ALU.add, [k1, "cs"], [K("U0", h)])
            for k in range(1, 7):
                a, b = (k - 1) % 2, k % 2
                for h in range(4):
                    Pp, Qp = B[h][f"P{a}"], B[h][f"Q{a}"]
                    p1, k1 = pst()
                    ph.mm(p1, Qp[:], Pp[:], True, True, [K(f"Q{a}", h), K(f"P{a}", h)], [k1])
                    ph.copy("act", B[h][f"P{b}"][:], p1, [k1], [K(f"P{b}", h)])
                if k <= 5:
                    for h in range(4):
                        Pp, Qp = B[h][f"P{a}"], B[h][f"Q{a}"]
                        p2, k2 = pst()
                        ph.mm(p2, Pp[:], Qp[:], True, True, [K(f"Q{a}", h), K(f"P{a}", h)], [k2])
                        ph.copy("dve", B[h][f"Q{b}"][:], p2, [k2], [K(f"Q{b}", h)])
                for h in range(4):
                    Up, Un, Pn = B[h][f"U{a}"], B[h][f"U{b}"], B[h][f"P{b}"]
                    p3, k3 = pst()
                    ph.mm(p3, Pn[:], Up[:], True, True, [K(f"P{b}", h), K(f"U{a}", h)], [k3])
                    ph.tt("dve", Un[:], Up[:], p3, ALU.add, [K(f"U{a}", h), k3], [K(f"U{b}", h)])
            UF = "U0"
            for h in range(4):
                p1, k1 = pst()
                ph.mm(p1, B[h][UF][:], B[h]["vb"][:], True, True, [K(UF, h), K("vb", h)], [k1])
                ph.copy("act", B[h]["u"][:], p1, [k1], [K("u", h)])
                p2, k2 = pst()
                ph.mm(p2, B[h]["kbg"][:], B[h][UF][:], True, True, [K(UF, h), K("kbg", h)], [k2])
                ph.copy("dve", B[h]["wT"][:], p2, [k2], [K("wT", h)])
            pw = {}
            for h in range(4):
                p1, k1 = pst()
                ph.mm(p1, B[h]["wT"][:], St[h][:], True, True, [K("wT", h), f"S{h}"], [k1])
                ph.tt("dve", B[h]["vnew"][:], B[h]["u"][:], p1, ALU.subtract, [K("u", h), k1], [K("vnew", h)])
            for h in range(4):
                p2, k2 = pst()
                ph.mm(p2, St[h][:], B[h]["qdT"][:], True, False, [f"S{h}", K("qdT", h)], [k2])
                ph.mm(p2, B[h]["vnew"][:], B[h]["qkT"][:], False, True, [K("vnew", h), K("qkT", h)], [k2])
                ph.copy("act", B[h]["ost"][:], p2, [k2], [K("ost", h)])
                ph.dma("sp", og_scr[h * 128:(h + 1) * 128, n * 128:(n + 1) * 128], B[h]["ost"][:], [K("ost", h)], ["og_scr"])
            for h in range(4):
                p3, k3 = pst()
                ph.mm(p3, B[h]["kdec"][:], B[h]["vnew"][:], True, True, [K("kdec", h), K("vnew", h)], [k3])
                ph.stt(St[h][:], St[h][:], CL[h][:, 2:3], p3, ALU.mult, ALU.add, [f"S{h}", K("col", h), k3], [f"S{h}"])

def phase_merge_cross(nc, xT_own, memT, og_scr, osb_scr, zT_scr, gate_scr, sel, gng, g_cross, g_mem,
                      w_sb_up, w_gdn_up, w_mix_out, w_cq, w_ckv, w_co, x2_scr, x1_dbg=None):
    NT = NOWN // 512
    with Phase(nc) as ph:
        wsb = ph.sb([64, 8, 1024], BF16)
        wgu = ph.sb([128, 4, 1024], BF16)
        wmo = ph.sb([128, 8, 1024], BF16)
        wcq = ph.sb([128, 8, 1024], BF16)
        wco = ph.sb([128, 8, 1024], BF16)
        kxT = ph.sb([128, 8, 256], BF16)
        vx = ph.sb([128, 2, 1024], BF16)
        ones32 = ph.sb([128, 128], F32)
        ones16 = ph.sb([128, 128], BF16)
        selt = ph.sb([128, 16], F32)
        gn = ph.sb([128, 1], F32)
        gcc = ph.sb([128, 8], F32)
        gmc = ph.sb([128, 8], F32)
        xt = ph.sb([128, 8, 512], F32)
        sq = ph.sb([128, 8, 512], F32)
        xn = ph.sb([128, 8, 512], BF16)
        rt = ph.sb([128, 512], F32)
        oga = sq[:, 0:4, :]
        ogb = sq[:, 4:8, :]
        zt = ph.sb([128, 4, 512], F32)
        ogn = ph.sb([128, 4, 512], BF16)
        osb = ph.sb([64, 8, 512], BF16)
        gt = ph.sb([128, 16, 512], BF16)
        mg = ph.sb([128, 8, 512], BF16)
        t1 = [ph.sb([128, 512], F32) for _ in range(2)]
        qx = ph.sb([128, 8, 512], BF16)
        pT = [ph.sb([128, 512], BF16) for _ in range(4)]
        oT = ph.sb([128, 8, 512], BF16)
        rden = [ph.sb([128, 512], F32) for _ in range(2)]
        ps_stat = ph.ps()
        ps_a = [ph.ps() for _ in range(2)]
        ps_b = [ph.ps() for _ in range(2)]
        ps_c = [ph.ps() for _ in range(2)]

        ph.dma("pool", wsb[:], w_sb_up.rearrange("(h d) f -> d h f", d=64), [], ["wsb"])
        ph.dma("pool", wgu[:], w_gdn_up.rearrange("(h d) f -> d h f", d=128), [], ["wgu"])
        for q in range(2):
            hs = slice(4 * q, 4 * q + 4)
            ph.dma("pool", wmo[:, hs, :], fm(w_mix_out, 0, 1024)[:, hs, :], [], ["wmo"])
            ph.dma("pool", wcq[:, hs, :], fm(w_cq, 0, 1024)[:, hs, :], [], ["wcq"])
            ph.dma("pool", wco[:, hs, :], fm(w_co, 0, 1024)[:, hs, :], [], ["wco"])
        ph.dma("sp", selt[:], sel, [], ["selt"])
        ph.dma("sp", gn[:], gng, [], ["gn"])
        ph.dma("sp", gcc[:], g_cross, [], ["gcol"])
        ph.dma("sp", gmc[:], g_mem, [], ["gmc"])
        ph.memset("dve", ones32[:], 1.0, ["ones32"])
        ph.memset("dve", ones16[:], 1.0, ["ones16"])

        ph.dma("sp", xt[:, :, 0:256], fm(memT, 0, 256), [], ["xt0"])
        for c in range(8):
            ph.act(sq[:, c, 0:256], xt[:, c, 0:256], AF.Square, ["xt0"], ["sq"])
        for c in range(8):
            ph.mm(ps_stat[:, 0:256], ones32[:], sq[:, c, 0:256], c == 0, c == 7, ["sq", "ones32"], ["ps_stat"])
        ph.act(rt[:, 0:256], ps_stat[:, 0:256], AF.Sqrt, ["ps_stat"], ["rt"], bias=EPS, scale=1.0 / 1024)
        ph.recip(rt[:, 0:256], rt[:, 0:256], ["rt"], ["rt"])
        for c in range(8):
            ph.stt(xn[:, c, 0:256], xt[:, c, 0:256], gmc[:, c:c + 1], rt[:, 0:256], ALU.mult, ALU.mult,
                   ["xt0", "rt", "gmc"], ["xn0"])
        wk = gt[:, 0:8, :]
        for blk in range(4):
            ph.dma("pool", wk, fm(w_ckv, 0, 2048)[:, :, blk * 512:(blk + 1) * 512], [], ["gt"])
            if blk < 2:
                for ff in range(4):
                    f = blk * 4 + ff
                    pb = ps_a[ff % 2]
                    for c in range(8):
                        ph.mm(pb[:, 0:256], wk[:, c, ff * 128:(ff + 1) * 128], xn[:, c, 0:256], c == 0, c == 7,
                              ["gt", "xn0"], [f"ps_a{ff % 2}"])
                    ph.copy("act", kxT[:, f, :], pb[:, 0:256], [f"ps_a{ff % 2}"], ["kxT"])
            else:
                for mt in range(2):
                    pb = ps_a[mt]
                    for c in range(8):
                        ph.mm(pb[:], xn[:, c, mt * 128:(mt + 1) * 128], wk[:, c, :], c == 0, c == 7,
                              ["gt", "xn0"], [f"ps_a{mt}"])
                    ph.copy("act", vx[:, mt, (blk - 2) * 512:(blk - 1) * 512], pb[:], [f"ps_a{mt}"], ["vx"])

        for j in range(NT):
            t0 = j * 512
            ph.dma("sp", xt[:, 0:4, :], fm(xT_own, t0, 512)[:, 0:4, :], [], ["xt0"])
            ph.dma("sp", xt[:, 4:8, :], fm(xT_own, t0, 512)[:, 4:8, :], [], ["xt0"])
            ogv = og_scr.rearrange("(h p) t -> p h t", p=128)
            ph.dma("sp", oga, ogv[:, :, (2 * j) * 512:(2 * j + 1) * 512], ["og_scr"], ["sq"])
            ph.dma("sp", ogb, ogv[:, :, (2 * j + 1) * 512:(2 * j + 2) * 512], ["og_scr"], ["sq"])
            ph.dma("sp", zt[:], zT_scr.rearrange("(h p) t -> p h t", p=128)[:, :, t0:t0 + 512], ["zT_scr"], ["zt"])
            ph.dma("sp", osb[:], osb_scr.rearrange("(h d) t -> d h t", d=64)[:, :, t0:t0 + 512], ["osb_scr"], ["osb"])
            gv_ = gate_scr.rearrange("(f p) t -> p f t", p=128)
            ph.dma("sp", gt[:, 0:8, :], gv_[:, 0:8, t0:t0 + 512], ["gate_scr"], ["gt"])
            ph.dma("sp", gt[:, 8:16, :], gv_[:, 8:16, t0:t0 + 512], ["gate_scr"], ["gt"])
            ph.ts("dve", oga, oga, selt[:, j:j + 1], ALU.mult, ["sq", "selt"], ["sq"])
            ph.stt(oga, ogb, selt[:, 8 + j:9 + j], oga, ALU.mult, ALU.add, ["sq", "selt"], ["sq"])
            for hh in range(4):
                pb = ps_a[hh % 2]
                pk = f"ps_a{hh % 2}"
                ph.act(rt[:], oga[:, hh, :], AF.Square, ["sq"], ["rt"])
                ph.mm(pb[:], ones32[:], rt[:], True, True, ["ones32", "rt"], [pk])
                ph.act(t1[hh % 2][:], pb[:], AF.Sqrt, [pk], [f"t1{hh % 2}"], bias=EPS, scale=1.0 / 128)
                ph.recip(t1[hh % 2][:], t1[hh % 2][:], [f"t1{hh % 2}"], [f"t1{hh % 2}"])
                ph.stt(t1[hh % 2][:], oga[:, hh, :], gn[:, 0:1], t1[hh % 2][:], ALU.mult, ALU.mult,
                       ["sq", "gn", f"t1{hh % 2}"], [f"t1{hh % 2}"])
                ph.tt("dve", ogn[:, hh, :], t1[hh % 2][:], zt[:, hh, :], ALU.mult, [f"t1{hh % 2}", "zt"], ["ogn"])
            for f in range(8):
                pa, pak = ps_a[f % 2], f"ps_a{f % 2}"
                pb, pbk = ps_b[f % 2], f"ps_b{f % 2}"
                for h in range(8):
                    ph.mm(pa[:], wsb[:, h, f * 128:(f + 1) * 128], osb[:, h, :], h == 0, h == 7, ["wsb", "osb"], [pak])
                for h in range(4):
                    ph.mm(pb[:], wgu[:, h, f * 128:(f + 1) * 128], ogn[:, h, :], h == 0, h == 3, ["wgu", "ogn"], [pbk])
                ph.tt("dve", t1[0][:], pa[:], gt[:, f, :], ALU.mult, [pak, "gt"], ["t10"])
                ph.tt("dve", t1[1][:], pb[:], gt[:, 8 + f, :], ALU.mult, [pbk, "gt"], ["t11"])
                ph.tt("pool", mg[:, f, :], t1[0][:], t1[1][:], ALU.add, ["t10", "t11"], ["mg"])
            for f in range(8):
                pc, pck = ps_c[f % 2], f"ps_c{f % 2}"
                for c in range(8):
                    ph.mm(pc[:], wmo[:, c, f * 128:(f + 1) * 128], mg[:, c, :], c == 0, c == 7, ["wmo", "mg"], [pck])
                ph.tt("dve", xt[:, f, :], xt[:, f, :], pc[:], ALU.add, ["xt0", pck], ["xt0"])
            if x1_dbg is not None:
                ph.dma("sp", fm(x1_dbg, t0, 512), xt[:], ["xt0"], ["x1_dbg"])
            norm_tile(ph, xt, sq, xn, gcc, ones32, ps_stat, rt, 0, None)
            for f in range(8):
                pc, pck = ps_c[f % 2], f"ps_c{f % 2}"
                for c in range(8):
                    ph.mm(pc[:], wcq[:, c, f * 128:(f + 1) * 128], xn[:, c, :], c == 0, c == 7, ["wcq", "xn0"], [pck])
                ph.act(qx[:, f, :], pc[:], AF.Copy, [pck], ["qx"], scale=1.0 / 16)
            for hd in range(4):
                for mt in range(2):
                    pa, pak = ps_a[mt], f"ps_a{mt}"
                    for cc in range(2):
                        ph.mm(pa[:], kxT[:, 2 * hd + cc, mt * 128:(mt + 1) * 128], qx[:, 2 * hd + cc, :], cc == 0, cc == 1,
                              ["kxT", "qx"], [pak])
                    i4 = (hd % 2) * 2 + mt
                    ph.act(pT[i4][:], pa[:], AF.Exp, [pak], [f"pT{i4}"])
                pb, pbk = ps_b[hd % 2], f"ps_b{hd % 2}"
                for mt in range(2):
                    i4 = (hd % 2) * 2 + mt
                    ph.mm(pb[:], ones16[:], pT[i4][:], mt == 0, mt == 1, ["ones16", f"pT{i4}"], [pbk])
                rd = rden[hd % 2]
                ph.recip(rd[:], pb[:], [pbk], [f"rden{hd % 2}"])
                for cc in range(2):
                    pc, pck = ps_c[cc], f"ps_c{cc}"
                    for mt in range(2):
                        i4 = (hd % 2) * 2 + mt
                        ph.mm(pc[:], vx[:, mt, hd * 256 + cc * 128:hd * 256 + (cc + 1) * 128], pT[i4][:], mt == 0, mt == 1,
                              ["vx", f"pT{i4}"], [pck])
                    ph.tt("dve", oT[:, 2 * hd + cc, :], pc[:], rd[:], ALU.mult, [pck, f"rden{hd % 2}"], ["oT"])
            for f in range(8):
                pc, pck = ps_c[f % 2], f"ps_c{f % 2}"
                for c in range(8):
                    ph.mm(pc[:], wco[:, c, f * 128:(f + 1) * 128], oT[:, c, :], c == 0, c == 7, ["wco", "oT"], [pck])
                ph.tt("dve", xt[:, f, :], xt[:, f, :], pc[:], ALU.add, ["xt0", pck], ["xt0"])
            ph.dma("sp", fm(x2_scr, t0, 512)[:, 0:4, :], xt[:, 0:4, :], ["xt0"], ["x2_scr"])
            ph.dma("sp", fm(x2_scr, t0, 512)[:, 4:8, :], xt[:, 4:8, :], ["xt0"], ["x2_scr"])


DELTA = 2e-5


def phase_peer(nc, x2_scr, g_peer, g_final, w_pq, skT_in, sel_in, cst, uT, pv, outT, ntiles=8, negroups=32):
    with Phase(nc) as ph:
        wpq = ph.sb([128, 8, 2048], BF16)
        skT = ph.sb([128, 16, 128], BF16)
        selm = ph.sb([48, 16, 128], BF16)
        idb = ph.sb([128, 128], BF16)
        ones32 = ph.sb([128, 128], F32)
        gpc = ph.sb([128, 8], F32)
        gfc = ph.sb([128, 8], F32)
        xt = ph.sb([128, 8, 512], F32)
        sq = ph.sb([128, 8, 512], F32)
        xn = ph.sb([128, 8, 512], BF16)
        rt = ph.sb([128, 512], F32)
        qT = ph.sb([128, 16, 512], BF16)
        top = ph.sb([128, 16, 16], F32)
        cand = ph.sb([128, 256], F32)
        cand2 = ph.sb([128, 256], F32)
        best = ph.sb([128, 16], F32)
        junk = ph.sb([128, 16], F32)
        sm8 = ph.sb([128, 32], F32)
        rws = ph.sb([128, 16], F32)
        res = ph.sb([128, 16], F32)
        spl = ph.sb([128, 48], BF16)
        rows = ph.sb([48, 512], BF16)
        theta = ph.sb([128, 8, 512], F32)
        ug = [ph.sb([128, 8, 512], BF16) for _ in range(2)]
        vg = [ph.sb([128, 4, 1024], BF16) for _ in range(2)]
        gA = [ph.sb([128, 512], F32) for _ in range(4)]
        EE = [ph.sb([128, 512], BF16) for _ in range(4)]
        MK = [ph.sb([128, 512], BF16) for _ in range(4)]
        GH = [ph.sb([128, 512], BF16) for _ in range(6)]
        Hg = [ph.sb([128, 4, 512], BF16) for _ in range(2)]
        ps_q = [ph.ps() for _ in range(2)]
        ps_stat = ps_q[0]
        ps_A = [ph.ps()]
        ps_P = [ph.ps() for _ in range(4)]
        ps_O = ph.ps()

        for q in range(4):
            ph.dma("pool", wpq[:, 2 * q:2 * q + 2, :], fm(w_pq, 0, 2048)[:, 2 * q:2 * q + 2, :], [], ["wpq"])
        ph.dma("pool", skT[:], skT_in, [], ["skT"])
        ph.dma("pool", selm[:], sel_in, [], ["selm"])
        ph.dma("pool", idb[:], cst[:, C_ID:C_ID + 128], [], ["idb"])
        ph.dma("sp", gpc[:], g_peer, [], ["gcol"])
        ph.dma("sp", gfc[:], g_final, [], ["gfc"])
        ph.memset("dve", ones32[:], 1.0, ["ones32"])

        s_tm = sq[:, 0:4, :].rearrange("p a (b c) -> p (a b) c", c=128)
        s_w = sq[:, 4:8, :].rearrange("p a (b c) -> p (a b) c", c=128)
        ndma = 0
        for j in range(ntiles):
            t0 = j * 512
            ph.dma("sp", xt[:, 0:4, :], fm(x2_scr, t0, 512)[:, 0:4, :], ["x2_scr"], ["xt0"])
            ph.dma("sp", xt[:, 4:8, :], fm(x2_scr, t0, 512)[:, 4:8, :], ["x2_scr"], ["xt0"])
            norm_tile(ph, xt, sq, xn, gpc, ones32, ps_stat, rt, 0, "ps_q0")
            for f in range(16):
                pb, pk = ps_q[f % 2], f"ps_q{f % 2}"
                for c in range(8):
                    ph.mm(pb[:], wpq[:, c, f * 128:(f + 1) * 128], xn[:, c, :], c == 0, c == 7, ["wpq", "xn0"], [pk])
                ph.copy("act", qT[:, f, :], pb[:], [pk], ["qT"])
            for u in range(4):
                us = slice(u * 128, (u + 1) * 128)
                for g4 in range(4):
                    pb, pk = ps_q[g4 % 2], f"ps_q{g4 % 2}"
                    for k4 in range(4):
                        hp = g4 * 4 + k4
                        ph.mm(pb[:, k4 * 128:(k4 + 1) * 128], qT[:, hp, us], skT[:, hp, :], True, True, ["qT", "skT"], [pk])
                    ph.copy("act", s_tm[:, g4 * 4:(g4 + 1) * 4, :], pb[:].rearrange("p (a c) -> p a c", c=128), [pk], ["sq"])
                for hp in range(16):
                    ph.P.add("dve", lambda e, hp=hp: e.max(out=top[:, hp, 0:8], in_=s_tm[:, hp, :]), ["sq"], ["top"])
                    ph.P.add("dve", lambda e, hp=hp: e.match_replace(out=s_w[:, hp, :], in_to_replace=top[:, hp, 0:8],
                                                                     in_values=s_tm[:, hp, :], imm_value=-1e30),
                             ["sq", "top"], ["sq"])
                    ph.P.add("dve", lambda e, hp=hp: e.max(out=top[:, hp, 8:16], in_=s_w[:, hp, :]), ["sq"], ["top"])
                for h in range(8):
                    c3 = cand[:].rearrange("p (a b) -> p a b", a=16)
                    in0 = top[:, 2 * h, :].rearrange("p (a o) -> p a o", o=1).to_broadcast([128, 16, 16])
                    in1 = top[:, 2 * h + 1, :].rearrange("p (o b) -> p o b", o=1).to_broadcast([128, 16, 16])
                    ph.tt("dve", c3, in0, in1, ALU.add, ["top"], ["cand"])
                    ph.P.add("dve", lambda e: e.max(out=best[:, 0:8], in_=cand[:]), ["cand"], ["best"])
                    ph.P.add("dve", lambda e: e.match_replace(out=cand2[:], in_to_replace=best[:, 0:8], in_values=cand[:],
                                                              imm_value=-1e30), ["cand", "best"], ["cand2"])
                    ph.P.add("dve", lambda e: e.max(out=best[:, 8:16], in_=cand2[:]), ["cand2"], ["best"])
                    ph.ts("dve", sm8[:, h:h + 1], best[:, 0:1], -1.0, ALU.mult, ["best"], ["sm8"])
                    ph.memset("dve", sm8[:, 8 + h:9 + h], 0.0, ["sm8"])
                    ph.act(junk[:], best[:], AF.Exp, ["best", "sm8"], ["junk", "sm8"], bias=sm8[:, h:h + 1], scale=1.0,
                           accum_out=sm8[:, 8 + h:9 + h])
                    ph.copy("dve", sm8[:, 24 + h:25 + h], best[:, 15:16], ["best"], ["sm8"])
                ph.act(sm8[:, 16:24], sm8[:, 8:16], AF.Ln, ["sm8"], ["sm8"])
                ph.tt("dve", rws[:, 8:16], sm8[:, 0:8], sm8[:, 16:24], ALU.subtract, ["sm8"], ["rws"])
                ph.stt(rws[:, 0:8], sm8[:, 24:32], -DELTA, rws[:, 8:16], ALU.add, ALU.add, ["sm8", "rws"], ["rws"])
                ph.copy("dve", spl[:, 0:16], rws[:], ["rws"], ["spl"])
                ph.tt("dve", res[:], rws[:], spl[:, 0:16], ALU.subtract, ["rws", "spl"], ["res"])
                ph.copy("dve", spl[:, 16:32], res[:], ["res"], ["spl"])
                ph.tt("dve", res[:], res[:], spl[:, 16:32], ALU.subtract, ["res", "spl"], ["res"])
                ph.copy("dve", spl[:, 32:48], res[:], ["res"], ["spl"])
                pb, pk = ps_q[u % 2], f"ps_q{u % 2}"
                ph.mm(pb[0:48, 0:128], spl[:], idb[:], True, True, ["spl", "idb"], [pk])
                ph.copy("act", rows[:, us], pb[0:48, 0:128], [pk], ["rows"])
            for h in range(8):
                pb, pk = ps_q[h % 2], f"ps_q{h % 2}"
                ph.mm(pb[:], selm[:, h, :], rows[:], True, True, ["selm", "rows"], [pk])
                ph.copy("act", theta[:, h, :], pb[:], [pk], ["theta"])
            it = 0
            def load_eg(eg):
                gs = eg % 2
                ph.dma("pool", ug[gs][:], fm(uT, eg * 512, 512), [], [f"ug{gs}"])
                ph.dma("pool", vg[gs][:], pv.rearrange("(g p) d -> p g d", p=128)[:, eg * 4:(eg + 1) * 4, :], [], [f"vg{gs}"])

            load_eg(0)
            for eg in range(negroups):
                gs = eg % 2
                if eg + 1 < negroups:
                    load_eg(eg + 1)
                def SA(ii):
                    for c in range(8):
                        ph.mm(ps_A[0][:], ug[gs][:, c, ii * 128:(ii + 1) * 128], xn[:, c, :], c == 0, c == 7,
                              [f"ug{gs}", "xn0"], ["ps_A0"])
                    ph.act(gA[ii][:], ps_A[0][:], AF.Gelu, ["ps_A0"], [f"gA{ii}"])

                def A_(n):
                    ii, h = n // 8, n % 8
                    i = eg * 4 + ii
                    g = base + n
                    p4 = g % 4
                    pp, ppk = ps_P[p4], f"ps_P{p4}"
                    ph.mm(pp[:], skT[:, 2 * h + 1, :], qT[:, 2 * h + 1, :], True, False, ["skT", "qT"], [ppk])
                    ph.mm(pp[:], skT[:, 2 * h, i:i + 1].to_broadcast([128, 128]), qT[:, 2 * h, :], False, False,
                          ["skT", "qT"], [ppk])
                    ph.mm(pp[:], selm[:, 8 + h, :], rows[:], False, True, ["selm", "rows"], [ppk])
                    ph.act(EE[p4][:], pp[:], AF.Exp, [ppk], [f"EE{p4}"])
                    ph.tt("dve", MK[p4][:], pp[:], theta[:, h, :], ALU.is_ge, [ppk, "theta"], [f"MK{p4}"])

                def B_(n):
                    g = base + n
                    p4, p6 = g % 4, g % 6
                    ph.tt("dve", GH[p6][:], MK[p4][:], EE[p4][:], ALU.mult, [f"MK{p4}", f"EE{p4}"], [f"GH{p6}"])

                def C_(n):
                    ii, h = n // 8, n % 8
                    i = eg * 4 + ii
                    a2 = i % 2
                    p6 = (base + n) % 6
                    ph.mm(ps_q[a2][:], idb[:], GH[p6][:], h == 0, h == 7, ["idb", f"GH{p6}"], [f"ps_q{a2}"])
                    if h == 7:
                        ph.tt("dve", Hg[gs][:, ii, :], gA[ii][:], ps_q[a2][:], ALU.mult, [f"gA{ii}", f"ps_q{a2}"], [f"Hg{gs}"])

                base = it
                it += 32
                for ii in range(4):
                    SA(ii)
                for n in range(32 + 3):
                    if n < 32:
                        A_(n)
                    if 1 <= n <= 32:
                        B_(n - 1)
                    if n >= 3:
                        C_(n - 3)
                for f in range(8):
                    for ii in range(4):
                        ph.mm(ps_O[:], vg[gs][:, ii, f * 128:(f + 1) * 128], Hg[gs][:, ii, :], ii == 0, ii == 3,
                              [f"vg{gs}", f"Hg{gs}"], ["ps_O"])
                    ph.tt("dve", xt[:, f, :], xt[:, f, :], ps_O[:], ALU.add, ["xt0", "ps_O"], ["xt0"])
            for c in range(8):
                ph.act(sq[:, c, :], xt[:, c, :], AF.Square, ["xt0"], ["sq"])
            for c in range(8):
                ph.mm(ps_stat[:], ones32[:], sq[:, c, :], c == 0, c == 7, ["sq", "ones32"], ["ps_q0"])
            ph.act(rt[:], ps_stat[:], AF.Sqrt, ["ps_q0"], ["rt"], bias=EPS, scale=1.0 / 1024)
            ph.recip(rt[:], rt[:], ["rt"], ["rt"])
            for c in range(8):
                ph.stt(sq[:, c, :], xt[:, c, :], gfc[:, c:c + 1], rt[:], ALU.mult, ALU.mult, ["xt0", "rt", "gfc"], ["sq"])
            ph.dma("sp", fm(outT, t0, 512)[:, 0:4, :], sq[:, 0:4, :], ["sq"], ["outT"])
            ph.dma("sp", fm(outT, t0, 512)[:, 4:8, :], sq[:, 4:8, :], ["sq"], ["outT"])


_NC = None


def kernel(**inputs):
    global _NC
    if _NC is None:
        _NC = build()
    maps = prepare(inputs)
    res = run_bass_kernel_spmd(_NC, maps, core_ids=list(range(8)))
    out = np.empty((4, SEQ, D), np.float32)
    for c in range(8):
        b, p = c // 2, c % 2
        o = np.asarray(res.results[c]["outT"]).T
        for j, t in enumerate(tile_ids(p)):
            out[b, t * 512:(t + 1) * 512] = o[j * 512:(j + 1) * 512]
    return out
```
